# Optimizing a Trainium2 kernel written in Bass

```python
import math
import jax, jax.numpy as jnp
from jax import lax
import numpy as np

D_MODEL = 1024
BATCH = 4
SEQ = 8192
DEPTH = 2

GRID_W = 64
HEAD_DIM = 128
ATTN_WIDTH = D_MODEL
N_HEADS = ATTN_WIDTH // HEAD_DIM
N_KV_HEADS = 2
GROUP = N_HEADS // N_KV_HEADS
KV_WIDTH = N_KV_HEADS * HEAD_DIM
Q_BLOCK = 128
ROPE_THETA = 10000.0
ROPE_HALF = HEAD_DIM // 2
POOL_WINDOWS = (2, 4, 8, 16)
N_POOL_GROUPS = 4
POOL_WIDTH = D_MODEL // 2
POOL_GROUP_DIM = POOL_WIDTH // N_POOL_GROUPS
IN_COLS = ATTN_WIDTH + 2 * KV_WIDTH + POOL_WIDTH + 2 * D_MODEL
N_PEER_HEADS = 8
PEER_QUERY_DIM = 512
SUBKEY_DIM = PEER_QUERY_DIM // 2
N_KEYS = 128
N_EXPERTS = N_KEYS * N_KEYS
PEER_TOPK = 16
TOKEN_BLOCK = 128
N_ADA = 6
EPS = 1e-6

kernel_name = "hybrid_gqa_pool_peer_encoder"


def rms_norm(x, w):
    xf = x.astype(jnp.float32)
    y = xf * lax.rsqrt(jnp.mean(xf * xf, axis=-1, keepdims=True) + EPS)
    return (y * w.astype(jnp.float32)).astype(x.dtype)


def modulate(xn, shift, scale):
    return xn * (1.0 + scale[:, None, :]) + shift[:, None, :]


def axial_rope_angles(seq_len):
    rows = seq_len // GRID_W
    t = jnp.arange(rows * GRID_W)
    row = (t // GRID_W).astype(jnp.float32)
    col = (t % GRID_W).astype(jnp.float32)
    inv = 1.0 / (ROPE_THETA ** (jnp.arange(0, ROPE_HALF, 2, dtype=jnp.float32) / ROPE_HALF))
    return row[:, None] * inv, col[:, None] * inv


def rotate_half_dims(xh, ang):
    half = ROPE_HALF // 2
    cos = jnp.cos(ang)[None, :, None, :]
    sin = jnp.sin(ang)[None, :, None, :]
    xf = xh.astype(jnp.float32)
    x1, x2 = xf[..., :half], xf[..., half:]
    return jnp.concatenate([x1 * cos - x2 * sin, x2 * cos + x1 * sin], axis=-1).astype(xh.dtype)


def apply_axial_rope(x, ang_r, ang_c):
    return jnp.concatenate([rotate_half_dims(x[..., :ROPE_HALF], ang_r),
                            rotate_half_dims(x[..., ROPE_HALF:], ang_c)], axis=-1)


def blocked_bidirectional_gqa(q, k, v):
    B, S = q.shape[0], q.shape[1]
    nb = S // Q_BLOCK
    scale = 1.0 / math.sqrt(HEAD_DIM)
    qb = q.reshape(B, nb, Q_BLOCK, N_KV_HEADS, GROUP, HEAD_DIM).swapaxes(0, 1)

    def one_block(qblk):
        s = jnp.einsum('bqkgd,bskd->bkgqs', qblk, k).astype(jnp.float32) * scale
        p = jax.nn.softmax(s, axis=-1).astype(v.dtype)
        return jnp.einsum('bkgqs,bskd->bqkgd', p, v)

    ob = lax.map(one_block, qb)
    return ob.swapaxes(0, 1).reshape(B, S, N_HEADS * HEAD_DIM)


def multiscale_pool(u, pool_mix_w, pool_scale):
    B, S, _ = u.shape
    ug = u.reshape(B, S, N_POOL_GROUPS, POOL_GROUP_DIM).astype(jnp.float32)
    t = jnp.arange(S)
    outs = []
    for gi, w in enumerate(POOL_WINDOWS):
        xg = ug[:, :, gi, :]
        csum = jnp.concatenate([jnp.zeros((B, 1, POOL_GROUP_DIM), jnp.float32),
                                jnp.cumsum(xg, axis=1)], axis=1)
        lo = jnp.clip(t - w // 2, 0, S)
        hi = jnp.clip(t + w // 2, 0, S)
        cnt = (hi - lo).astype(jnp.float32)[None, :, None]
        outs.append((csum[:, hi] - csum[:, lo]) / cnt - xg)
    m = jnp.stack(outs, axis=2).astype(u.dtype)
    mixed = jnp.einsum('bsgc,gcd->bsgd', m, pool_mix_w).reshape(B, S, POOL_WIDTH)
    return mixed * pool_scale


def peer_ffn(xm, w_query, sub_keys, expert_u, expert_v):
    B, S, D = xm.shape
    nb = S // TOKEN_BLOCK
    xb = xm.reshape(B, nb, TOKEN_BLOCK, D).swapaxes(0, 1)

    def one_block(xblk):
        q = (xblk @ w_query).reshape(B, TOKEN_BLOCK, N_PEER_HEADS, 2, SUBKEY_DIM)
        s = jnp.einsum('bthpd,hpnd->bthpn', q, sub_keys).astype(jnp.float32)
        vals, idx = lax.top_k(s, PEER_TOPK)
        cand = (vals[..., 0, :, None] + vals[..., 1, None, :]).reshape(B, TOKEN_BLOCK, N_PEER_HEADS, PEER_TOPK * PEER_TOPK)
        cand_idx = (idx[..., 0, :, None] * N_KEYS + idx[..., 1, None, :]).reshape(B, TOKEN_BLOCK, N_PEER_HEADS, PEER_TOPK * PEER_TOPK)
        top_s, pos = lax.top_k(cand, PEER_TOPK)
        eidx = jnp.take_along_axis(cand_idx, pos, axis=-1)
        g = jax.nn.softmax(top_s, axis=-1).astype(xblk.dtype)
        u = expert_u[eidx]
        h = jnp.einsum('btd,bthkd->bthk', xblk, u)
        act = jax.nn.gelu(h, approximate=False) * g
        return jnp.einsum('bthk,bthkd->btd', act, expert_v[eidx])

    yb = lax.map(one_block, xb)
    return yb.swapaxes(0, 1).reshape(B, S, D)


def setup_inputs(seed: int = 0) -> dict:
    key = jax.random.key(seed)
    ks = jax.random.split(key, 20)
    f32 = jnp.float32
    nrm = lambda k, shape, s: (jax.random.normal(k, shape, f32) * s)
    return {
        "x": nrm(ks[0], (BATCH, SEQ, D_MODEL), 1.0),
        "c": nrm(ks[1], (BATCH, D_MODEL), 1.0),
        "ada_w": nrm(ks[2], (DEPTH, D_MODEL, N_ADA * D_MODEL), 0.5 * D_MODEL ** -0.5),
        "ada_b": nrm(ks[3], (DEPTH, N_ADA * D_MODEL), 0.01),
        "norm_mix_w": 1.0 + nrm(ks[4], (DEPTH, D_MODEL), 0.05),
        "w_in": nrm(ks[5], (DEPTH, D_MODEL, IN_COLS), D_MODEL ** -0.5),
        "q_norm_w": 1.0 + nrm(ks[6], (DEPTH, HEAD_DIM), 0.05),
        "k_norm_w": 1.0 + nrm(ks[7], (DEPTH, HEAD_DIM), 0.05),
        "w_attn_o": nrm(ks[8], (DEPTH, ATTN_WIDTH, D_MODEL), ATTN_WIDTH ** -0.5),
        "pool_mix_w": nrm(ks[9], (DEPTH, N_POOL_GROUPS, POOL_GROUP_DIM, POOL_GROUP_DIM), POOL_GROUP_DIM ** -0.5),
        "pool_scale": 1.0 + nrm(ks[10], (DEPTH, POOL_WIDTH), 0.05),
        "w_pool_o": nrm(ks[11], (DEPTH, POOL_WIDTH, D_MODEL), POOL_WIDTH ** -0.5),
        "w_out": nrm(ks[12], (DEPTH, D_MODEL, D_MODEL), D_MODEL ** -0.5),
        "norm_ffn_w": 1.0 + nrm(ks[13], (DEPTH, D_MODEL), 0.05),
        "w_query": nrm(ks[14], (DEPTH, D_MODEL, N_PEER_HEADS * PEER_QUERY_DIM), D_MODEL ** -0.5),
        "sub_keys": nrm(ks[15], (DEPTH, N_PEER_HEADS, 2, N_KEYS, SUBKEY_DIM), SUBKEY_DIM ** -0.5),
        "expert_u": nrm(ks[16], (DEPTH, N_EXPERTS, D_MODEL), D_MODEL ** -0.5),
        "expert_v": nrm(ks[17], (DEPTH, N_EXPERTS, D_MODEL), N_PEER_HEADS ** -0.5),
        "final_norm_w": 1.0 + nrm(ks[18], (D_MODEL,), 0.05),
    }


def reference(x, c, ada_w, ada_b, norm_mix_w, w_in, q_norm_w, k_norm_w, w_attn_o, pool_mix_w,
              pool_scale, w_pool_o, w_out, norm_ffn_w, w_query, sub_keys, expert_u, expert_v,
              final_norm_w):
    B, S, _ = x.shape
    ang_r, ang_c = axial_rope_angles(S)
    c_act = jax.nn.silu(c)
    splits = [ATTN_WIDTH, ATTN_WIDTH + KV_WIDTH, ATTN_WIDTH + 2 * KV_WIDTH,
              ATTN_WIDTH + 2 * KV_WIDTH + POOL_WIDTH, ATTN_WIDTH + 2 * KV_WIDTH + POOL_WIDTH + D_MODEL]
    for l in range(DEPTH):
        mod = c_act @ ada_w[l] + ada_b[l]
        sh1, sc1, g1, sh2, sc2, g2 = jnp.split(mod, N_ADA, axis=-1)

        xm = modulate(rms_norm(x, norm_mix_w[l]), sh1, sc1)
        proj = xm @ w_in[l]
        q, k, v, u_pool, ga, gp = jnp.split(proj, splits, axis=-1)

        q = rms_norm(q.reshape(B, S, N_HEADS, HEAD_DIM), q_norm_w[l])
        k = rms_norm(k.reshape(B, S, N_KV_HEADS, HEAD_DIM), k_norm_w[l])
        v = v.reshape(B, S, N_KV_HEADS, HEAD_DIM)
        q = apply_axial_rope(q, ang_r, ang_c).reshape(B, S, N_KV_HEADS, GROUP, HEAD_DIM)
        k = apply_axial_rope(k, ang_r, ang_c)
        attn = blocked_bidirectional_gqa(q, k, v) @ w_attn_o[l]

        pool = multiscale_pool(u_pool, pool_mix_w[l], pool_scale[l]) @ w_pool_o[l]

        merged = jax.nn.sigmoid(ga) * attn + jax.nn.sigmoid(gp) * pool
        x = x + g1[:, None, :] * (merged @ w_out[l])

        xf = modulate(rms_norm(x, norm_ffn_w[l]), sh2, sc2)
        x = x + g2[:, None, :] * peer_ffn(xf, w_query[l], sub_keys[l], expert_u[l], expert_v[l])

    return rms_norm(x, final_norm_w)
```

```python
import math
from contextlib import ExitStack
import numpy as np
import concourse.bass as bass
import concourse.mybir as mybir
from concourse.bass_utils import run_bass_kernel_spmd

F32 = mybir.dt.float32
BF16 = mybir.dt.bfloat16
U32 = mybir.dt.uint32
AF = mybir.ActivationFunctionType
ALU = mybir.AluOpType
AX = mybir.AxisListType

D = 1024
NTOK = 4096
SEQ = 8192
EPS = 1e-6
NV = 96
NCORES = 8


class Ev:
    __slots__ = ("op", "sem", "val")

    def __init__(self, op=None, sem=None, val=None):
        self.op, self.sem, self.val = op, sem, val


class Op:
    __slots__ = ("fn", "waits", "inc", "eng", "dsem", "cum", "dinc")

    def __init__(self, fn, eng):
        self.fn, self.eng, self.waits, self.inc, self.dsem, self.cum, self.dinc = fn, eng, [], False, None, None, None


class Buf:
    def __init__(self, name, t=None):
        self.name, self.t = name, t
        self.last_w = None
        self.reads = []
        self.dsem = None
        self.dcount = 0

    def __getitem__(self, k):
        return self.t[k]


ENGS = ("pe", "act", "dve", "pool", "sp")


class Tracker:
    def __init__(self, nc, es):
        self.nc = nc
        self.es = es
        self.esem = {e: es.enter_context(nc.semaphore("sem_" + e)) for e in ENGS}
        self.ebase = {e: 0 for e in ENGS}
        self.ops = {e: [] for e in ENGS}
        self.waited = {e: {} for e in ENGS}
        self.bufs = []
        self.nsem = 0
        self.free = []

    def buf(self, name, t=None, transient=False):
        b = Buf(name, t)
        b.transient = transient
        self.bufs.append(b)
        return b

    def _deps(self, op, reads, writes):
        w = []
        for b in reads:
            if b.last_w is not None:
                w.append(b.last_w)
        for b in writes:
            if b.last_w is not None:
                w.append(b.last_w)
            w.extend(b.reads)
        for ev in w:
            if ev.op is not None:
                if ev.op.eng == "pe" and op.eng == "pe":
                    continue
                ev.op.inc = True
            op.waits.append(ev)

    def emit_interleaved(self, lists):
        its = [list(l) for l in lists]
        for l in its:
            while l:
                kind, args = l.pop(0)
                (self.op if kind == "op" else self.dma)(*args)

    def op(self, eng, fn, reads=(), writes=()):
        if getattr(self, "defer", None) is not None:
            self.defer.append(("op", (eng, fn, tuple(reads), tuple(writes))))
            return None
        o = Op(fn, eng)
        self._deps(o, reads, writes)
        ev = Ev(op=o)
        for b in reads:
            b.reads.append(ev)
        for b in writes:
            b.last_w = ev
            b.reads = []
        self.ops[eng].append(o)
        return o

    def dma(self, eng, out_ap, in_ap, src, dst):
        o = Op(lambda e: e.dma_start(out=out_ap, in_=in_ap), eng)
        self._deps(o, [src], [dst])
        if dst.dsem is None:
            if self.free and eng == "sp":
                dst.dsem, dst.dcount = self.free.pop()
            else:
                dst.dsem = self.es.enter_context(self.nc.semaphore("dsem%d" % self.nsem))
                self.nsem += 1
        dst.dcount += 1
        o.dsem = dst.dsem
        ev = Ev(sem=dst.dsem, val=16 * dst.dcount)
        if not getattr(dst, "background", False):
            src.reads.append(ev)
        dst.last_w = ev
        dst.reads = []
        self.ops[eng].append(o)
        return o

    def cc(self, in_t, out_t, groups, src, dst):
        o = Op(lambda e: e.collective_compute("AllGather", ALU.bypass, replica_groups=groups,
                                              ins=[in_t.ap().opt()], outs=[out_t.ap().opt()]), "pool")
        self._deps(o, [src], [dst])
        sem = self.es.enter_context(self.nc.semaphore("ccsem%d" % self.nsem))
        self.nsem += 1
        o.dsem = sem
        o.dinc = "cc"
        ev = Ev(sem=sem, val=1)
        src.reads.append(ev)
        dst.last_w = ev
        dst.reads = []
        self.ops["pool"].append(o)
        return o

    def flush(self, final_bufs=()):
        fin = Op(lambda e: e.nop(), "sp")
        for b in self.bufs:
            if getattr(b, "background", False):
                continue
            for ev in ([b.last_w] if b.last_w is not None else []) + b.reads:
                if ev.op is None:
                    fin.waits.append(ev)
        self.ops["sp"].append(fin)
        for e in ENGS:
            c = self.ebase[e]
            for o in self.ops[e]:
                if o.inc:
                    c += 1
                o.cum = c
        nc = self
        with self.nc.Block() as block:
            def run(ename, eng):
                waited = self.waited[ename]
                for o in self.ops[ename]:
                    for ev in o.waits:
                        if ev.op is not None:
                            sem, val = self.esem[ev.op.eng], ev.op.cum
                        else:
                            sem, val = ev.sem, ev.val
                        key = id(sem)
                        if waited.get(key, 0) < val:
                            eng.wait_ge(sem, val)
                            waited[key] = val
                    ins = o.fn(eng)
                    if o.dinc == "cc":
                        ins.then_inc(o.dsem)
                    elif o.dsem is not None:
                        ins.then_inc(o.dsem, 16)
                    elif o.inc:
                        ins.then_inc(self.esem[ename], 1)

            @block.sync
            def _(e):
                run("sp", e)

            @block.gpsimd
            def _(e):
                run("pool", e)

            @block.scalar
            def _(e):
                run("act", e)

            @block.vector
            def _(e):
                run("dve", e)

            @block.tensor
            def _(e):
                run("pe", e)
        for e in ENGS:
            self.ebase[e] = self.ops[e][-1].cum if self.ops[e] else self.ebase[e]
            self.ops[e] = []
        if self.ebase["pe"] > 0:
            self.nphase = getattr(self, "nphase", 0) + 1
            self.esem["pe"] = self.es.enter_context(self.nc.semaphore("sem_pe_%d" % self.nphase))
            self.ebase["pe"] = 0
        keep = []
        for b in self.bufs:
            if getattr(b, "background", False):
                b.reads = [ev for ev in b.reads if ev.op is None]
                keep.append(b)
                continue
            b.last_w = None
            b.reads = []
            if b.transient:
                if b.dsem is not None:
                    self.free.append((b.dsem, b.dcount))
            else:
                keep.append(b)
        self.bufs = keep


class Ctx:
    pass


def sb(C, name, shape, dt):
    C.nalloc = getattr(C, "nalloc", 0) + 1
    t = C.es.enter_context(C.nc.sbuf_tensor("sb%d_%s" % (C.nalloc, name), shape, dt))
    return C.T.buf(name, t, transient=(C.es is not C.es0))


def setup_common(nc, es, T):
    C = Ctx()
    C.nc, C.es, C.T = nc, es, T
    C.es0 = es
    C.ps = []
    for i in range(8):
        t = es.enter_context(nc.psum_tensor("ps%d" % i, [128, 512], F32))
        C.ps.append(T.buf("ps%d" % i, t))
    return C


def load_consts(C, vecs_d, consts_d):
    T = C.T
    C.vecs = sb(C, "vecs", [128, NV], F32)
    C.cst = sb(C, "cst", [128, 640], F32)
    C.vecs_d = T.buf("vecs_d")
    T.dma("sp", C.vecs[:], vecs_d[:, :], C.vecs_d, C.vecs)
    T.dma("sp", C.cst[:], consts_d[:, :], C.vecs_d, C.cst)
    C.ones_bf = sb(C, "ones_bf", [128, 128], BF16)
    C.mc = sb(C, "mc", [128, 48], F32)
    T.op("dve", lambda e: e.tensor_copy(out=C.ones_bf[:], in_=C.cst[:, 0:128]), [C.cst], [C.ones_bf])


def load_consts_F(C, vecs_ds, consts_d):
    T = C.T
    C.cst = sb(C, "cst", [128, 640], F32)
    C.vecs_d = T.buf("vecs_d")
    T.dma("sp", C.cst[:], consts_d[:, :], C.vecs_d, C.cst)
    vl = []
    for i, vd in enumerate(vecs_ds):
        v = sb(C, "vecs%d" % i, [128, NV], F32)
        T.dma("sp", v[:], vd[:, :], C.vecs_d, v)
        vl.append(v)
    C.ones_bf = sb(C, "ones_bf", [128, 128], BF16)
    C.mc = sb(C, "mc", [128, 48], F32)
    T.op("dve", lambda e: e.tensor_copy(out=C.ones_bf[:], in_=C.cst[:, 0:128]), [C.cst], [C.ones_bf])
    return vl


def compute_mod(C, ada_w_d):
    T = C.T
    nc = C.nc
    cact = sb(C, "cact", [128, 8], F32)
    T.op("act", lambda e: e.activation(out=cact[:], in_=C.vecs[:, 0:8], func=AF.Silu), [C.vecs], [cact])
    adaw = [sb(C, "adaw%d" % i, [128, 8, 1024], F32) for i in range(2)]
    ada_src = T.buf("ada_src")
    mps = C.ps[7]
    for m in range(6):
        wb = adaw[m % 2]
        T.dma("sp", wb[:], ada_w_d[:, m * 1024:(m + 1) * 1024].rearrange("(k p) n -> p k n", p=128), ada_src, wb)
        for j in range(8):
            jc = m * 8 + j
            for kc in range(8):
                T.op("pe", lambda e, wb=wb, j=j, kc=kc, jc=jc: e.matmul(
                    mps[:, jc:jc + 1], lhsT=wb[:, kc, j * 128:(j + 1) * 128], rhs=cact[:, kc:kc + 1],
                    start=(kc == 0), stop=(kc == 7)), [wb, cact], [mps])
    mod = sb(C, "mod", [128, 48], F32)
    T.op("dve", lambda e: e.tensor_tensor(out=mod[:], in0=mps[:, 0:48], in1=C.vecs[:, 8:56], op=ALU.add), [mps, C.vecs], [mod])
    mc = C.mc
    T.op("dve", lambda e: e.scalar_tensor_tensor(out=mc[:, 0:8], in0=mod[:, 8:16], scalar=1.0, in1=C.vecs[:, 56:64],
                                                 op0=ALU.add, op1=ALU.mult), [mod, C.vecs], [mc])
    T.op("dve", lambda e: e.tensor_copy(out=mc[:, 8:16], in_=mod[:, 0:8]), [mod], [mc])
    T.op("dve", lambda e: e.tensor_copy(out=mc[:, 16:24], in_=mod[:, 16:24]), [mod], [mc])
    T.op("dve", lambda e: e.scalar_tensor_tensor(out=mc[:, 24:32], in0=mod[:, 32:40], scalar=1.0, in1=C.vecs[:, 64:72],
                                                 op0=ALU.add, op1=ALU.mult), [mod, C.vecs], [mc])
    T.op("dve", lambda e: e.tensor_copy(out=mc[:, 32:40], in_=mod[:, 24:32]), [mod], [mc])
    T.op("dve", lambda e: e.tensor_copy(out=mc[:, 40:48], in_=mod[:, 40:48]), [mod], [mc])


def rms_mod_block(C, xt, TB, acol, bcol, sq, tmp, rstd, xm, ss_ps):
    T = C.T
    T.op("act", lambda e: e.activation(out=sq[:], in_=xt[:], func=AF.Square), [xt], [sq])
    for kc in range(8):
        T.op("pe", lambda e, kc=kc: e.matmul(ss_ps[:, 0:TB], lhsT=C.cst[:, 0:128], rhs=sq[:, kc, :],
                                             start=(kc == 0), stop=(kc == 7)), [C.cst, sq], [ss_ps])
    T.op("act", lambda e: e.activation(out=rstd[:], in_=ss_ps[:, 0:TB], func=AF.Sqrt, scale=1.0 / D, bias=C.cst[:, 512:513]),
         [ss_ps, C.cst], [rstd])
    T.op("dve", lambda e: e.reciprocal(out=rstd[:], in_=rstd[:]), [rstd], [rstd])
    T.op("dve", lambda e: e.tensor_tensor(out=tmp[:], in0=xt[:], in1=rstd[:].unsqueeze(1).broadcast_to([128, 8, TB]),
                                          op=ALU.mult), [xt, rstd], [tmp])
    ab, ao = acol
    for kc in range(8):
        if bcol is not None:
            bb, bo = bcol
            T.op("act", lambda e, kc=kc: e.activation(out=xm[:, kc, :], in_=tmp[:, kc, :], func=AF.Identity,
                                                      scale=ab[:, ao + kc:ao + kc + 1], bias=bb[:, bo + kc:bo + kc + 1]),
                 [tmp, ab, bb], [xm])
        else:
            T.op("act", lambda e, kc=kc: e.activation(out=xm[:, kc, :], in_=tmp[:, kc, :], func=AF.Identity,
                                                      scale=ab[:, ao + kc:ao + kc + 1]), [tmp, ab], [xm])


def head_norm_rope(C, qps, TB, wcol, extra, cosb, sinb, hb, out_ap, out_buf, ps_ss, ps_rot):
    T = C.T
    qs, sq, rstd, t1, t2 = hb
    T.op("act", lambda e: e.activation(out=qs[:, 0:TB], in_=qps[:, 0:TB], func=AF.Identity, scale=C.vecs[:, wcol:wcol + 1]),
         [qps, C.vecs], [qs])
    T.op("act", lambda e: e.activation(out=sq[:, 0:TB], in_=qps[:, 0:TB], func=AF.Square), [qps], [sq])
    T.op("pe", lambda e: e.matmul(ps_ss[:, 0:TB], lhsT=C.cst[:, 0:128], rhs=sq[:, 0:TB], start=True, stop=True),
         [C.cst, sq], [ps_ss])
    T.op("pe", lambda e: e.matmul(ps_rot[:, 0:TB], lhsT=C.cst[:, 256:384], rhs=qs[:, 0:TB], start=True, stop=True),
         [C.cst, qs], [ps_rot])
    T.op("act", lambda e: e.activation(out=rstd[:, 0:TB], in_=ps_ss[:, 0:TB], func=AF.Sqrt, scale=1.0 / 128, bias=C.cst[:, 512:513]),
         [ps_ss, C.cst], [rstd])
    T.op("dve", lambda e: e.reciprocal(out=rstd[:, 0:TB], in_=rstd[:, 0:TB]), [rstd], [rstd])
    if extra != 1.0:
        T.op("dve", lambda e: e.tensor_scalar(out=rstd[:, 0:TB], in0=rstd[:, 0:TB], scalar1=float(extra), scalar2=None,
                                              op0=ALU.mult), [rstd], [rstd])
    T.op("pool", lambda e: e.tensor_tensor(out=t1[:, 0:TB], in0=qs[:, 0:TB], in1=cosb[:, 0:TB], op=ALU.mult), [qs, cosb], [t1])
    T.op("dve", lambda e: e.tensor_tensor(out=t2[:, 0:TB], in0=ps_rot[:, 0:TB], in1=sinb[:, 0:TB], op=ALU.mult),
         [ps_rot, sinb], [t2])
    T.op("pool", lambda e: e.tensor_tensor(out=t1[:, 0:TB], in0=t1[:, 0:TB], in1=t2[:, 0:TB], op=ALU.add), [t1, t2], [t1])
    T.op("dve", lambda e: e.tensor_tensor(out=out_ap, in0=t1[:, 0:TB], in1=rstd[:, 0:TB], op=ALU.mult), [t1, rstd], [out_buf])


def alloc_rms_bufs(C, TB, tag=""):
    sq = sb(C, "rsq" + tag, [128, 8, TB], F32)
    tmp = sb(C, "rtmp" + tag, [128, 8, TB], F32)
    rstd = sb(C, "rrstd" + tag, [128, TB], F32)
    return sq, tmp, rstd


def alloc_head_bufs(C, TB, tag=""):
    return [sb(C, "hb%d%s" % (i, tag), [128, TB], F32) for i in range(5)]


def build_A():
    nc = bass.Bass("TRN2", target_bir_lowering=False)
    xT_d = nc.dram_tensor("xT", [D, NTOK], F32, kind="ExternalInput").ap()
    vecs_d = nc.dram_tensor("vecs", [128, NV], F32, kind="ExternalInput").ap()
    consts_d = nc.dram_tensor("consts", [128, 640], F32, kind="ExternalInput").ap()
    ada_w_d = nc.dram_tensor("ada_w", [D, 6 * D], F32, kind="ExternalInput").ap()
    w_in_d = nc.dram_tensor("w_in", [D, 4096], F32, kind="ExternalInput").ap()
    cos_d = nc.dram_tensor("cosT", [128, NTOK], F32, kind="ExternalInput").ap()
    sin_d = nc.dram_tensor("sinT", [128, NTOK], F32, kind="ExternalInput").ap()
    kT_d = nc.dram_tensor("kT", [256, NTOK], BF16, kind="ExternalOutput").ap()
    v_d = nc.dram_tensor("v", [NTOK, 256], BF16, kind="ExternalOutput").ap()
    uh_d = nc.dram_tensor("uh", [512, 16], F32, kind="ExternalOutput").ap()
    TB = 512
    with ExitStack() as es:
        T = Tracker(nc, es)
        C = setup_common(nc, es, T)
        load_consts(C, vecs_d, consts_d)
        compute_mod(C, ada_w_d)
        T.flush()
        wsrc = T.buf("wsrc")
        wk = sb(C, "wkvp", [128, 8, 1024], BF16)
        for kc in range(8):
            T.dma("pool", wk[:, kc, :], w_in_d[kc * 128:(kc + 1) * 128, 1024:2048], wsrc, wk)
        xsrc = T.buf("xsrc")
        kout, vout, uout = T.buf("kout"), T.buf("vout"), T.buf("uout")
        xts = [sb(C, "xt%d" % i, [128, 8, TB], F32) for i in range(2)]
        cosb = [sb(C, "cos%d" % i, [128, TB], F32) for i in range(2)]
        sinb = [sb(C, "sin%d" % i, [128, TB], F32) for i in range(2)]
        sq, tmp, rstd = alloc_rms_bufs(C, TB)
        xm = sb(C, "xm", [128, 8, TB], BF16)
        hb = alloc_head_bufs(C, TB)
        kst = [sb(C, "kst%d" % i, [128, TB], BF16) for i in range(2)]
        vst = [sb(C, "vst%d" % i, [128, 256], BF16) for i in range(2)]
        ust = sb(C, "ust", [128, 4, 8], F32)
        nblk = NTOK // TB

        def load_blk(b):
            t0 = b * TB
            T.dma("sp", xts[b % 2][:], xT_d[:, t0:t0 + TB].rearrange("(k p) t -> p k t", p=128), xsrc, xts[b % 2])
            T.dma("sp", cosb[b % 2][:], cos_d[:, t0:t0 + TB], xsrc, cosb[b % 2])
            T.dma("sp", sinb[b % 2][:], sin_d[:, t0:t0 + TB], xsrc, sinb[b % 2])

        load_blk(0)
        for b in range(nblk):
            t0 = b * TB
            if b + 1 < nblk:
                load_blk(b + 1)
            xt = xts[b % 2]
            rms_mod_block(C, xt, TB, (C.mc, 0), (C.mc, 8), sq, tmp, rstd, xm, C.ps[0])
            for g in range(2):
                qps = C.ps[1 + g]
                for kc in range(8):
                    T.op("pe", lambda e, kc=kc, g=g, qps=qps: e.matmul(qps[:, 0:TB], lhsT=wk[:, kc, g * 128:(g + 1) * 128],
                                                                       rhs=xm[:, kc, :], start=(kc == 0), stop=(kc == 7)),
                         [wk, xm], [qps])
                ko = kst[g]
                head_norm_rope(C, qps, TB, 81, 1.0, cosb[b % 2], sinb[b % 2], hb, ko[:], ko, C.ps[3], C.ps[4])
                T.dma("pool", kT_d[g * 128:(g + 1) * 128, t0:t0 + TB], ko[:], ko, kout)
            for tt in range(TB // 128):
                vps = C.ps[5 + (tt % 2)]
                for kc in range(8):
                    T.op("pe", lambda e, kc=kc, tt=tt, vps=vps: e.matmul(vps[:, 0:256], lhsT=xm[:, kc, tt * 128:(tt + 1) * 128],
                                                                         rhs=wk[:, kc, 256:512], start=(kc == 0), stop=(kc == 7)),
                         [wk, xm], [vps])
                vo = vst[tt % 2]
                T.op("act", lambda e, vo=vo, vps=vps: e.copy(out=vo[:], in_=vps[:, 0:256]), [vps], [vo])
                T.dma("pool", v_d[t0 + tt * 128:t0 + (tt + 1) * 128, :], vo[:], vo, vout)
            if b == 0 or b == nblk - 1:
                for g in range(4):
                    ups = C.ps[7]
                    for kc in range(8):
                        T.op("pe", lambda e, kc=kc, g=g, ups=ups: e.matmul(ups[:, 0:TB], lhsT=wk[:, kc, 512 + g * 128:512 + (g + 1) * 128],
                                                                           rhs=xm[:, kc, :], start=(kc == 0), stop=(kc == 7)),
                             [wk, xm], [ups])
                    lo = 0 if b == 0 else TB - 8
                    T.op("act", lambda e, g=g, ups=ups, lo=lo: e.copy(out=ust[:, g, :], in_=ups[:, lo:lo + 8]), [ups], [ust])
                o0 = 0 if b == 0 else 8
                T.dma("pool", uh_d[:, o0:o0 + 8].rearrange("(g p) t -> p g t", p=128), ust[:], ust, uout)
        T.flush()
    return nc


def phase(C):
    class _P:
        def __enter__(self_):
            self_.es = ExitStack()
            self_.es.__enter__()
            C.es = self_.es
            return self_

        def __exit__(self_, *a):
            if a[0] is None:
                C.T.flush()
            C.es = C.es0
            return self_.es.__exit__(*a)
    return _P()


def build_B(stop_after=99, dbg_sel='x1'):
    nc = bass.Bass("TRN2", target_bir_lowering=False)
    def din(name, shape, dt=F32):
        return nc.dram_tensor(name, shape, dt, kind="ExternalInput").ap()
    xT_d = din("xT", [D, NTOK])
    vecs_d = din("vecs", [128, NV])
    consts_d = din("consts", [128, 640])
    ada_w_d = din("ada_w", [D, 6 * D])
    w_in_d = din("w_in", [D, 4096])
    cos_d = din("cosT", [128, NTOK])
    sin_d = din("sinT", [128, NTOK])
    kT_d = din("kTf", [256, SEQ], BF16)
    v_d = din("vf", [SEQ, 256], BF16)
    uhalo_d = din("uhalo", [512, 16])
    invcnt_d = din("invcnt", [128, 4 * NTOK])
    wao_d = din("w_attn_o", [D, D])
    wpo_d = din("w_pool_o", [512, D])
    wout_d = din("w_out", [D, D])
    mixw_d = din("pool_mix_w", [512, 128])
    wq_d = din("w_query", [D, 4096])
    skt_d = din("skt", [4096, 128])
    ut_d = din("expert_uT", [D, 16384])
    ev_d = din("expert_v", [16384, D])
    xo_d = nc.dram_tensor("xT_out", [D, NTOK], F32, kind="ExternalOutput").ap()
    xn_d = nc.dram_tensor("xTn_out", [D, NTOK], F32, kind="ExternalOutput").ap()
    dbg_d = nc.dram_tensor("dbg", [D, NTOK], F32, kind="ExternalOutput").ap()
    def scr(name, shape, dt):
        return nc.dram_tensor(name, shape, dt).ap()
    qT_s = scr("qT_s", [D, NTOK], BF16)
    gT_s = scr("gT_s", [2048, NTOK], BF16)
    uT_s = scr("uT_s", [512, NTOK + 16], F32)
    pg_s = scr("pg_s", [D, NTOK], F32)
    x1_s = scr("x1_s", [D, NTOK], F32)
    xf_s = scr("xf_s", [D, NTOK], BF16)
    lt_s = scr("lt_s", [3 * 128, NTOK], F32)
    utb_s = scr("utb_s", [D, 16384], BF16)
    evb_s = scr("evb_s", [16384, D], BF16)

    with ExitStack() as es:
        T = Tracker(nc, es)
        C = setup_common(nc, es, T)
        ps = C.ps
        src = T.buf("ext_src")
        qT_b, gT_b, uT_b, pg_b, x1_b, xf_b, lt_b, utb_b, evb_b = [T.buf(n) for n in
            ("qT_b", "gT_b", "uT_b", "pg_b", "x1_b", "xf_b", "lt_b", "utb_b", "evb_b")]
        xo_b, xn_b, dbg_b = T.buf("xo_b"), T.buf("xn_b"), T.buf("dbg_b")
        load_consts(C, vecs_d, consts_d)

        def dump_dbg():
            with phase(C):
                if dbg_sel == 'mc':
                    T.dma("sp", dbg_d[0:128, 0:48], C.mc[:], C.mc, dbg_b)
                elif dbg_sel == 'q':
                    T.dma("pool", dbg_d[:, :], qT_s[:, :], qT_b, dbg_b)
                elif dbg_sel == 'g':
                    T.dma("pool", dbg_d[:, :], gT_s[0:1024, :], gT_b, dbg_b)
                elif dbg_sel == 'gp':
                    T.dma("pool", dbg_d[:, :], gT_s[1024:2048, :], gT_b, dbg_b)
                elif dbg_sel == 'u':
                    T.dma("pool", dbg_d[0:512, :], uT_s[:, 8:8 + NTOK], uT_b, dbg_b)
                elif dbg_sel == 'pg':
                    T.dma("pool", dbg_d[:, :], pg_s[:, :], pg_b, dbg_b)
                else:
                    T.dma("pool", dbg_d[:, :], x1_s[:, :], x1_b, dbg_b)

        with phase(C):
            compute_mod(C, ada_w_d)
            for r in range(8):
                T.dma("pool", utb_s[r * 128:(r + 1) * 128, :], ut_d[r * 128:(r + 1) * 128, :], src, utb_b)
            for r in range(16):
                T.dma("pool", evb_s[r * 1024:(r + 1) * 1024, :], ev_d[r * 1024:(r + 1) * 1024, :], src, evb_b)
            T.dma("sp", uT_s[:, 0:8], uhalo_d[:, 0:8], src, uT_b)
            T.dma("sp", uT_s[:, NTOK + 8:NTOK + 16], uhalo_d[:, 8:16], src, uT_b)
        if stop_after <= 0:
            dump_dbg()
            return nc

        TB = 512
        nblk = NTOK // TB
        with phase(C):
            win = sb(C, "win", [128, 8, 4096], BF16)
            for kc in range(8):
                T.dma("pool", win[:, kc, :], w_in_d[kc * 128:(kc + 1) * 128, :], src, win)
            xts = [sb(C, "xt%d" % i, [128, 8, TB], F32) for i in range(2)]
            cosb = [sb(C, "cos%d" % i, [128, TB], F32) for i in range(2)]
            sinb = [sb(C, "sin%d" % i, [128, TB], F32) for i in range(2)]
            sq, tmp, rstd = alloc_rms_bufs(C, TB)
            xm = sb(C, "xm", [128, 8, TB], BF16)
            hb = alloc_head_bufs(C, TB)
            qst = [sb(C, "qst%d" % i, [128, TB], BF16) for i in range(2)]
            gst = [sb(C, "gst%d" % i, [128, TB], BF16) for i in range(2)]
            ust = [sb(C, "ust%d" % i, [128, TB], F32) for i in range(2)]

            def load_blk(b):
                t0 = b * TB
                T.dma("sp", xts[b % 2][:], xT_d[:, t0:t0 + TB].rearrange("(k p) t -> p k t", p=128), src, xts[b % 2])
                T.dma("sp", cosb[b % 2][:], cos_d[:, t0:t0 + TB], src, cosb[b % 2])
                T.dma("sp", sinb[b % 2][:], sin_d[:, t0:t0 + TB], src, sinb[b % 2])

            load_blk(0)
            for b in range(nblk):
                t0 = b * TB
                if b + 1 < nblk:
                    load_blk(b + 1)
                rms_mod_block(C, xts[b % 2], TB, (C.mc, 0), (C.mc, 8), sq, tmp, rstd, xm, ps[0])
                for h in range(8):
                    qps = ps[1 + h % 2]
                    for kc in range(8):
                        T.op("pe", lambda e, kc=kc, h=h, qps=qps: e.matmul(qps[:, 0:TB], lhsT=win[:, kc, h * 128:(h + 1) * 128],
                                                                           rhs=xm[:, kc, :], start=(kc == 0), stop=(kc == 7)),
                             [win, xm], [qps])
                    qo = qst[h % 2]
                    head_norm_rope(C, qps, TB, 80, 1.0 / math.sqrt(128.0), cosb[b % 2], sinb[b % 2], hb, qo[:], qo, ps[3], ps[4])
                    T.dma("pool", qT_s[h * 128:(h + 1) * 128, t0:t0 + TB], qo[:], qo, qT_b)
                for gc in range(16):
                    gps = ps[5 + gc % 2]
                    c0 = 2048 + gc * 128
                    for kc in range(8):
                        T.op("pe", lambda e, kc=kc, c0=c0, gps=gps: e.matmul(gps[:, 0:TB], lhsT=win[:, kc, c0:c0 + 128],
                                                                             rhs=xm[:, kc, :], start=(kc == 0), stop=(kc == 7)),
                             [win, xm], [gps])
                    go = gst[gc % 2]
                    T.op("act", lambda e, go=go, gps=gps: e.activation(out=go[:], in_=gps[:, 0:TB], func=AF.Sigmoid), [gps], [go])
                    T.dma("pool", gT_s[gc * 128:(gc + 1) * 128, t0:t0 + TB], go[:], go, gT_b)
                for g in range(4):
                    ups = ps[7]
                    c0 = 1536 + g * 128
                    for kc in range(8):
                        T.op("pe", lambda e, kc=kc, c0=c0, ups=ups: e.matmul(ups[:, 0:TB], lhsT=win[:, kc, c0:c0 + 128],
                                                                             rhs=xm[:, kc, :], start=(kc == 0), stop=(kc == 7)),
                             [win, xm], [ups])
                    uo = ust[g % 2]
                    T.op("dve", lambda e, uo=uo, ups=ups: e.tensor_copy(out=uo[:], in_=ups[:, 0:TB]), [ups], [uo])
                    T.dma("pool", uT_s[g * 128:(g + 1) * 128, 8 + t0:8 + t0 + TB], uo[:], uo, uT_b)
        if stop_after <= 1:
            dump_dbg()
            return nc

        with phase(C):
            mixw = sb(C, "mixw", [128, 4, 128], BF16)
            T.dma("pool", mixw[:], mixw_d.rearrange("(g p) n -> p g n", p=128), src, mixw)
            wpo = sb(C, "wpo", [128, 4, 1024], BF16)
            T.dma("pool", wpo[:], wpo_d.rearrange("(g p) n -> p g n", p=128), src, wpo)
            HB = TB + 16
            ub = [sb(C, "ub%d" % i, [128, 4, HB], F32) for i in range(2)]
            icb = [sb(C, "icb%d" % i, [128, 4, TB], F32) for i in range(2)]
            w2 = sb(C, "w2", [128, 4, HB], F32)
            w4 = sb(C, "w4", [128, 4, HB], F32)
            w8 = sb(C, "w8", [128, 4, HB], F32)
            w16 = sb(C, "w16", [128, HB], F32)
            mt = sb(C, "mt", [128, 4, TB], BF16)
            mtf = sb(C, "mtf", [128, 4, TB], F32)
            mx = sb(C, "mx", [128, 4, TB], BF16)
            gpb = [sb(C, "gpb%d" % i, [128, TB], BF16) for i in range(2)]
            pgo = [sb(C, "pgo%d" % i, [128, TB], F32) for i in range(2)]

            def load_u(b):
                t0 = b * TB
                T.dma("sp", ub[b % 2][:], uT_s[:, t0:t0 + HB].rearrange("(g p) t -> p g t", p=128), uT_b, ub[b % 2])
                T.dma("sp", icb[b % 2][:], invcnt_d.rearrange("p (g t) -> p g t", g=4)[:, :, t0:t0 + TB], src, icb[b % 2])

            load_u(0)
            for b in range(nblk):
                t0 = b * TB
                if b + 1 < nblk:
                    load_u(b + 1)
                u = ub[b % 2]
                ic = icb[b % 2]
                T.op("dve", lambda e, u=u: e.tensor_tensor(out=w2[:, :, 1:HB], in0=u[:, :, 0:HB - 1], in1=u[:, :, 1:HB], op=ALU.add), [u], [w2])
                T.op("dve", lambda e: e.tensor_tensor(out=w4[:, 1:4, 2:HB - 1], in0=w2[:, 1:4, 1:HB - 2], in1=w2[:, 1:4, 3:HB], op=ALU.add), [w2], [w4])
                T.op("dve", lambda e: e.tensor_tensor(out=w8[:, 2:4, 4:HB - 3], in0=w4[:, 2:4, 2:HB - 5], in1=w4[:, 2:4, 6:HB - 1], op=ALU.add), [w4], [w8])
                T.op("dve", lambda e: e.tensor_tensor(out=w16[:, 8:HB - 7], in0=w8[:, 3, 4:HB - 11], in1=w8[:, 3, 12:HB - 3], op=ALU.add), [w8], [w16])
                wins = [w2[:, 0, 8:8 + TB], w4[:, 1, 8:8 + TB], w8[:, 2, 8:8 + TB], w16[:, 8:8 + TB]]
                wbufs = [w2, w4, w8, w16]
                for g in range(4):
                    T.op("dve", lambda e, g=g, ic=ic: e.tensor_tensor(out=mtf[:, g, :], in0=wins[g], in1=ic[:, g, :], op=ALU.mult), [wbufs[g], ic], [mtf])
                    T.op("dve", lambda e, g=g, u=u: e.tensor_tensor(out=mt[:, g, :], in0=mtf[:, g, :], in1=u[:, g, 8:8 + TB], op=ALU.subtract), [mtf, u], [mt])
                    mps = ps[g % 2]
                    T.op("pe", lambda e, g=g, mps=mps: e.matmul(mps[:, 0:TB], lhsT=mixw[:, g, :], rhs=mt[:, g, :], start=True, stop=True), [mixw, mt], [mps])
                    T.op("act", lambda e, g=g, mps=mps: e.activation(out=mx[:, g, :], in_=mps[:, 0:TB], func=AF.Identity,
                                                                     scale=C.vecs[:, 82 + g:83 + g]), [mps, C.vecs], [mx])
                for dc in range(8):
                    pps = ps[2 + dc % 2]
                    gb = gpb[dc % 2]
                    T.dma("sp", gb[:], gT_s[1024 + dc * 128:1024 + (dc + 1) * 128, t0:t0 + TB], gT_b, gb)
                    for g in range(4):
                        T.op("pe", lambda e, g=g, dc=dc, pps=pps: e.matmul(pps[:, 0:TB], lhsT=wpo[:, g, dc * 128:(dc + 1) * 128],
                                                                           rhs=mx[:, g, :], start=(g == 0), stop=(g == 3)), [wpo, mx], [pps])
                    po = pgo[dc % 2]
                    T.op("dve", lambda e, po=po, pps=pps, gb=gb: e.tensor_tensor(out=po[:], in0=pps[:, 0:TB], in1=gb[:], op=ALU.mult), [pps, gb], [po])
                    T.dma("pool", pg_s[dc * 128:(dc + 1) * 128, t0:t0 + TB], po[:], po, pg_b)
        if stop_after <= 2:
            dump_dbg()
            return nc

        with phase(C):
            kt = sb(C, "kt", [128, 2, SEQ], BF16)
            for g in range(2):
                for hf in range(2):
                    T.dma("sp", kt[:, g, hf * 4096:(hf + 1) * 4096], kT_d[g * 128:(g + 1) * 128, hf * 4096:(hf + 1) * 4096], src, kt)
            vt = sb(C, "vt", [128, 64, 256], BF16)
            for q4 in range(4):
                T.dma("sp", vt[:, q4 * 16:(q4 + 1) * 16, :], v_d[q4 * 2048:(q4 + 1) * 2048, :].rearrange("(k p) n -> p k n", p=128), src, vt)
            wao = sb(C, "wao", [128, 8, 1024], BF16)
            wout = sb(C, "wout", [128, 8, 1024], BF16)
            for kc in range(8):
                T.dma("pool", wao[:, kc, :], wao_d[kc * 128:(kc + 1) * 128, :], src, wao)
                T.dma("pool", wout[:, kc, :], wout_d[kc * 128:(kc + 1) * 128, :], src, wout)
            qtb = [sb(C, "qtb%d" % i, [128, 8, TB], BF16) for i in range(2)]
            OT = sb(C, "OT", [128, 8, TB], BF16)
            mg = sb(C, "mg", [128, 8, TB], BF16)
            mgf = [sb(C, "mgf%d" % i, [128, TB], F32) for i in range(2)]
            pt = [sb(C, "pt%d" % i, [128, TB], BF16) for i in range(3)]
            rec = sb(C, "rec", [128, TB], F32)
            gab = [sb(C, "gab%d" % i, [128, TB], BF16) for i in range(2)]
            pgb = [sb(C, "pgb%d" % i, [128, TB], F32) for i in range(2)]
            xb = [sb(C, "xb%d" % i, [128, TB], F32) for i in range(2)]
            xob = [sb(C, "xob%d" % i, [128, TB], F32) for i in range(2)]

            def load_q(b):
                T.dma("sp", qtb[b % 2][:], qT_s[:, b * TB:(b + 1) * TB].rearrange("(h p) t -> p h t", p=128), qT_b, qtb[b % 2])

            load_q(0)
            for b in range(nblk):
                t0 = b * TB
                if b + 1 < nblk:
                    load_q(b + 1)
                qb = qtb[b % 2]
                for h in range(8):
                    g = h // 4
                    ops_, zps = ps[2 + h % 2], ps[4 + h % 2]
                    NKC = SEQ // 128

                    def s_mm(kc, h=h, g=g, qb=qb):
                        sp_ = ps[kc % 2]
                        T.op("pe", lambda e: e.matmul(sp_[:, 0:TB], lhsT=kt[:, g, kc * 128:(kc + 1) * 128], rhs=qb[:, h, :],
                                                      start=True, stop=True), [kt, qb], [sp_])
                    s_mm(0)
                    for kc in range(NKC):
                        if kc + 1 < NKC:
                            s_mm(kc + 1)
                        sp_ = ps[kc % 2]
                        p_ = pt[kc % 3]
                        T.op("act", lambda e, sp_=sp_, p_=p_: e.activation(out=p_[:], in_=sp_[:, 0:TB], func=AF.Exp), [sp_], [p_])
                        T.op("pe", lambda e, kc=kc, g=g, p_=p_, ops_=ops_: e.matmul(ops_[:, 0:TB], lhsT=vt[:, kc, g * 128:(g + 1) * 128], rhs=p_[:],
                                                                                   start=(kc == 0), stop=(kc == NKC - 1)), [vt, p_], [ops_])
                        T.op("pe", lambda e, kc=kc, p_=p_, zps=zps: e.matmul(zps[:, 0:TB], lhsT=C.ones_bf[:], rhs=p_[:],
                                                                            start=(kc == 0), stop=(kc == NKC - 1)), [C.ones_bf, p_], [zps])
                    T.op("dve", lambda e, zps=zps: e.reciprocal(out=rec[:], in_=zps[:, 0:TB]), [zps], [rec])
                    T.op("dve", lambda e, h=h, ops_=ops_: e.tensor_tensor(out=OT[:, h, :], in0=ops_[:, 0:TB], in1=rec[:], op=ALU.mult), [ops_, rec], [OT])
                for dc in range(8):
                    aps = ps[6 + dc % 2]
                    ga, pgt = gab[dc % 2], pgb[dc % 2]
                    T.dma("sp", ga[:], gT_s[dc * 128:(dc + 1) * 128, t0:t0 + TB], gT_b, ga)
                    T.dma("sp", pgt[:], pg_s[dc * 128:(dc + 1) * 128, t0:t0 + TB], pg_b, pgt)
                    for h in range(8):
                        T.op("pe", lambda e, h=h, dc=dc, aps=aps: e.matmul(aps[:, 0:TB], lhsT=wao[:, h, dc * 128:(dc + 1) * 128], rhs=OT[:, h, :],
                                                                           start=(h == 0), stop=(h == 7)), [wao, OT], [aps])
                    mf = mgf[dc % 2]
                    T.op("dve", lambda e, aps=aps, ga=ga, mf=mf: e.tensor_tensor(out=mf[:], in0=aps[:, 0:TB], in1=ga[:], op=ALU.mult), [aps, ga], [mf])
                    T.op("pool", lambda e, dc=dc, mf=mf, pgt=pgt: e.tensor_tensor(out=mg[:, dc, :], in0=mf[:], in1=pgt[:], op=ALU.add), [mf, pgt], [mg])
                for dc in range(8):
                    yps = ps[6 + dc % 2]
                    xi, xo = xb[dc % 2], xob[dc % 2]
                    T.dma("sp", xi[:], xT_d[dc * 128:(dc + 1) * 128, t0:t0 + TB], src, xi)
                    for k in range(8):
                        T.op("pe", lambda e, k=k, dc=dc, yps=yps: e.matmul(yps[:, 0:TB], lhsT=wout[:, k, dc * 128:(dc + 1) * 128], rhs=mg[:, k, :],
                                                                           start=(k == 0), stop=(k == 7)), [wout, mg], [yps])
                    T.op("dve", lambda e, dc=dc, yps=yps, xi=xi, xo=xo: e.scalar_tensor_tensor(
                        out=xo[:], in0=yps[:, 0:TB], scalar=C.mc[:, 16 + dc:17 + dc], in1=xi[:], op0=ALU.mult, op1=ALU.add), [yps, xi, C.mc], [xo])
                    T.dma("pool", x1_s[dc * 128:(dc + 1) * 128, t0:t0 + TB], xo[:], xo, x1_b)
        if stop_after <= 3:
            dump_dbg()
            return nc

        TA = 256
        nblka = NTOK // TA
        with phase(C):
            wq = sb(C, "wq", [128, 8, 4096], BF16)
            for kc in range(8):
                T.dma("pool", wq[:, kc, :], wq_d[kc * 128:(kc + 1) * 128, :], src, wq)
            skt = sb(C, "skt", [128, 32, 128], BF16)
            T.dma("pool", skt[:], skt_d.rearrange("(c p) n -> p c n", p=128), src, skt)
            xts = [sb(C, "xt%d" % i, [128, 8, TA], F32) for i in range(2)]
            sq, tmp, rstd = alloc_rms_bufs(C, TA)
            xf = sb(C, "xf", [128, 8, TA], BF16)
            qp = sb(C, "qp", [128, 32, TA], BF16)
            S = sb(C, "S", [128, 16, 128], F32)
            v16 = sb(C, "v16", [128, 16, 16], F32)
            i16 = sb(C, "i16", [128, 16, 16], U32)
            idxf = sb(C, "idxf", [128, 16, 16], F32)
            tmpS = sb(C, "tmpS", [128, 128], F32)
            cand = sb(C, "cand", [128, 8, 256], F32)
            tmpC = sb(C, "tmpC", [128, 256], F32)
            tv = sb(C, "tv", [128, 8, 16], F32)
            pos = sb(C, "pos", [128, 8, 16], U32)
            posf = sb(C, "posf", [128, 128], F32)
            pa = sb(C, "pa", [128, 128], F32)
            pb = sb(C, "pb", [128, 128], F32)
            big = sb(C, "big", [128, 2048], F32)
            big2 = sb(C, "big2", [128, 2048], F32)
            Il = sb(C, "Il", [128, 128], F32)
            Jl = sb(C, "Jl", [128, 128], F32)
            ee = sb(C, "ee", [128, 128], F32)
            zz = sb(C, "zz", [128, 8], F32)
            gl = sb(C, "gl", [128, 128], F32)
            LT = sb(C, "LT", [128, 3, TA], F32)

            def b4(t, off, dims):
                tt_ = t.t
                pstep = 1
                for d_ in tt_.shape[1:]:
                    pstep *= d_
                return bass.AP(tt_, off, [[pstep, 128]] + dims)

            def load_x1(b):
                T.dma("sp", xts[b % 2][:], x1_s[:, b * TA:(b + 1) * TA].rearrange("(k p) t -> p k t", p=128), x1_b, xts[b % 2])

            load_x1(0)
            for b in range(nblka):
                t0 = b * TA
                if b + 1 < nblka:
                    load_x1(b + 1)
                rms_mod_block(C, xts[b % 2], TA, (C.mc, 24), (C.mc, 32), sq, tmp, rstd, xf, ps[7])
                T.dma("pool", xf_s[:, t0:t0 + TA].rearrange("(k p) t -> p k t", p=128), xf[:], xf, xf_b)
                for cc in range(32):
                    qps = ps[4 + cc % 2]
                    for kc in range(8):
                        T.op("pe", lambda e, kc=kc, cc=cc, qps=qps: e.matmul(qps[:, 0:TA], lhsT=wq[:, kc, cc * 128:(cc + 1) * 128], rhs=xf[:, kc, :],
                                                                             start=(kc == 0), stop=(kc == 7)), [wq, xf], [qps])
                    if cc % 2 == 0:
                        T.op("act", lambda e, cc=cc, qps=qps: e.copy(out=qp[:, cc, :], in_=qps[:, 0:TA]), [qps], [qp])
                    else:
                        T.op("dve", lambda e, cc=cc, qps=qps: e.tensor_copy(out=qp[:, cc, :], in_=qps[:, 0:TA]), [qps], [qp])
                for tt in range(TA // 128):
                    for hp in range(16):
                        sps = ps[hp // 4]
                        for dk in range(2):
                            T.op("pe", lambda e, hp=hp, dk=dk, tt=tt, sps=sps: e.matmul(
                                sps[:, (hp % 4) * 128:(hp % 4 + 1) * 128], lhsT=qp[:, hp * 2 + dk, tt * 128:(tt + 1) * 128],
                                rhs=skt[:, hp * 2 + dk, :], start=(dk == 0), stop=(dk == 1)), [qp, skt], [sps])
                    for bk in range(4):
                        T.op("act", lambda e, bk=bk: e.copy(out=S[:, bk * 4:(bk + 1) * 4, :], in_=ps[bk][:, 0:512].rearrange("p (a n) -> p a n", a=4)),
                             [ps[bk]], [S])
                    for hp in range(16):
                        T.op("dve", lambda e, hp=hp: e.max(out=v16[:, hp, 0:8], in_=S[:, hp, :]), [S], [v16])
                        T.op("dve", lambda e, hp=hp: e.max_index(out=i16[:, hp, 0:8], in_max=v16[:, hp, 0:8], in_values=S[:, hp, :]), [S, v16], [i16])
                        T.op("dve", lambda e, hp=hp: e.match_replace(out=tmpS[:], in_to_replace=v16[:, hp, 0:8], in_values=S[:, hp, :], imm_value=-1e30),
                             [S, v16], [tmpS])
                        T.op("dve", lambda e, hp=hp: e.max(out=v16[:, hp, 8:16], in_=tmpS[:]), [tmpS], [v16])
                        T.op("dve", lambda e, hp=hp: e.max_index(out=i16[:, hp, 8:16], in_max=v16[:, hp, 8:16], in_values=tmpS[:]), [tmpS, v16], [i16])
                    T.op("dve", lambda e: e.tensor_copy(out=idxf[:], in_=i16[:]), [i16], [idxf])
                    T.op("dve", lambda e: e.tensor_tensor(out=b4(cand, 0, [[256, 8], [16, 16], [1, 16]]),
                                                          in0=b4(v16, 0, [[32, 8], [1, 16], [0, 16]]),
                                                          in1=b4(v16, 16, [[32, 8], [0, 16], [1, 16]]), op=ALU.add), [v16], [cand])
                    for h in range(8):
                        T.op("dve", lambda e, h=h: e.max(out=tv[:, h, 0:8], in_=cand[:, h, :]), [cand], [tv])
                        T.op("dve", lambda e, h=h: e.max_index(out=pos[:, h, 0:8], in_max=tv[:, h, 0:8], in_values=cand[:, h, :]), [cand, tv], [pos])
                        T.op("dve", lambda e, h=h: e.match_replace(out=tmpC[:], in_to_replace=tv[:, h, 0:8], in_values=cand[:, h, :], imm_value=-1e30),
                             [cand, tv], [tmpC])
                        T.op("dve", lambda e, h=h: e.max(out=tv[:, h, 8:16], in_=tmpC[:]), [tmpC], [tv])
                        T.op("dve", lambda e, h=h: e.max_index(out=pos[:, h, 8:16], in_max=tv[:, h, 8:16], in_values=tmpC[:]), [tmpC, tv], [pos])
                    T.op("dve", lambda e: e.tensor_copy(out=posf[:], in_=pos[:].rearrange("p h k -> p (h k)")), [pos], [posf])
                    T.op("dve", lambda e: e.tensor_tensor(out=b4(big, 0, [[16, 128], [1, 16]]), in0=b4(posf, 0, [[1, 128], [0, 16]]),
                                                          in1=bass.AP(C.cst.t, 529, [[640, 128], [0, 128], [1, 16]]), op=ALU.is_ge), [posf, C.cst], [big])
                    T.op("dve", lambda e: e.tensor_reduce(out=pa[:], in_=b4(big, 0, [[16, 128], [1, 16]]), axis=AX.X, op=ALU.add), [big], [pa])
                    T.op("dve", lambda e: e.scalar_tensor_tensor(out=pb[:], in0=pa[:], scalar=-16.0, in1=posf[:], op0=ALU.mult, op1=ALU.add), [pa, posf], [pb])
                    for (pp, poff, outl) in ((pa, 0, Il), (pb, 16, Jl)):
                        T.op("dve", lambda e, pp=pp: e.tensor_tensor(out=b4(big, 0, [[16, 128], [1, 16]]), in0=b4(pp, 0, [[1, 128], [0, 16]]),
                                                                     in1=bass.AP(C.cst.t, 513, [[640, 128], [0, 128], [1, 16]]), op=ALU.is_equal), [pp, C.cst], [big])
                        T.op("dve", lambda e, poff=poff: e.tensor_tensor(out=b4(big2, 0, [[256, 8], [16, 16], [1, 16]]), in0=b4(big, 0, [[256, 8], [16, 16], [1, 16]]),
                                                                         in1=b4(idxf, poff, [[32, 8], [0, 16], [1, 16]]), op=ALU.mult), [big, idxf], [big2])
                        T.op("dve", lambda e, outl=outl: e.tensor_reduce(out=outl[:], in_=b4(big2, 0, [[16, 128], [1, 16]]), axis=AX.X, op=ALU.add), [big2], [outl])
                    T.op("dve", lambda e: e.tensor_tensor(out=ee[:].rearrange("p (h k) -> p h k", h=8), in0=tv[:],
                                                          in1=b4(tv, 0, [[16, 8], [0, 16]]), op=ALU.subtract), [tv], [ee])
                    T.op("act", lambda e: e.activation(out=ee[:], in_=ee[:], func=AF.Exp), [ee], [ee])
                    T.op("dve", lambda e: e.tensor_reduce(out=zz[:], in_=ee[:].rearrange("p (h k) -> p h k", h=8), axis=AX.X, op=ALU.add), [ee], [zz])
                    T.op("dve", lambda e: e.reciprocal(out=zz[:], in_=zz[:]), [zz], [zz])
                    T.op("dve", lambda e: e.tensor_tensor(out=gl[:].rearrange("p (h k) -> p h k", h=8), in0=ee[:].rearrange("p (h k) -> p h k", h=8),
                                                          in1=b4(zz, 0, [[1, 8], [0, 16]]), op=ALU.mult), [ee, zz], [gl])
                    for li, lst in enumerate((Il, Jl, gl)):
                        tps = ps[5 + li]
                        T.op("pe", lambda e, lst=lst, tps=tps: e.transpose(out=tps[:, 0:128], in_=lst[:], identity=C.cst[:, 128:256]), [lst, C.cst], [tps])
                        T.op("act", lambda e, li=li, tt=tt, tps=tps: e.copy(out=LT[:, li, tt * 128:(tt + 1) * 128], in_=tps[:, 0:128]), [tps], [LT])
                T.dma("pool", lt_s[:, t0:t0 + TA].rearrange("(l p) t -> p l t", p=128), LT[:], LT, lt_b)
        if stop_after <= 4:
            with phase(C):
                cp = sb(C, "cp", [128, 3, NTOK], F32)
                T.dma("sp", cp[:], lt_s.rearrange("(l p) t -> p l t", p=128), lt_b, cp)
                T.dma("sp", dbg_d[0:384, :].rearrange("(l p) t -> p l t", p=128), cp[:], cp, dbg_b)
            return nc

        TP = 256
        with phase(C):
            wall = sb(C, "wall", [128, TP, 128], BF16)
            xfb = [sb(C, "xfb%d" % i, [128, 8, TP], BF16) for i in range(2)]
            ltb = [sb(C, "ltb%d" % i, [128, 3, TP], F32) for i in range(2)]
            x1b = [sb(C, "x1b%d" % i, [128, 8, TP], F32) for i in range(2)]
            NR = 8
            At = [sb(C, "At%d" % i, [128, 128], BF16) for i in range(NR)]
            Bt = [sb(C, "Bt%d" % i, [128, 128], BF16) for i in range(NR)]
            NW = 3
            utt = [sb(C, "utt%d" % i, [128, 8, 512], BF16) for i in range(NW)]
            evt = [sb(C, "evt%d" % i, [128, 4, 1024], BF16) for i in range(NW)]
            gel = [sb(C, "gel%d" % i, [128, TP], F32) for i in range(2)]
            abf = [sb(C, "abf%d" % i, [128, TP], BF16) for i in range(3)]
            x2 = sb(C, "x2", [128, 8, TP], F32)
            sq, tmp, rstd = alloc_rms_bufs(C, TP, "f")
            xnb = sb(C, "xnb", [128, 8, TP], F32)
            npb = NTOK // TP

            def load_pb(b):
                t0 = b * TP
                T.dma("sp", xfb[b % 2][:], xf_s[:, t0:t0 + TP].rearrange("(k p) t -> p k t", p=128), xf_b, xfb[b % 2])
                T.dma("sp", ltb[b % 2][:], lt_s[:, t0:t0 + TP].rearrange("(l p) t -> p l t", p=128), lt_b, ltb[b % 2])
                T.dma("sp", x1b[b % 2][:], x1_s[:, t0:t0 + TP].rearrange("(k p) t -> p k t", p=128), x1_b, x1b[b % 2])

            def load_w(gi):
                e0 = gi * 512
                T.dma("sp", utt[gi % NW][:], utb_s[:, e0:e0 + 512].rearrange("(k p) e -> p k e", p=128), utb_b, utt[gi % NW])
                T.dma("sp", evt[gi % NW][:], evb_s[e0:e0 + 512, :].rearrange("(c p) n -> p c n", p=128), evb_b, evt[gi % NW])

            load_pb(0)
            for b in range(npb):
                t0 = b * TP
                if b + 1 < npb:
                    load_pb(b + 1)
                xfq, lt, x1q = xfb[b % 2], ltb[b % 2], x1b[b % 2]
                load_w(0)
                load_w(1)
                for t in range(TP):
                    a_, b_ = At[t % NR], Bt[t % NR]
                    T.op("pool", lambda e, a_=a_, t=t, lt=lt: e.tensor_scalar(out=a_[:], in0=C.cst[:, 384:512], scalar1=lt[:, 0, t:t + 1], scalar2=None,
                                                                               op0=ALU.is_equal), [C.cst, lt], [a_])
                    T.op("dve", lambda e, b_=b_, t=t, lt=lt: e.tensor_scalar(out=b_[:], in0=C.cst[:, 384:512], scalar1=lt[:, 1, t:t + 1], scalar2=lt[:, 2, t:t + 1],
                                                                              op0=ALU.is_equal, op1=ALU.mult), [C.cst, lt], [b_])
                    wps = ps[6 + (t // 4) % 2]
                    T.op("pe", lambda e, a_=a_, b_=b_, t=t, wps=wps: e.matmul(wps[:, (t % 4) * 128:(t % 4 + 1) * 128], lhsT=b_[:], rhs=a_[:], start=True, stop=True),
                         [a_, b_], [wps])
                    if t % 4 == 3:
                        T.op("act", lambda e, t=t, wps=wps: e.copy(out=wall[:, t - 3:t + 1, :], in_=wps[:, 0:512].rearrange("p (a n) -> p a n", a=4)), [wps], [wall])
                def h_mm(i, xfq=xfq):
                    hps = ps[4 + i % 2]
                    ub_ = utt[(i // 4) % NW]
                    for kc in range(8):
                        T.op("pe", lambda e, kc=kc, i=i, hps=hps, ub_=ub_: e.matmul(hps[:, 0:TP], lhsT=ub_[:, kc, (i % 4) * 128:(i % 4 + 1) * 128], rhs=xfq[:, kc, :],
                                                                                    start=(kc == 0), stop=(kc == 7)), [ub_, xfq], [hps])
                h_mm(0)
                for i in range(128):
                    if i % 4 == 0 and i // 4 + 2 < 32:
                        load_w(i // 4 + 2)
                    if i + 1 < 128:
                        h_mm(i + 1)
                    hps = ps[4 + i % 2]
                    ge, ab = gel[i % 2], abf[i % 3]
                    T.op("act", lambda e, hps=hps, ge=ge: e.activation(out=ge[:], in_=hps[:, 0:TP], func=AF.Gelu), [hps], [ge])
                    meng = "dve" if i % 2 == 0 else "pool"
                    T.op(meng, lambda e, i=i, ge=ge, ab=ab: e.tensor_tensor(out=ab[:], in0=ge[:], in1=wall[:, :, i], op=ALU.mult), [ge, wall], [ab])
                    eb_ = evt[(i // 4) % NW]
                    for dc in range(8):
                        ops_ = ps[dc // 2]
                        T.op("pe", lambda e, dc=dc, i=i, ab=ab, eb_=eb_, ops_=ops_: e.matmul(
                            ops_[:, (dc % 2) * 256:(dc % 2) * 256 + TP], lhsT=eb_[:, i % 4, dc * 128:(dc + 1) * 128], rhs=ab[:],
                            start=(i == 0 and dc % 2 == 0), stop=(i == 127)), [eb_, ab], [ops_])
                for dc in range(8):
                    ops_ = ps[dc // 2]
                    T.op("dve", lambda e, dc=dc, ops_=ops_, x1q=x1q: e.scalar_tensor_tensor(
                        out=x2[:, dc, :], in0=ops_[:, (dc % 2) * 256:(dc % 2) * 256 + TP], scalar=C.mc[:, 40 + dc:41 + dc], in1=x1q[:, dc, :],
                        op0=ALU.mult, op1=ALU.add), [ops_, x1q, C.mc], [x2])
                T.dma("pool", xo_d[:, t0:t0 + TP].rearrange("(k p) t -> p k t", p=128), x2[:], x2, xo_b)
                rms_mod_block(C, x2, TP, (C.vecs, 72), None, sq, tmp, rstd, xnb, ps[4])
                T.dma("pool", xn_d[:, t0:t0 + TP].rearrange("(k p) t -> p k t", p=128), xnb[:], xnb, xn_b)
    return nc


def build_F():
    stop_after = 99
    nc = bass.Bass("TRN2", target_bir_lowering=False)
    def din(name, shape, dt=F32):
        return nc.dram_tensor(name, shape, dt, kind="ExternalInput").ap()
    xT_ext = din("xT", [D, NTOK])
    consts_d = din("consts", [128, 640])
    cos_d = din("cosT", [128, NTOK])
    sin_d = din("sinT", [128, NTOK])
    invcnt_d = din("invcnt", [128, 4 * NTOK])
    LW = []
    for l in range(2):
        LW.append(dict(
            vecs=din("vecs%d" % l, [128, NV]), ada_w=din("ada_w%d" % l, [D, 6 * D]), w_in=din("w_in%d" % l, [D, 4096]),
            wao=din("w_attn_o%d" % l, [D, D]), wpo=din("w_pool_o%d" % l, [512, D]), wout=din("w_out%d" % l, [D, D]),
            mixw=din("pool_mix_w%d" % l, [512, 128]), wq=din("w_query%d" % l, [D, 4096]), skt=din("skt%d" % l, [4096, 128]),
            ut=din("expert_uT%d" % l, [D, 16384]), ev=din("expert_v%d" % l, [16384, D])))
    xn_d = nc.dram_tensor("xTn_out", [D, NTOK], F32, kind="ExternalOutput").ap()
    def scr(name, shape, dt):
        return nc.dram_tensor(name, shape, dt).ap()
    qT_s = scr("qT_s", [D, NTOK], BF16)
    gT_s = scr("gT_s", [2048, NTOK], BF16)
    uT_s = scr("uT_s", [512, NTOK + 16], F32)
    pg_s = scr("pg_s", [D, NTOK], F32)
    x1_s = scr("x1_s", [D, NTOK], F32)
    xf_s = scr("xf_s", [D, NTOK], BF16)
    lt_s = scr("lt_s", [3 * 128, NTOK], F32)
    utb_s = scr("utb_s", [D, 16384], BF16)
    evb_s = scr("evb_s", [16384, D], BF16)
    xcur_s = scr("xcur_s", [D, NTOK], F32)
    kx_in = nc.dram_tensor("kx_in", [256, NTOK], BF16)
    kx_out = nc.dram_tensor("kx_out", [512, NTOK], BF16)
    vx_in = nc.dram_tensor("vx_in", [NTOK, 256], BF16)
    vx_out = nc.dram_tensor("vx_out", [SEQ, 256], BF16)
    ux_in = nc.dram_tensor("ux_in", [512, 16], F32)
    ux_out = nc.dram_tensor("ux_out", [1024, 16], F32)
    PAIRS = [[0, 1], [2, 3], [4, 5], [6, 7]]

    with ExitStack() as es:
        T = Tracker(nc, es)
        C = setup_common(nc, es, T)
        ps = C.ps
        src = T.buf("ext_src")
        qT_b, gT_b, uT_b, pg_b, x1_b, xf_b, lt_b, utb_b, evb_b = [T.buf(n) for n in
            ("qT_b", "gT_b", "uT_b", "pg_b", "x1_b", "xf_b", "lt_b", "utb_b", "evb_b")]
        xn_b, xcur_b = T.buf("xn_b"), T.buf("xcur_b")
        kx_b, vx_b, ux_b, kxo_b, vxo_b, uxo_b = [T.buf(n) for n in ("kx_b", "vx_b", "ux_b", "kxo_b", "vxo_b", "uxo_b")]
        vecs_l = load_consts_F(C, [LW[0]["vecs"], LW[1]["vecs"]], consts_d)
        utb_b.background = True
        evb_b.background = True

        def layer(l):
            W = LW[l]
            ada_w_d, w_in_d, wao_d, wpo_d, wout_d = W["ada_w"], W["w_in"], W["wao"], W["wpo"], W["wout"]
            mixw_d, wq_d, skt_d, ut_d, ev_d = W["mixw"], W["wq"], W["skt"], W["ut"], W["ev"]
            C.vecs = vecs_l[l]
            x_in_d, x_in_b = (xT_ext, src) if l == 0 else (xcur_s, xcur_b)
            last = (l == 1)
            with phase(C):
                compute_mod(C, ada_w_d)
                for r in range(8):
                    T.dma("pool", utb_s[r * 128:(r + 1) * 128, :], ut_d[r * 128:(r + 1) * 128, :], src, utb_b)
                for r in range(16):
                    T.dma("pool", evb_s[r * 1024:(r + 1) * 1024, :], ev_d[r * 1024:(r + 1) * 1024, :], src, evb_b)

            TB = 512
            nblk = NTOK // TB
            with phase(C):
                win = sb(C, "win", [128, 8, 4096], BF16)
                for kc in range(8):
                    T.dma("pool", win[:, kc, :], w_in_d[kc * 128:(kc + 1) * 128, :], src, win)
                xts = [sb(C, "xt%d" % i, [128, 8, TB], F32) for i in range(2)]
                cosb = [sb(C, "cos%d" % i, [128, TB], F32) for i in range(2)]
                sinb = [sb(C, "sin%d" % i, [128, TB], F32) for i in range(2)]
                sq, tmp, rstd = alloc_rms_bufs(C, TB)
                xm = sb(C, "xm", [128, 8, TB], BF16)
                hb = alloc_head_bufs(C, TB)
                qst = [sb(C, "qst%d" % i, [128, TB], BF16) for i in range(2)]
                gst = [sb(C, "gst%d" % i, [128, TB], BF16) for i in range(2)]
                ust = [sb(C, "ust%d" % i, [128, TB], F32) for i in range(2)]
                vst = [sb(C, "vst%d" % i, [128, 256], BF16) for i in range(2)]

                def load_blk(b):
                    t0 = b * TB
                    T.dma("sp", xts[b % 2][:], x_in_d[:, t0:t0 + TB].rearrange("(k p) t -> p k t", p=128), x_in_b, xts[b % 2])
                    T.dma("sp", cosb[b % 2][:], cos_d[:, t0:t0 + TB], src, cosb[b % 2])
                    T.dma("sp", sinb[b % 2][:], sin_d[:, t0:t0 + TB], src, sinb[b % 2])

                load_blk(0)
                for b in range(nblk):
                    t0 = b * TB
                    if b + 1 < nblk:
                        load_blk(b + 1)
                    rms_mod_block(C, xts[b % 2], TB, (C.mc, 0), (C.mc, 8), sq, tmp, rstd, xm, ps[0])
                    for h in range(8):
                        qps = ps[1 + h % 2]
                        for kc in range(8):
                            T.op("pe", lambda e, kc=kc, h=h, qps=qps: e.matmul(qps[:, 0:TB], lhsT=win[:, kc, h * 128:(h + 1) * 128],
                                                                               rhs=xm[:, kc, :], start=(kc == 0), stop=(kc == 7)),
                                 [win, xm], [qps])
                        qo = qst[h % 2]
                        head_norm_rope(C, qps, TB, 80, 1.0 / math.sqrt(128.0), cosb[b % 2], sinb[b % 2], hb, qo[:], qo, ps[3], ps[4])
                        T.dma("pool", qT_s[h * 128:(h + 1) * 128, t0:t0 + TB], qo[:], qo, qT_b)
                    for g in range(2):
                        qps = ps[1 + g % 2]
                        for kc in range(8):
                            T.op("pe", lambda e, kc=kc, g=g, qps=qps: e.matmul(qps[:, 0:TB], lhsT=win[:, kc, 1024 + g * 128:1024 + (g + 1) * 128],
                                                                               rhs=xm[:, kc, :], start=(kc == 0), stop=(kc == 7)),
                                 [win, xm], [qps])
                        ko = qst[g % 2]
                        head_norm_rope(C, qps, TB, 81, 1.0, cosb[b % 2], sinb[b % 2], hb, ko[:], ko, ps[3], ps[4])
                        T.dma("pool", kx_in.ap()[g * 128:(g + 1) * 128, t0:t0 + TB], ko[:], ko, kx_b)
                    for tt in range(TB // 128):
                        vps = ps[5 + tt % 2]
                        for kc in range(8):
                            T.op("pe", lambda e, kc=kc, tt=tt, vps=vps: e.matmul(vps[:, 0:256], lhsT=xm[:, kc, tt * 128:(tt + 1) * 128],
                                                                                 rhs=win[:, kc, 1280:1536], start=(kc == 0), stop=(kc == 7)),
                                 [win, xm], [vps])
                        vo = vst[tt % 2]
                        T.op("act", lambda e, vo=vo, vps=vps: e.copy(out=vo[:], in_=vps[:, 0:256]), [vps], [vo])
                        T.dma("pool", vx_in.ap()[t0 + tt * 128:t0 + (tt + 1) * 128, :], vo[:], vo, vx_b)
                    for gc in range(16):
                        gps = ps[5 + gc % 2]
                        c0 = 2048 + gc * 128
                        for kc in range(8):
                            T.op("pe", lambda e, kc=kc, c0=c0, gps=gps: e.matmul(gps[:, 0:TB], lhsT=win[:, kc, c0:c0 + 128],
                                                                                 rhs=xm[:, kc, :], start=(kc == 0), stop=(kc == 7)),
                                 [win, xm], [gps])
                        go = gst[gc % 2]
                        T.op("act", lambda e, go=go, gps=gps: e.activation(out=go[:], in_=gps[:, 0:TB], func=AF.Sigmoid), [gps], [go])
                        T.dma("pool", gT_s[gc * 128:(gc + 1) * 128, t0:t0 + TB], go[:], go, gT_b)
                    for g in range(4):
                        ups = ps[7]
                        c0 = 1536 + g * 128
                        for kc in range(8):
                            T.op("pe", lambda e, kc=kc, c0=c0, ups=ups: e.matmul(ups[:, 0:TB], lhsT=win[:, kc, c0:c0 + 128],
                                                                                 rhs=xm[:, kc, :], start=(kc == 0), stop=(kc == 7)),
                                 [win, xm], [ups])
                        uo = ust[g % 2]
                        T.op("dve", lambda e, uo=uo, ups=ups: e.tensor_copy(out=uo[:], in_=ups[:, 0:TB]), [ups], [uo])
                        T.dma("pool", uT_s[g * 128:(g + 1) * 128, 8 + t0:8 + t0 + TB], uo[:], uo, uT_b)
                        if b == 0:
                            T.dma("pool", ux_in.ap()[g * 128:(g + 1) * 128, 0:8], uo[:, 0:8], uo, ux_b)
                        if b == nblk - 1:
                            T.dma("pool", ux_in.ap()[g * 128:(g + 1) * 128, 8:16], uo[:, TB - 8:TB], uo, ux_b)

            with phase(C):
                T.cc(kx_in, kx_out, PAIRS, kx_b, kxo_b)
                T.cc(vx_in, vx_out, PAIRS, vx_b, vxo_b)
                T.cc(ux_in, ux_out, PAIRS, ux_b, uxo_b)
                hl = sb(C, "hl", [128, 4, 8], F32)
                hh = sb(C, "hh", [128, 4, 8], F32)
                T.dma("sp", hl[:], ux_out.ap()[0:512, 8:16].rearrange("(g p) t -> p g t", p=128), uxo_b, hl)
                T.dma("sp", hh[:], ux_out.ap()[512:1024, 0:8].rearrange("(g p) t -> p g t", p=128), uxo_b, hh)
                T.op("dve", lambda e: e.tensor_scalar(out=hl[:], in0=hl[:], scalar1=C.vecs[:, 86:87], scalar2=None, op0=ALU.mult), [hl, C.vecs], [hl])
                T.op("dve", lambda e: e.tensor_scalar(out=hh[:], in0=hh[:], scalar1=C.vecs[:, 87:88], scalar2=None, op0=ALU.mult), [hh, C.vecs], [hh])
                T.dma("pool", uT_s[:, 0:8].rearrange("(g p) t -> p g t", p=128), hl[:], hl, uT_b)
                T.dma("pool", uT_s[:, NTOK + 8:NTOK + 16].rearrange("(g p) t -> p g t", p=128), hh[:], hh, uT_b)

            with phase(C):
                mixw = sb(C, "mixw", [128, 4, 128], BF16)
                T.dma("pool", mixw[:], mixw_d.rearrange("(g p) n -> p g n", p=128), src, mixw)
                wpo = sb(C, "wpo", [128, 4, 1024], BF16)
                T.dma("pool", wpo[:], wpo_d.rearrange("(g p) n -> p g n", p=128), src, wpo)
                HB = TB + 16
                ub = [sb(C, "ub%d" % i, [128, 4, HB], F32) for i in range(2)]
                icb = [sb(C, "icb%d" % i, [128, 4, TB], F32) for i in range(2)]
                w2 = sb(C, "w2", [128, 4, HB], F32)
                w4 = sb(C, "w4", [128, 4, HB], F32)
                w8 = sb(C, "w8", [128, 4, HB], F32)
                w16 = sb(C, "w16", [128, HB], F32)
                mt = sb(C, "mt", [128, 4, TB], BF16)
                mtf = sb(C, "mtf", [128, 4, TB], F32)
                mx = sb(C, "mx", [128, 4, TB], BF16)
                gpb = [sb(C, "gpb%d" % i, [128, TB], BF16) for i in range(2)]
                pgo = [sb(C, "pgo%d" % i, [128, TB], F32) for i in range(2)]

                def load_u(b):
                    t0 = b * TB
                    T.dma("sp", ub[b % 2][:], uT_s[:, t0:t0 + HB].rearrange("(g p) t -> p g t", p=128), uT_b, ub[b % 2])
                    T.dma("sp", icb[b % 2][:], invcnt_d.rearrange("p (g t) -> p g t", g=4)[:, :, t0:t0 + TB], src, icb[b % 2])

                load_u(0)
                for b in range(nblk):
                    t0 = b * TB
                    if b + 1 < nblk:
                        load_u(b + 1)
                    u = ub[b % 2]
                    ic = icb[b % 2]
                    T.op("dve", lambda e, u=u: e.tensor_tensor(out=w2[:, :, 1:HB], in0=u[:, :, 0:HB - 1], in1=u[:, :, 1:HB], op=ALU.add), [u], [w2])
                    T.op("dve", lambda e: e.tensor_tensor(out=w4[:, 1:4, 2:HB - 1], in0=w2[:, 1:4, 1:HB - 2], in1=w2[:, 1:4, 3:HB], op=ALU.add), [w2], [w4])
                    T.op("dve", lambda e: e.tensor_tensor(out=w8[:, 2:4, 4:HB - 3], in0=w4[:, 2:4, 2:HB - 5], in1=w4[:, 2:4, 6:HB - 1], op=ALU.add), [w4], [w8])
                    T.op("dve", lambda e: e.tensor_tensor(out=w16[:, 8:HB - 7], in0=w8[:, 3, 4:HB - 11], in1=w8[:, 3, 12:HB - 3], op=ALU.add), [w8], [w16])
                    wins = [w2[:, 0, 8:8 + TB], w4[:, 1, 8:8 + TB], w8[:, 2, 8:8 + TB], w16[:, 8:8 + TB]]
                    wbufs = [w2, w4, w8, w16]
                    for g in range(4):
                        T.op("dve", lambda e, g=g, ic=ic: e.tensor_tensor(out=mtf[:, g, :], in0=wins[g], in1=ic[:, g, :], op=ALU.mult), [wbufs[g], ic], [mtf])
                        T.op("dve", lambda e, g=g, u=u: e.tensor_tensor(out=mt[:, g, :], in0=mtf[:, g, :], in1=u[:, g, 8:8 + TB], op=ALU.subtract), [mtf, u], [mt])
                        mps = ps[g % 2]
                        T.op("pe", lambda e, g=g, mps=mps: e.matmul(mps[:, 0:TB], lhsT=mixw[:, g, :], rhs=mt[:, g, :], start=True, stop=True), [mixw, mt], [mps])
                        T.op("act", lambda e, g=g, mps=mps: e.activation(out=mx[:, g, :], in_=mps[:, 0:TB], func=AF.Identity,
                                                                         scale=C.vecs[:, 82 + g:83 + g]), [mps, C.vecs], [mx])
                    for dc in range(8):
                        pps = ps[2 + dc % 2]
                        gb = gpb[dc % 2]
                        T.dma("sp", gb[:], gT_s[1024 + dc * 128:1024 + (dc + 1) * 128, t0:t0 + TB], gT_b, gb)
                        for g in range(4):
                            T.op("pe", lambda e, g=g, dc=dc, pps=pps: e.matmul(pps[:, 0:TB], lhsT=wpo[:, g, dc * 128:(dc + 1) * 128],
                                                                               rhs=mx[:, g, :], start=(g == 0), stop=(g == 3)), [wpo, mx], [pps])
                        po = pgo[dc % 2]
                        T.op("dve", lambda e, po=po, pps=pps, gb=gb: e.tensor_tensor(out=po[:], in0=pps[:, 0:TB], in1=gb[:], op=ALU.mult), [pps, gb], [po])
                        T.dma("pool", pg_s[dc * 128:(dc + 1) * 128, t0:t0 + TB], po[:], po, pg_b)

            with phase(C):
                kt = sb(C, "kt", [128, 2, SEQ], BF16)
                for g in range(2):
                    for hf in range(2):
                        T.dma("sp", kt[:, g, hf * 4096:(hf + 1) * 4096], kx_out.ap()[hf * 256 + g * 128:hf * 256 + (g + 1) * 128, :], kxo_b, kt)
                vt = sb(C, "vt", [128, 64, 256], BF16)
                for q4 in range(4):
                    T.dma("sp", vt[:, q4 * 16:(q4 + 1) * 16, :], vx_out.ap()[q4 * 2048:(q4 + 1) * 2048, :].rearrange("(k p) n -> p k n", p=128), vxo_b, vt)
                wao = sb(C, "wao", [128, 8, 1024], BF16)
                wout = sb(C, "wout", [128, 8, 1024], BF16)
                for kc in range(8):
                    T.dma("pool", wao[:, kc, :], wao_d[kc * 128:(kc + 1) * 128, :], src, wao)
                    T.dma("pool", wout[:, kc, :], wout_d[kc * 128:(kc + 1) * 128, :], src, wout)
                qtb = [sb(C, "qtb%d" % i, [128, 8, TB], BF16) for i in range(2)]
                OT = sb(C, "OT", [128, 8, TB], BF16)
                mg = sb(C, "mg", [128, 8, TB], BF16)
                mgf = [sb(C, "mgf%d" % i, [128, TB], F32) for i in range(2)]
                pt = [sb(C, "pt%d" % i, [128, TB], BF16) for i in range(4)]
                rec = sb(C, "rec", [128, TB], F32)
                zacc = [sb(C, "zacc%d" % i, [128, TB], F32) for i in range(2)]
                gab = [sb(C, "gab%d" % i, [128, TB], BF16) for i in range(2)]
                pgb = [sb(C, "pgb%d" % i, [128, TB], F32) for i in range(2)]
                xb = [sb(C, "xb%d" % i, [128, TB], F32) for i in range(2)]
                xob = [sb(C, "xob%d" % i, [128, TB], F32) for i in range(2)]

                def load_q(b):
                    T.dma("sp", qtb[b % 2][:], qT_s[:, b * TB:(b + 1) * TB].rearrange("(h p) t -> p h t", p=128), qT_b, qtb[b % 2])

                load_q(0)
                for b in range(nblk):
                    t0 = b * TB
                    if b + 1 < nblk:
                        load_q(b + 1)
                    qb = qtb[b % 2]
                    SB = (0, 1, 4)
                    for h in range(8):
                        g = h // 4
                        ops_, zps = ps[2 + h % 2], ps[5]
                        NKC = SEQ // 128

                        def s_mm(kc, h=h, g=g, qb=qb):
                            sp_ = ps[SB[kc % 3]]
                            T.op("pe", lambda e: e.matmul(sp_[:, 0:TB], lhsT=kt[:, g, kc * 128:(kc + 1) * 128], rhs=qb[:, h, :],
                                                          start=True, stop=True), [kt, qb], [sp_])
                        s_mm(0)
                        s_mm(1)
                        for kc in range(NKC):
                            if kc + 2 < NKC:
                                s_mm(kc + 2)
                            sp_ = ps[SB[kc % 3]]
                            p_ = pt[kc % 4]
                            T.op("act", lambda e, sp_=sp_, p_=p_: e.activation(out=p_[:], in_=sp_[:, 0:TB], func=AF.Exp), [sp_], [p_])
                            T.op("pe", lambda e, kc=kc, g=g, p_=p_, ops_=ops_: e.matmul(ops_[:, 0:TB], lhsT=vt[:, kc, g * 128:(g + 1) * 128], rhs=p_[:],
                                                                                       start=(kc == 0), stop=(kc == NKC - 1)), [vt, p_], [ops_])
                            za = zacc[0]
                            if kc % 2 == 1:
                                T.op("pe", lambda e, kc=kc, p_=p_, zps=zps: e.matmul(zps[:, 0:TB], lhsT=C.ones_bf[:], rhs=p_[:],
                                                                                    start=(kc == 1), stop=False), [C.ones_bf, p_], [zps])
                            elif kc == 0:
                                T.op("dve", lambda e, p_=p_, za=za: e.tensor_copy(out=za[:], in_=p_[:]), [p_], [za])
                            else:
                                T.op("dve", lambda e, p_=p_, za=za: e.tensor_tensor(out=za[:], in0=za[:], in1=p_[:], op=ALU.add), [p_, za], [za])
                        T.op("pe", lambda e, zps=zps: e.matmul(zps[:, 0:TB], lhsT=C.cst[:, 0:128], rhs=zacc[0][:], start=False, stop=True), [C.cst, zacc[0]], [zps])
                        T.op("dve", lambda e, zps=zps: e.reciprocal(out=rec[:], in_=zps[:, 0:TB]), [zps], [rec])
                        T.op("dve", lambda e, h=h, ops_=ops_: e.tensor_tensor(out=OT[:, h, :], in0=ops_[:, 0:TB], in1=rec[:], op=ALU.mult), [ops_, rec], [OT])
                    for dc in range(8):
                        aps = ps[6 + dc % 2]
                        ga, pgt = gab[dc % 2], pgb[dc % 2]
                        T.dma("sp", ga[:], gT_s[dc * 128:(dc + 1) * 128, t0:t0 + TB], gT_b, ga)
                        T.dma("sp", pgt[:], pg_s[dc * 128:(dc + 1) * 128, t0:t0 + TB], pg_b, pgt)
                        for h in range(8):
                            T.op("pe", lambda e, h=h, dc=dc, aps=aps: e.matmul(aps[:, 0:TB], lhsT=wao[:, h, dc * 128:(dc + 1) * 128], rhs=OT[:, h, :],
                                                                               start=(h == 0), stop=(h == 7)), [wao, OT], [aps])
                        mf = mgf[dc % 2]
                        T.op("dve", lambda e, aps=aps, ga=ga, mf=mf: e.tensor_tensor(out=mf[:], in0=aps[:, 0:TB], in1=ga[:], op=ALU.mult), [aps, ga], [mf])
                        T.op("pool", lambda e, dc=dc, mf=mf, pgt=pgt: e.tensor_tensor(out=mg[:, dc, :], in0=mf[:], in1=pgt[:], op=ALU.add), [mf, pgt], [mg])
                    for dc in range(8):
                        yps = ps[6 + dc % 2]
                        xi, xo = xb[dc % 2], xob[dc % 2]
                        T.dma("sp", xi[:], x_in_d[dc * 128:(dc + 1) * 128, t0:t0 + TB], x_in_b, xi)
                        for k in range(8):
                            T.op("pe", lambda e, k=k, dc=dc, yps=yps: e.matmul(yps[:, 0:TB], lhsT=wout[:, k, dc * 128:(dc + 1) * 128], rhs=mg[:, k, :],
                                                                               start=(k == 0), stop=(k == 7)), [wout, mg], [yps])
                        T.op("dve", lambda e, dc=dc, yps=yps, xi=xi, xo=xo: e.scalar_tensor_tensor(
                            out=xo[:], in0=yps[:, 0:TB], scalar=C.mc[:, 16 + dc:17 + dc], in1=xi[:], op0=ALU.mult, op1=ALU.add), [yps, xi, C.mc], [xo])
                        T.dma("pool", x1_s[dc * 128:(dc + 1) * 128, t0:t0 + TB], xo[:], xo, x1_b)

            TA = 256
            nblka = NTOK // TA
            with phase(C):
                wq = sb(C, "wq", [128, 8, 4096], BF16)
                for kc in range(8):
                    T.dma("pool", wq[:, kc, :], wq_d[kc * 128:(kc + 1) * 128, :], src, wq)
                skt = sb(C, "skt", [128, 32, 128], BF16)
                T.dma("pool", skt[:], skt_d.rearrange("(c p) n -> p c n", p=128), src, skt)
                xts = [sb(C, "xt%d" % i, [128, 8, TA], F32) for i in range(2)]
                sq, tmp, rstd = alloc_rms_bufs(C, TA)
                xf = sb(C, "xf", [128, 8, TA], BF16)
                qp = sb(C, "qp", [128, 32, TA], BF16)
                def alloc_set(k):
                    return dict(
                        S=sb(C, "S_%d" % k, [128, 16, 128], F32),
                        v16=sb(C, "v16_%d" % k, [128, 16, 16], F32),
                        i16=sb(C, "i16_%d" % k, [128, 16, 16], U32),
                        idxf=sb(C, "idxf_%d" % k, [128, 16, 16], F32),
                        tmpS=sb(C, "tmpS_%d" % k, [128, 128], F32),
                        cand=sb(C, "cand_%d" % k, [128, 8, 256], F32),
                        tmpC=sb(C, "tmpC_%d" % k, [128, 256], F32),
                        tv=sb(C, "tv_%d" % k, [128, 8, 16], F32),
                        pos=sb(C, "pos_%d" % k, [128, 8, 16], U32),
                        posf=sb(C, "posf_%d" % k, [128, 128], F32),
                        pa=sb(C, "pa_%d" % k, [128, 128], F32),
                        pb=sb(C, "pb_%d" % k, [128, 128], F32),
                        big=sb(C, "big_%d" % k, [128, 2048], F32),
                        Il=sb(C, "Il_%d" % k, [128, 128], F32),
                        Jl=sb(C, "Jl_%d" % k, [128, 128], F32),
                        ee=sb(C, "ee_%d" % k, [128, 128], F32),
                        zz=sb(C, "zz_%d" % k, [128, 8], F32),
                        gl=sb(C, "gl_%d" % k, [128, 128], F32),
                    )
                sets = [alloc_set(0), alloc_set(1)]
                LT = sb(C, "LT", [128, 3, TA], F32)

                def b4(t, off, dims):
                    tt_ = t.t
                    pstep = 1
                    for d_ in tt_.shape[1:]:
                        pstep *= d_
                    return bass.AP(tt_, off, [[pstep, 128]] + dims)

                def chain(tt, S, v16, i16, idxf, tmpS, cand, tmpC, tv, pos, posf, pa, pb, big, Il, Jl, ee, zz, gl):
                    for hp in range(16):
                        T.op("dve", lambda e, hp=hp: e.max(out=v16[:, hp, 0:8], in_=S[:, hp, :]), [S], [v16])
                        T.op("dve", lambda e, hp=hp: e.max_index(out=i16[:, hp, 0:8], in_max=v16[:, hp, 0:8], in_values=S[:, hp, :]), [S, v16], [i16])
                        T.op("dve", lambda e, hp=hp: e.match_replace(out=tmpS[:], in_to_replace=v16[:, hp, 0:8], in_values=S[:, hp, :], imm_value=-1e30),
                             [S, v16], [tmpS])
                        T.op("dve", lambda e, hp=hp: e.max(out=v16[:, hp, 8:16], in_=tmpS[:]), [tmpS], [v16])
                        T.op("dve", lambda e, hp=hp: e.max_index(out=i16[:, hp, 8:16], in_max=v16[:, hp, 8:16], in_values=tmpS[:]), [tmpS, v16], [i16])
                    T.op("dve", lambda e: e.tensor_copy(out=idxf[:], in_=i16[:]), [i16], [idxf])
                    T.op("dve", lambda e: e.tensor_tensor(out=b4(cand, 0, [[256, 8], [16, 16], [1, 16]]),
                                                          in0=b4(v16, 0, [[32, 8], [1, 16], [0, 16]]),
                                                          in1=b4(v16, 16, [[32, 8], [0, 16], [1, 16]]), op=ALU.add), [v16], [cand])
                    for h in range(8):
                        T.op("dve", lambda e, h=h: e.max(out=tv[:, h, 0:8], in_=cand[:, h, :]), [cand], [tv])
                        T.op("dve", lambda e, h=h: e.max_index(out=pos[:, h, 0:8], in_max=tv[:, h, 0:8], in_values=cand[:, h, :]), [cand, tv], [pos])
                        T.op("dve", lambda e, h=h: e.match_replace(out=tmpC[:], in_to_replace=tv[:, h, 0:8], in_values=cand[:, h, :], imm_value=-1e30),
                             [cand, tv], [tmpC])
                        T.op("dve", lambda e, h=h: e.max(out=tv[:, h, 8:16], in_=tmpC[:]), [tmpC], [tv])
                        T.op("dve", lambda e, h=h: e.max_index(out=pos[:, h, 8:16], in_max=tv[:, h, 8:16], in_values=tmpC[:]), [tmpC, tv], [pos])
                    T.op("dve", lambda e: e.tensor_copy(out=posf[:], in_=pos[:].rearrange("p h k -> p (h k)")), [pos], [posf])
                    T.op("dve", lambda e: e.tensor_tensor(out=b4(big, 0, [[16, 128], [1, 16]]), in0=b4(posf, 0, [[1, 128], [0, 16]]),
                                                          in1=bass.AP(C.cst.t, 529, [[640, 128], [0, 128], [1, 16]]), op=ALU.is_ge), [posf, C.cst], [big])
                    T.op("dve", lambda e: e.tensor_reduce(out=pa[:], in_=b4(big, 0, [[16, 128], [1, 16]]), axis=AX.X, op=ALU.add), [big], [pa])
                    T.op("dve", lambda e: e.scalar_tensor_tensor(out=pb[:], in0=pa[:], scalar=-16.0, in1=posf[:], op0=ALU.mult, op1=ALU.add), [pa, posf], [pb])
                    for (pp, poff, outl) in ((pa, 0, Il), (pb, 16, Jl)):
                        T.op("dve", lambda e, pp=pp: e.tensor_tensor(out=b4(big, 0, [[16, 128], [1, 16]]), in0=b4(pp, 0, [[1, 128], [0, 16]]),
                                                                     in1=bass.AP(C.cst.t, 513, [[640, 128], [0, 128], [1, 16]]), op=ALU.is_equal), [pp, C.cst], [big])
                        T.op("dve", lambda e, poff=poff: e.tensor_tensor(out=b4(big, 0, [[256, 8], [16, 16], [1, 16]]), in0=b4(big, 0, [[256, 8], [16, 16], [1, 16]]),
                                                                         in1=b4(idxf, poff, [[32, 8], [0, 16], [1, 16]]), op=ALU.mult), [big, idxf], [big])
                        T.op("dve", lambda e, outl=outl: e.tensor_reduce(out=outl[:], in_=b4(big, 0, [[16, 128], [1, 16]]), axis=AX.X, op=ALU.add), [big], [outl])
                    T.op("dve", lambda e: e.tensor_tensor(out=ee[:].rearrange("p (h k) -> p h k", h=8), in0=tv[:],
                                                          in1=b4(tv, 0, [[16, 8], [0, 16]]), op=ALU.subtract), [tv], [ee])
                    T.op("act", lambda e: e.activation(out=ee[:], in_=ee[:], func=AF.Exp), [ee], [ee])
                    T.op("dve", lambda e: e.tensor_reduce(out=zz[:], in_=ee[:].rearrange("p (h k) -> p h k", h=8), axis=AX.X, op=ALU.add), [ee], [zz])
                    T.op("dve", lambda e: e.reciprocal(out=zz[:], in_=zz[:]), [zz], [zz])
                    T.op("dve", lambda e: e.tensor_tensor(out=gl[:].rearrange("p (h k) -> p h k", h=8), in0=ee[:].rearrange("p (h k) -> p h k", h=8),
                                                          in1=b4(zz, 0, [[1, 8], [0, 16]]), op=ALU.mult), [ee, zz], [gl])
                    for li, lst in enumerate((Il, Jl, gl)):
                        tps = ps[5 + li]
                        T.op("pe", lambda e, lst=lst, tps=tps: e.transpose(out=tps[:, 0:128], in_=lst[:], identity=C.cst[:, 128:256]), [lst, C.cst], [tps])
                        T.op("act", lambda e, li=li, tt=tt, tps=tps: e.copy(out=LT[:, li, tt * 128:(tt + 1) * 128], in_=tps[:, 0:128]), [tps], [LT])

                def load_x1(b):
                    T.dma("sp", xts[b % 2][:], x1_s[:, b * TA:(b + 1) * TA].rearrange("(k p) t -> p k t", p=128), x1_b, xts[b % 2])

                load_x1(0)
                for b in range(nblka):
                    t0 = b * TA
                    if b + 1 < nblka:
                        load_x1(b + 1)
                    rms_mod_block(C, xts[b % 2], TA, (C.mc, 24), (C.mc, 32), sq, tmp, rstd, xf, ps[7])
                    T.dma("pool", xf_s[:, t0:t0 + TA].rearrange("(k p) t -> p k t", p=128), xf[:], xf, xf_b)
                    for cc in range(32):
                        qps = ps[4 + cc % 2]
                        for kc in range(8):
                            T.op("pe", lambda e, kc=kc, cc=cc, qps=qps: e.matmul(qps[:, 0:TA], lhsT=wq[:, kc, cc * 128:(cc + 1) * 128], rhs=xf[:, kc, :],
                                                                                 start=(kc == 0), stop=(kc == 7)), [wq, xf], [qps])
                        if cc % 2 == 0:
                            T.op("act", lambda e, cc=cc, qps=qps: e.copy(out=qp[:, cc, :], in_=qps[:, 0:TA]), [qps], [qp])
                        else:
                            T.op("dve", lambda e, cc=cc, qps=qps: e.tensor_copy(out=qp[:, cc, :], in_=qps[:, 0:TA]), [qps], [qp])
                    for tt in range(TA // 128):
                        for hp in range(16):
                            sps = ps[hp // 4]
                            for dk in range(2):
                                T.op("pe", lambda e, hp=hp, dk=dk, tt=tt, sps=sps: e.matmul(
                                    sps[:, (hp % 4) * 128:(hp % 4 + 1) * 128], lhsT=qp[:, hp * 2 + dk, tt * 128:(tt + 1) * 128],
                                    rhs=skt[:, hp * 2 + dk, :], start=(dk == 0), stop=(dk == 1)), [qp, skt], [sps])
                        S_ = sets[tt]["S"]
                        for bk in range(4):
                            T.op("act", lambda e, bk=bk, S_=S_: e.copy(out=S_[:, bk * 4:(bk + 1) * 4, :], in_=ps[bk][:, 0:512].rearrange("p (a n) -> p a n", a=4)),
                                 [ps[bk]], [S_])
                    chains = []
                    for tt in range(TA // 128):
                        T.defer = []
                        chain(tt, **sets[tt])
                        chains.append(T.defer)
                        T.defer = None
                    T.emit_interleaved(chains)
                    T.dma("pool", lt_s[:, t0:t0 + TA].rearrange("(l p) t -> p l t", p=128), LT[:], LT, lt_b)
            TP = 256
            with phase(C):
                walls = [sb(C, "wall%d" % i, [128, TP, 128], BF16) for i in range(2)]
                xfb = [sb(C, "xfb%d" % i, [128, 8, TP], BF16) for i in range(1)]
                ltb = [sb(C, "ltb%d" % i, [128, 3, TP], F32) for i in range(2)]
                NR = 4
                TG = 8
                iobf = sb(C, "iobf", [128, 128], BF16)
                T.op("dve", lambda e: e.tensor_copy(out=iobf[:], in_=C.cst[:, 384:512]), [C.cst], [iobf])
                ltbf = sb(C, "ltbf", [128, 2, TP], BF16)
                A16 = [sb(C, "A16_%d" % i, [128, TG, 128], BF16) for i in range(2)]
                E16 = [sb(C, "E16_%d" % i, [128, TG, 128], BF16) for i in range(2)]
                Bt = [sb(C, "Bt%d" % i, [128, 128], BF16) for i in range(NR)]
                NW = 3
                utt = [sb(C, "utt%d" % i, [128, 8, 512], BF16) for i in range(NW)]
                evt = [sb(C, "evt%d" % i, [128, 4, 1024], BF16) for i in range(NW)]
                gel = [sb(C, "gel%d" % i, [128, TP], F32) for i in range(3)]
                abf = [sb(C, "abf%d" % i, [128, TP], BF16) for i in range(3)]
                x1s = [sb(C, "x1s%d" % i, [128, TP], F32) for i in range(2)]
                x2s = [sb(C, "x2s%d" % i, [128, TP], F32) for i in range(1)]
                npb = NTOK // TP

                def load_xf(b):
                    t0 = b * TP
                    T.dma("sp", xfb[0][:], xf_s[:, t0:t0 + TP].rearrange("(k p) t -> p k t", p=128), xf_b, xfb[0])

                def load_lt(b):
                    t0 = b * TP
                    T.dma("sp", ltb[b % 2][:], lt_s[:, t0:t0 + TP].rearrange("(l p) t -> p l t", p=128), lt_b, ltb[b % 2])

                def load_w(gi):
                    e0 = gi * 512
                    T.dma("sp", utt[gi % NW][:], utb_s[:, e0:e0 + 512].rearrange("(k p) e -> p k e", p=128), utb_b, utt[gi % NW])
                    T.dma("sp", evt[gi % NW][:], evb_s[e0:e0 + 512, :].rearrange("(c p) n -> p c n", p=128), evb_b, evt[gi % NW])

                def wb_produce(b, tg):
                    lt = ltb[b % 2]
                    a16, e16 = A16[tg % 2], E16[tg % 2]
                    if tg == 0:
                        T.op("dve", lambda e: e.tensor_copy(out=ltbf[:], in_=lt[:, 0:2, :]), [lt], [ltbf])
                    iota_b = bass.AP(iobf.t, 0, [[128, 128], [0, TG], [1, 128]])
                    I_b = bass.AP(ltbf.t, 0 * TP + tg * TG, [[2 * TP, 128], [1, TG], [0, 128]])
                    J_b = bass.AP(ltbf.t, 1 * TP + tg * TG, [[2 * TP, 128], [1, TG], [0, 128]])
                    T.op("dve", lambda e: e.tensor_tensor(out=a16[:], in0=iota_b, in1=I_b, op=ALU.is_equal), [iobf, ltbf], [a16])
                    T.op("dve", lambda e: e.tensor_tensor(out=e16[:], in0=iota_b, in1=J_b, op=ALU.is_equal), [iobf, ltbf], [e16])

                def wb_consume(b, tg, j0, j1):
                    lt = ltb[b % 2]
                    wall = walls[b % 2]
                    a16, e16 = A16[tg % 2], E16[tg % 2]
                    for j in range(j0, j1):
                        t = tg * TG + j
                        b_ = Bt[t % NR]
                        T.op("act", lambda e, b_=b_, j=j, t=t: e.activation(out=b_[:], in_=e16[:, j, :], func=AF.Identity,
                                                                            scale=lt[:, 2, t:t + 1]), [e16, lt], [b_])
                        wps = ps[6 + (t // 4) % 2]
                        T.op("pe", lambda e, b_=b_, j=j, t=t, wps=wps: e.matmul(wps[:, (t % 4) * 128:(t % 4 + 1) * 128], lhsT=b_[:], rhs=a16[:, j, :],
                                                                                start=True, stop=True), [a16, b_], [wps])
                        if t % 4 == 3:
                            T.op("act", lambda e, t=t, wps=wps: e.copy(out=wall[:, t - 3:t + 1, :], in_=wps[:, 0:512].rearrange("p (a n) -> p a n", a=4)), [wps], [wall])

                def h_mm(i, xfq):
                    hps = ps[4 + i % 2]
                    ub_ = utt[(i // 4) % NW]
                    for kc in range(8):
                        T.op("pe", lambda e, kc=kc: e.matmul(hps[:, 0:TP], lhsT=ub_[:, kc, (i % 4) * 128:(i % 4 + 1) * 128], rhs=xfq[:, kc, :],
                                                             start=(kc == 0), stop=(kc == 7)), [ub_, xfq], [hps])

                def dense_act(i, wall):
                    hps = ps[4 + i % 2]
                    ge, ab = gel[i % 3], abf[i % 3]
                    T.op("act", lambda e: e.activation(out=ge[:], in_=hps[:, 0:TP], func=AF.Gelu), [hps], [ge])
                    meng = "dve" if i % 2 == 0 else "pool"
                    T.op(meng, lambda e: e.tensor_tensor(out=ab[:], in0=ge[:], in1=wall[:, :, i], op=ALU.mult), [ge, wall], [ab])

                def dense_out(i):
                    ab = abf[i % 3]
                    eb_ = evt[(i // 4) % NW]
                    for dc in range(8):
                        ops_ = ps[dc // 2]
                        T.op("pe", lambda e, dc=dc, ops_=ops_: e.matmul(
                            ops_[:, (dc % 2) * 256:(dc % 2) * 256 + TP], lhsT=eb_[:, i % 4, dc * 128:(dc + 1) * 128], rhs=ab[:],
                            start=(i == 0 and dc % 2 == 0), stop=(i == 127)), [eb_, ab], [ops_])

                def epi_load(b, dc):
                    t0 = b * TP
                    T.dma("sp", x1s[dc % 2][:], x1_s[dc * 128:(dc + 1) * 128, t0:t0 + TP], x1_b, x1s[dc % 2])

                def epilogue(b):
                    t0 = b * TP
                    for dc in range(8):
                        ops_ = ps[dc // 2]
                        xi, xo = x1s[dc % 2], x2s[0]
                        T.op("dve", lambda e, dc=dc, ops_=ops_, xi=xi, xo=xo: e.scalar_tensor_tensor(
                            out=xo[:], in0=ops_[:, (dc % 2) * 256:(dc % 2) * 256 + TP], scalar=C.mc[:, 40 + dc:41 + dc], in1=xi[:],
                            op0=ALU.mult, op1=ALU.add), [ops_, xi, C.mc], [xo])
                        T.dma("pool", xcur_s[dc * 128:(dc + 1) * 128, t0:t0 + TP], xo[:], xo, xcur_b)
                        if dc + 2 < 8:
                            epi_load(b, dc + 2)

                load_lt(0)
                load_xf(0)
                for tg in range(TP // TG):
                    wb_produce(0, tg)
                    wb_consume(0, tg, 0, TG)
                for b in range(npb):
                    if b + 1 < npb:
                        load_lt(b + 1)
                    xfq = xfb[0]
                    wall = walls[b % 2]
                    load_w(0)
                    load_w(1)
                    h_mm(0, xfq)
                    for i in range(128):
                        if i + 1 < 128:
                            h_mm(i + 1, xfq)
                        dense_act(i, wall)
                        if i >= 1:
                            dense_out(i - 1)
                        if i % 4 == 0 and i // 4 + 2 < 32:
                            load_w(i // 4 + 2)
                        if b + 1 < npb:
                            tg = i // 4
                            if i % 4 == 0:
                                wb_produce(b + 1, tg)
                            elif i % 4 >= 2:
                                q4 = i % 4 - 2
                                wb_consume(b + 1, tg, q4 * 4, q4 * 4 + 4)
                    epi_load(b, 0)
                    epi_load(b, 1)
                    dense_out(127)
                    if b + 1 < npb:
                        load_xf(b + 1)
                    epilogue(b)

        layer(0)
        layer(1)
        TBf = 512
        with phase(C):
            C.vecs = vecs_l[1]
            xts = [sb(C, "xtf%d" % i, [128, 8, TBf], F32) for i in range(2)]
            sq, tmp, rstd = alloc_rms_bufs(C, TBf, "f")
            xnb = [sb(C, "xnb%d" % i, [128, 8, TBf], F32) for i in range(2)]
            for b in range(NTOK // TBf):
                t0 = b * TBf
                T.dma("sp", xts[b % 2][:], xcur_s[:, t0:t0 + TBf].rearrange("(k p) t -> p k t", p=128), xcur_b, xts[b % 2])
                rms_mod_block(C, xts[b % 2], TBf, (C.vecs, 72), None, sq, tmp, rstd, xnb[b % 2], ps[0])
                T.dma("pool", xn_d[:, t0:t0 + TBf].rearrange("(k p) t -> p k t", p=128), xnb[b % 2][:], xnb[b % 2], xn_b)
    return nc


def rope_tables(half):
    t = np.arange(half * NTOK, (half + 1) * NTOK)
    row = (t // 64).astype(np.float32)
    col = (t % 64).astype(np.float32)
    inv = (1.0 / (np.float32(10000.0) ** (np.arange(0, 64, 2, dtype=np.float32) / np.float32(64)))).astype(np.float32)
    ang = np.zeros((128, NTOK), np.float32)
    for d in range(128):
        pos = row if d < 64 else col
        ang[d] = pos * inv[d % 32]
    return np.cos(ang).astype(np.float32), np.sin(ang).astype(np.float32)


def make_consts():
    c = np.zeros((128, 640), np.float32)
    c[:, 0:128] = 1.0
    c[:, 128:256] = np.eye(128, dtype=np.float32)
    for m in range(128):
        if m % 64 < 32:
            c[m + 32, 256 + m] = -1.0
        else:
            c[m - 32, 256 + m] = 1.0
    c[:, 384:512] = np.arange(128, dtype=np.float32)[None, :]
    c[:, 512] = EPS
    c[:, 513:529] = np.arange(16, dtype=np.float32)[None, :]
    c[:, 529:545] = 16.0 * (np.arange(16, dtype=np.float32)[None, :] + 1.0)
    return c


def col8(v):
    return np.ascontiguousarray(v.reshape(-1, 128).T)


def make_vecs(inp, l, b):
    v = np.zeros((128, NV), np.float32)
    v[:, 0:8] = col8(inp["c"][b])
    v[:, 8:56] = col8(inp["ada_b"][l])
    v[:, 56:64] = col8(inp["norm_mix_w"][l])
    v[:, 64:72] = col8(inp["norm_ffn_w"][l])
    v[:, 72:80] = col8(inp["final_norm_w"])
    v[:, 80] = inp["q_norm_w"][l]
    v[:, 81] = inp["k_norm_w"][l]
    v[:, 82:86] = col8(inp["pool_scale"][l])
    return v


def make_vecs_F(inp, l, b, half):
    v = make_vecs(inp, l, b)
    v[:, 86] = 1.0 if half == 1 else 0.0
    v[:, 87] = 1.0 if half == 0 else 0.0
    return v


_PROGS = {}


def get_prog(name, **kw):
    key = (name, tuple(sorted(kw.items())))
    if key not in _PROGS:
        _PROGS[key] = {"A": build_A, "B": build_B, "F": build_F}[name](**kw)
    return _PROGS[key]


def run_A(inp, l, xTs):
    consts = make_consts()
    maps = []
    for c in range(NCORES):
        b, half = c // 2, c % 2
        cs, sn = rope_tables(half)
        maps.append({"xT": xTs[c], "vecs": make_vecs(inp, l, b), "consts": consts,
                     "ada_w": np.ascontiguousarray(inp["ada_w"][l]), "w_in": np.ascontiguousarray(inp["w_in"][l]),
                     "cosT": cs, "sinT": sn})
    res = run_bass_kernel_spmd(get_prog("A"), maps, core_ids=list(range(NCORES)))
    return res.results


def invcnt_table(half):
    t = np.arange(half * NTOK, (half + 1) * NTOK)
    out = np.zeros((4, NTOK), np.float32)
    for gi, w in enumerate((2, 4, 8, 16)):
        lo = np.clip(t - w // 2, 0, SEQ)
        hi = np.clip(t + w // 2, 0, SEQ)
        out[gi] = (1.0 / (hi - lo).astype(np.float32)).astype(np.float32)
    return np.ascontiguousarray(np.broadcast_to(out.reshape(1, 4 * NTOK), (128, 4 * NTOK)))


def run_B(inp, l, xTs, ra, **kw):
    consts = make_consts()
    ada_w = np.ascontiguousarray(inp["ada_w"][l])
    w_in = np.ascontiguousarray(inp["w_in"][l])
    wao = np.ascontiguousarray(inp["w_attn_o"][l])
    wpo = np.ascontiguousarray(inp["w_pool_o"][l])
    wout = np.ascontiguousarray(inp["w_out"][l])
    mixw = np.ascontiguousarray(inp["pool_mix_w"][l].reshape(512, 128))
    wq = np.ascontiguousarray(inp["w_query"][l])
    skt = np.ascontiguousarray(inp["sub_keys"][l].reshape(8, 2, 128, 2, 128).transpose(0, 1, 3, 4, 2).reshape(4096, 128))
    ut = np.ascontiguousarray(inp["expert_u"][l].T)
    ev = np.ascontiguousarray(inp["expert_v"][l])
    maps = []
    for c in range(NCORES):
        b, half = c // 2, c % 2
        p = c ^ 1
        lo, hi = (c, p) if half == 0 else (p, c)
        cs, sn = rope_tables(half)
        kTf = np.ascontiguousarray(np.concatenate([ra[lo]["kT"], ra[hi]["kT"]], axis=1))
        vf = np.ascontiguousarray(np.concatenate([ra[lo]["v"], ra[hi]["v"]], axis=0))
        uh = np.zeros((512, 16), np.float32)
        if half == 1:
            uh[:, 0:8] = ra[p]["uh"][:, 8:16]
        else:
            uh[:, 8:16] = ra[p]["uh"][:, 0:8]
        maps.append({"xT": xTs[c], "vecs": make_vecs(inp, l, b), "consts": consts, "ada_w": ada_w, "w_in": w_in,
                     "cosT": cs, "sinT": sn, "kTf": kTf, "vf": vf, "uhalo": uh, "invcnt": invcnt_table(half),
                     "w_attn_o": wao, "w_pool_o": wpo, "w_out": wout, "pool_mix_w": mixw, "w_query": wq, "skt": skt,
                     "expert_uT": ut, "expert_v": ev})
    res = run_bass_kernel_spmd(get_prog("B", **kw), maps, core_ids=list(range(NCORES)))
    return res.results


def layer_weights(inp, l):
    return {
        "ada_w%d" % l: np.ascontiguousarray(inp["ada_w"][l]),
        "w_in%d" % l: np.ascontiguousarray(inp["w_in"][l]),
        "w_attn_o%d" % l: np.ascontiguousarray(inp["w_attn_o"][l]),
        "w_pool_o%d" % l: np.ascontiguousarray(inp["w_pool_o"][l]),
        "w_out%d" % l: np.ascontiguousarray(inp["w_out"][l]),
        "pool_mix_w%d" % l: np.ascontiguousarray(inp["pool_mix_w"][l].reshape(512, 128)),
        "w_query%d" % l: np.ascontiguousarray(inp["w_query"][l]),
        "skt%d" % l: np.ascontiguousarray(inp["sub_keys"][l].reshape(8, 2, 128, 2, 128).transpose(0, 1, 3, 4, 2).reshape(4096, 128)),
        "expert_uT%d" % l: np.ascontiguousarray(inp["expert_u"][l].T),
        "expert_v%d" % l: np.ascontiguousarray(inp["expert_v"][l]),
    }


def kernel(**inputs):
    inp = {k: np.asarray(v) for k, v in inputs.items()}
    x = inp["x"]
    consts = make_consts()
    shared = {}
    for l in range(2):
        shared.update(layer_weights(inp, l))
    maps = []
    for c in range(NCORES):
        b, half = c // 2, c % 2
        cs, sn = rope_tables(half)
        m = {"xT": np.ascontiguousarray(x[b, half * NTOK:(half + 1) * NTOK, :].T), "consts": consts, "cosT": cs, "sinT": sn,
             "invcnt": invcnt_table(half), "vecs0": make_vecs_F(inp, 0, b, half), "vecs1": make_vecs_F(inp, 1, b, half)}
        m.update(shared)
        maps.append(m)
    res = run_bass_kernel_spmd(get_prog("F"), maps, core_ids=list(range(NCORES)))
    out = np.zeros((4, SEQ, D), np.float32)
    for c in range(NCORES):
        out[c // 2, (c % 2) * NTOK:(c % 2 + 1) * NTOK, :] = res.results[c]["xTn_out"].T
    return out
```

```python
import math
from contextlib import ExitStack
import numpy as np
import concourse.bass as bass
import concourse.mybir as mybir
from concourse.bass_utils import run_bass_kernel_spmd

F32 = mybir.dt.float32
BF16 = mybir.dt.bfloat16
U32 = mybir.dt.uint32
AF = mybir.ActivationFunctionType
ALU = mybir.AluOpType
AX = mybir.AxisListType

D = 1024
NTOK = 4096
SEQ = 8192
EPS = 1e-6
NV = 96
NCORES = 8


class Ev:
    __slots__ = ("op", "sem", "val")

    def __init__(self, op=None, sem=None, val=None):
        self.op, self.sem, self.val = op, sem, val


class Op:
    __slots__ = ("fn", "waits", "inc", "eng", "dsem", "cum", "dinc")

    def __init__(self, fn, eng):
        self.fn, self.eng, self.waits, self.inc, self.dsem, self.cum, self.dinc = fn, eng, [], False, None, None, None


class Buf:
    def __init__(self, name, t=None):
        self.name, self.t = name, t
        self.last_w = None
        self.reads = []
        self.dsem = None
        self.dcount = 0

    def __getitem__(self, k):
        return self.t[k]


ENGS = ("pe", "act", "dve", "pool", "sp")


class Tracker:
    def __init__(self, nc, es):
        self.nc = nc
        self.es = es
        self.esem = {e: es.enter_context(nc.semaphore("sem_" + e)) for e in ENGS}
        self.ebase = {e: 0 for e in ENGS}
        self.ops = {e: [] for e in ENGS}
        self.waited = {e: {} for e in ENGS}
        self.bufs = []
        self.nsem = 0
        self.free = []

    def buf(self, name, t=None, transient=False):
        b = Buf(name, t)
        b.transient = transient
        self.bufs.append(b)
        return b

    def _deps(self, op, reads, writes):
        w = []
        for b in reads:
            if b.last_w is not None:
                w.append(b.last_w)
        for b in writes:
            if b.last_w is not None:
                w.append(b.last_w)
            w.extend(b.reads)
        for ev in w:
            if ev.op is not None:
                if ev.op.eng == "pe" and op.eng == "pe":
                    continue
                ev.op.inc = True
            op.waits.append(ev)

    def emit_interleaved(self, lists):
        its = [list(l) for l in lists]
        for l in its:
            while l:
                kind, args = l.pop(0)
                (self.op if kind == "op" else self.dma)(*args)

    def op(self, eng, fn, reads=(), writes=()):
        if getattr(self, "defer", None) is not None:
            self.defer.append(("op", (eng, fn, tuple(reads), tuple(writes))))
            return None
        o = Op(fn, eng)
        self._deps(o, reads, writes)
        ev = Ev(op=o)
        for b in reads:
            b.reads.append(ev)
        for b in writes:
            b.last_w = ev
            b.reads = []
        self.ops[eng].append(o)
        return o

    def dma(self, eng, out_ap, in_ap, src, dst):
        o = Op(lambda e: e.dma_start(out=out_ap, in_=in_ap), eng)
        self._deps(o, [src], [dst])
        if dst.dsem is None:
            dst.dsem_kind = eng
            if self.free and eng == "sp":
                dst.dsem, dst.dcount = self.free.pop()
            else:
                dst.dsem = self.es.enter_context(self.nc.semaphore("dsem%d" % self.nsem))
                self.nsem += 1
        dst.dcount += 1
        o.dsem = dst.dsem
        ev = Ev(sem=dst.dsem, val=16 * dst.dcount)
        if not getattr(dst, "background", False):
            src.reads.append(ev)
        dst.last_w = ev
        dst.reads = []
        self.ops[eng].append(o)
        return o

    def cc(self, in_t, out_t, groups, src, dst):
        o = Op(lambda e: e.collective_compute("AllGather", ALU.bypass, replica_groups=groups,
                                              ins=[in_t.ap().opt()], outs=[out_t.ap().opt()]), "pool")
        self._deps(o, [src], [dst])
        sem = self.es.enter_context(self.nc.semaphore("ccsem%d" % self.nsem))
        self.nsem += 1
        o.dsem = sem
        o.dinc = "cc"
        ev = Ev(sem=sem, val=1)
        src.reads.append(ev)
        dst.last_w = ev
        dst.reads = []
        self.ops["pool"].append(o)
        return o

    def flush(self, final_bufs=()):
        fin = Op(lambda e: e.nop(), "sp")
        for b in self.bufs:
            if getattr(b, "background", False):
                continue
            for ev in ([b.last_w] if b.last_w is not None else []) + b.reads:
                if ev.op is None:
                    fin.waits.append(ev)
        self.ops["sp"].append(fin)
        for e in ENGS:
            c = self.ebase[e]
            for o in self.ops[e]:
                if o.inc:
                    c += 1
                o.cum = c
        nc = self
        with self.nc.Block() as block:
            def run(ename, eng):
                waited = self.waited[ename]
                for o in self.ops[ename]:
                    for ev in o.waits:
                        if ev.op is not None:
                            sem, val = self.esem[ev.op.eng], ev.op.cum
                        else:
                            sem, val = ev.sem, ev.val
                        key = id(sem)
                        if waited.get(key, 0) < val:
                            eng.wait_ge(sem, val)
                            waited[key] = val
                    ins = o.fn(eng)
                    if o.dinc == "cc":
                        ins.then_inc(o.dsem)
                    elif o.dsem is not None:
                        ins.then_inc(o.dsem, 16)
                    elif o.inc:
                        ins.then_inc(self.esem[ename], 1)

            @block.sync
            def _(e):
                run("sp", e)

            @block.gpsimd
            def _(e):
                run("pool", e)

            @block.scalar
            def _(e):
                run("act", e)

            @block.vector
            def _(e):
                run("dve", e)

            @block.tensor
            def _(e):
                run("pe", e)
        for e in ENGS:
            self.ebase[e] = self.ops[e][-1].cum if self.ops[e] else self.ebase[e]
            self.ops[e] = []
        if self.ebase["pe"] > 0:
            self.nphase = getattr(self, "nphase", 0) + 1
            self.esem["pe"] = self.es.enter_context(self.nc.semaphore("sem_pe_%d" % self.nphase))
            self.ebase["pe"] = 0
        keep = []
        for b in self.bufs:
            if getattr(b, "background", False):
                b.reads = [ev for ev in b.reads if ev.op is None]
                keep.append(b)
                continue
            b.last_w = None
            b.reads = []
            if b.transient:
                if b.dsem is not None and getattr(b, "dsem_kind", "sp") == "sp":
                    self.free.append((b.dsem, b.dcount))
            else:
                keep.append(b)
        self.bufs = keep


class Ctx:
    pass


def sb(C, name, shape, dt):
    C.nalloc = getattr(C, "nalloc", 0) + 1
    t = C.es.enter_context(C.nc.sbuf_tensor("sb%d_%s" % (C.nalloc, name), shape, dt))
    return C.T.buf(name, t, transient=(C.es is not C.es0))


def setup_common(nc, es, T):
    C = Ctx()
    C.nc, C.es, C.T = nc, es, T
    C.es0 = es
    C.ps = []
    for i in range(8):
        t = es.enter_context(nc.psum_tensor("ps%d" % i, [128, 512], F32))
        C.ps.append(T.buf("ps%d" % i, t))
    return C


def load_consts(C, vecs_d, consts_d):
    T = C.T
    C.vecs = sb(C, "vecs", [128, NV], F32)
    C.cst = sb(C, "cst", [128, 640], F32)
    C.vecs_d = T.buf("vecs_d")
    T.dma("sp", C.vecs[:], vecs_d[:, :], C.vecs_d, C.vecs)
    T.dma("sp", C.cst[:], consts_d[:, :], C.vecs_d, C.cst)
    C.ones_bf = sb(C, "ones_bf", [128, 128], BF16)
    C.mc = sb(C, "mc", [128, 48], F32)
    T.op("dve", lambda e: e.tensor_copy(out=C.ones_bf[:], in_=C.cst[:, 0:128]), [C.cst], [C.ones_bf])


def load_consts_F(C, vecs_ds, consts_d):
    T = C.T
    C.cst = sb(C, "cst", [128, 640], F32)
    C.vecs_d = T.buf("vecs_d")
    T.dma("sp", C.cst[:], consts_d[:, :], C.vecs_d, C.cst)
    vl = []
    for i, vd in enumerate(vecs_ds):
        v = sb(C, "vecs%d" % i, [128, NV], F32)
        T.dma("sp", v[:], vd[:, :], C.vecs_d, v)
        vl.append(v)
    C.ones_bf = sb(C, "ones_bf", [128, 128], BF16)
    C.mc = sb(C, "mc", [128, 48], F32)
    T.op("dve", lambda e: e.tensor_copy(out=C.ones_bf[:], in_=C.cst[:, 0:128]), [C.cst], [C.ones_bf])
    return vl


def compute_mod(C, ada_w_d):
    T = C.T
    nc = C.nc
    cact = sb(C, "cact", [128, 8], F32)
    T.op("act", lambda e: e.activation(out=cact[:], in_=C.vecs[:, 0:8], func=AF.Silu), [C.vecs], [cact])
    adaw = [sb(C, "adaw%d" % i, [128, 8, 1024], F32) for i in range(2)]
    ada_src = T.buf("ada_src")
    mps = C.ps[7]
    for m in range(6):
        wb = adaw[m % 2]
        T.dma("sp", wb[:], ada_w_d[:, m * 1024:(m + 1) * 1024].rearrange("(k p) n -> p k n", p=128), ada_src, wb)
        for j in range(8):
            jc = m * 8 + j
            for kc in range(8):
                T.op("pe", lambda e, wb=wb, j=j, kc=kc, jc=jc: e.matmul(
                    mps[:, jc:jc + 1], lhsT=wb[:, kc, j * 128:(j + 1) * 128], rhs=cact[:, kc:kc + 1],
                    start=(kc == 0), stop=(kc == 7)), [wb, cact], [mps])
    mod = sb(C, "mod", [128, 48], F32)
    T.op("dve", lambda e: e.tensor_tensor(out=mod[:], in0=mps[:, 0:48], in1=C.vecs[:, 8:56], op=ALU.add), [mps, C.vecs], [mod])
    mc = C.mc
    T.op("dve", lambda e: e.scalar_tensor_tensor(out=mc[:, 0:8], in0=mod[:, 8:16], scalar=1.0, in1=C.vecs[:, 56:64],
                                                 op0=ALU.add, op1=ALU.mult), [mod, C.vecs], [mc])
    T.op("dve", lambda e: e.tensor_copy(out=mc[:, 8:16], in_=mod[:, 0:8]), [mod], [mc])
    T.op("dve", lambda e: e.tensor_copy(out=mc[:, 16:24], in_=mod[:, 16:24]), [mod], [mc])
    T.op("dve", lambda e: e.scalar_tensor_tensor(out=mc[:, 24:32], in0=mod[:, 32:40], scalar=1.0, in1=C.vecs[:, 64:72],
                                                 op0=ALU.add, op1=ALU.mult), [mod, C.vecs], [mc])
    T.op("dve", lambda e: e.tensor_copy(out=mc[:, 32:40], in_=mod[:, 24:32]), [mod], [mc])
    T.op("dve", lambda e: e.tensor_copy(out=mc[:, 40:48], in_=mod[:, 40:48]), [mod], [mc])


def rms_mod_block(C, xt, TB, acol, bcol, sq, tmp, rstd, xm, ss_ps):
    T = C.T
    T.op("act", lambda e: e.activation(out=sq[:], in_=xt[:], func=AF.Square), [xt], [sq])
    for kc in range(8):
        T.op("pe", lambda e, kc=kc: e.matmul(ss_ps[:, 0:TB], lhsT=C.cst[:, 0:128], rhs=sq[:, kc, :],
                                             start=(kc == 0), stop=(kc == 7)), [C.cst, sq], [ss_ps])
    T.op("act", lambda e: e.activation(out=rstd[:], in_=ss_ps[:, 0:TB], func=AF.Sqrt, scale=1.0 / D, bias=C.cst[:, 512:513]),
         [ss_ps, C.cst], [rstd])
    T.op("dve", lambda e: e.reciprocal(out=rstd[:], in_=rstd[:]), [rstd], [rstd])
    T.op("dve", lambda e: e.tensor_tensor(out=tmp[:], in0=xt[:], in1=rstd[:].unsqueeze(1).broadcast_to([128, 8, TB]),
                                          op=ALU.mult), [xt, rstd], [tmp])
    ab, ao = acol
    for kc in range(8):
        if bcol is not None:
            bb, bo = bcol
            T.op("act", lambda e, kc=kc: e.activation(out=xm[:, kc, :], in_=tmp[:, kc, :], func=AF.Identity,
                                                      scale=ab[:, ao + kc:ao + kc + 1], bias=bb[:, bo + kc:bo + kc + 1]),
                 [tmp, ab, bb], [xm])
        else:
            T.op("act", lambda e, kc=kc: e.activation(out=xm[:, kc, :], in_=tmp[:, kc, :], func=AF.Identity,
                                                      scale=ab[:, ao + kc:ao + kc + 1]), [tmp, ab], [xm])


def head_norm_rope(C, qps, TB, wcol, extra, cosb, sinb, hb, out_ap, out_buf, ps_ss, ps_rot):
    T = C.T
    qs, sq, rstd, t1, t2 = hb
    T.op("act", lambda e: e.activation(out=qs[:, 0:TB], in_=qps[:, 0:TB], func=AF.Identity, scale=C.vecs[:, wcol:wcol + 1]),
         [qps, C.vecs], [qs])
    T.op("act", lambda e: e.activation(out=sq[:, 0:TB], in_=qps[:, 0:TB], func=AF.Square), [qps], [sq])
    T.op("pe", lambda e: e.matmul(ps_ss[:, 0:TB], lhsT=C.cst[:, 0:128], rhs=sq[:, 0:TB], start=True, stop=True),
         [C.cst, sq], [ps_ss])
    T.op("pe", lambda e: e.matmul(ps_rot[:, 0:TB], lhsT=C.cst[:, 256:384], rhs=qs[:, 0:TB], start=True, stop=True),
         [C.cst, qs], [ps_rot])
    T.op("act", lambda e: e.activation(out=rstd[:, 0:TB], in_=ps_ss[:, 0:TB], func=AF.Sqrt, scale=1.0 / 128, bias=C.cst[:, 512:513]),
         [ps_ss, C.cst], [rstd])
    T.op("dve", lambda e: e.reciprocal(out=rstd[:, 0:TB], in_=rstd[:, 0:TB]), [rstd], [rstd])
    if extra != 1.0:
        T.op("dve", lambda e: e.tensor_scalar(out=rstd[:, 0:TB], in0=rstd[:, 0:TB], scalar1=float(extra), scalar2=None,
                                              op0=ALU.mult), [rstd], [rstd])
    T.op("pool", lambda e: e.tensor_tensor(out=t1[:, 0:TB], in0=qs[:, 0:TB], in1=cosb[:, 0:TB], op=ALU.mult), [qs, cosb], [t1])
    T.op("dve", lambda e: e.tensor_tensor(out=t2[:, 0:TB], in0=ps_rot[:, 0:TB], in1=sinb[:, 0:TB], op=ALU.mult),
         [ps_rot, sinb], [t2])
    T.op("pool", lambda e: e.tensor_tensor(out=t1[:, 0:TB], in0=t1[:, 0:TB], in1=t2[:, 0:TB], op=ALU.add), [t1, t2], [t1])
    T.op("dve", lambda e: e.tensor_tensor(out=out_ap, in0=t1[:, 0:TB], in1=rstd[:, 0:TB], op=ALU.mult), [t1, rstd], [out_buf])


def alloc_rms_bufs(C, TB, tag=""):
    sq = sb(C, "rsq" + tag, [128, 8, TB], F32)
    tmp = sb(C, "rtmp" + tag, [128, 8, TB], F32)
    rstd = sb(C, "rrstd" + tag, [128, TB], F32)
    return sq, tmp, rstd


def alloc_head_bufs(C, TB, tag=""):
    return [sb(C, "hb%d%s" % (i, tag), [128, TB], F32) for i in range(5)]


def build_A():
    nc = bass.Bass("TRN2", target_bir_lowering=False)
    xT_d = nc.dram_tensor("xT", [D, NTOK], F32, kind="ExternalInput").ap()
    vecs_d = nc.dram_tensor("vecs", [128, NV], F32, kind="ExternalInput").ap()
    consts_d = nc.dram_tensor("consts", [128, 640], F32, kind="ExternalInput").ap()
    ada_w_d = nc.dram_tensor("ada_w", [D, 6 * D], F32, kind="ExternalInput").ap()
    w_in_d = nc.dram_tensor("w_in", [D, 4096], F32, kind="ExternalInput").ap()
    cos_d = nc.dram_tensor("cosT", [128, NTOK], F32, kind="ExternalInput").ap()
    sin_d = nc.dram_tensor("sinT", [128, NTOK], F32, kind="ExternalInput").ap()
    kT_d = nc.dram_tensor("kT", [256, NTOK], BF16, kind="ExternalOutput").ap()
    v_d = nc.dram_tensor("v", [NTOK, 256], BF16, kind="ExternalOutput").ap()
    uh_d = nc.dram_tensor("uh", [512, 16], F32, kind="ExternalOutput").ap()
    TB = 512
    with ExitStack() as es:
        T = Tracker(nc, es)
        C = setup_common(nc, es, T)
        load_consts(C, vecs_d, consts_d)
        compute_mod(C, ada_w_d)
        T.flush()
        wsrc = T.buf("wsrc")
        wk = sb(C, "wkvp", [128, 8, 1024], BF16)
        for kc in range(8):
            T.dma("pool", wk[:, kc, :], w_in_d[kc * 128:(kc + 1) * 128, 1024:2048], wsrc, wk)
        xsrc = T.buf("xsrc")
        kout, vout, uout = T.buf("kout"), T.buf("vout"), T.buf("uout")
        xts = [sb(C, "xt%d" % i, [128, 8, TB], F32) for i in range(2)]
        cosb = [sb(C, "cos%d" % i, [128, TB], F32) for i in range(2)]
        sinb = [sb(C, "sin%d" % i, [128, TB], F32) for i in range(2)]
        sq, tmp, rstd = alloc_rms_bufs(C, TB)
        xm = sb(C, "xm", [128, 8, TB], BF16)
        hb = alloc_head_bufs(C, TB)
        kst = [sb(C, "kst%d" % i, [128, TB], BF16) for i in range(2)]
        vst = [sb(C, "vst%d" % i, [128, 256], BF16) for i in range(2)]
        ust = sb(C, "ust", [128, 4, 8], F32)
        nblk = NTOK // TB

        def load_blk(b):
            t0 = b * TB
            T.dma("sp", xts[b % 2][:], xT_d[:, t0:t0 + TB].rearrange("(k p) t -> p k t", p=128), xsrc, xts[b % 2])
            T.dma("sp", cosb[b % 2][:], cos_d[:, t0:t0 + TB], xsrc, cosb[b % 2])
            T.dma("sp", sinb[b % 2][:], sin_d[:, t0:t0 + TB], xsrc, sinb[b % 2])

        load_blk(0)
        for b in range(nblk):
            t0 = b * TB
            if b + 1 < nblk:
                load_blk(b + 1)
            xt = xts[b % 2]
            rms_mod_block(C, xt, TB, (C.mc, 0), (C.mc, 8), sq, tmp, rstd, xm, C.ps[0])
            for g in range(2):
                qps = C.ps[1 + g]
                for kc in range(8):
                    T.op("pe", lambda e, kc=kc, g=g, qps=qps: e.matmul(qps[:, 0:TB], lhsT=wk[:, kc, g * 128:(g + 1) * 128],
                                                                       rhs=xm[:, kc, :], start=(kc == 0), stop=(kc == 7)),
                         [wk, xm], [qps])
                ko = kst[g]
                head_norm_rope(C, qps, TB, 81, 1.0, cosb[b % 2], sinb[b % 2], hb, ko[:], ko, C.ps[3], C.ps[4])
                T.dma("pool", kT_d[g * 128:(g + 1) * 128, t0:t0 + TB], ko[:], ko, kout)
            for tt in range(TB // 128):
                vps = C.ps[5 + (tt % 2)]
                for kc in range(8):
                    T.op("pe", lambda e, kc=kc, tt=tt, vps=vps: e.matmul(vps[:, 0:256], lhsT=xm[:, kc, tt * 128:(tt + 1) * 128],
                                                                         rhs=wk[:, kc, 256:512], start=(kc == 0), stop=(kc == 7)),
                         [wk, xm], [vps])
                vo = vst[tt % 2]
                T.op("act", lambda e, vo=vo, vps=vps: e.copy(out=vo[:], in_=vps[:, 0:256]), [vps], [vo])
                T.dma("pool", v_d[t0 + tt * 128:t0 + (tt + 1) * 128, :], vo[:], vo, vout)
            if b == 0 or b == nblk - 1:
                for g in range(4):
                    ups = C.ps[7]
                    for kc in range(8):
                        T.op("pe", lambda e, kc=kc, g=g, ups=ups: e.matmul(ups[:, 0:TB], lhsT=wk[:, kc, 512 + g * 128:512 + (g + 1) * 128],
                                                                           rhs=xm[:, kc, :], start=(kc == 0), stop=(kc == 7)),
                             [wk, xm], [ups])
                    lo = 0 if b == 0 else TB - 8
                    T.op("act", lambda e, g=g, ups=ups, lo=lo: e.copy(out=ust[:, g, :], in_=ups[:, lo:lo + 8]), [ups], [ust])
                o0 = 0 if b == 0 else 8
                T.dma("pool", uh_d[:, o0:o0 + 8].rearrange("(g p) t -> p g t", p=128), ust[:], ust, uout)
        T.flush()
    return nc


def phase(C):
    class _P:
        def __enter__(self_):
            self_.es = ExitStack()
            self_.es.__enter__()
            C.es = self_.es
            return self_

        def __exit__(self_, *a):
            if a[0] is None:
                C.T.flush()
            C.es = C.es0
            return self_.es.__exit__(*a)
    return _P()


def build_B(stop_after=99, dbg_sel='x1'):
    nc = bass.Bass("TRN2", target_bir_lowering=False)
    def din(name, shape, dt=F32):
        return nc.dram_tensor(name, shape, dt, kind="ExternalInput").ap()
    xT_d = din("xT", [D, NTOK])
    vecs_d = din("vecs", [128, NV])
    consts_d = din("consts", [128, 640])
    ada_w_d = din("ada_w", [D, 6 * D])
    w_in_d = din("w_in", [D, 4096])
    cos_d = din("cosT", [128, NTOK])
    sin_d = din("sinT", [128, NTOK])
    kT_d = din("kTf", [256, SEQ], BF16)
    v_d = din("vf", [SEQ, 256], BF16)
    uhalo_d = din("uhalo", [512, 16])
    invcnt_d = din("invcnt", [128, 4 * NTOK])
    wao_d = din("w_attn_o", [D, D])
    wpo_d = din("w_pool_o", [512, D])
    wout_d = din("w_out", [D, D])
    mixw_d = din("pool_mix_w", [512, 128])
    wq_d = din("w_query", [D, 4096])
    skt_d = din("skt", [4096, 128])
    ut_d = din("expert_uT", [D, 16384])
    ev_d = din("expert_v", [16384, D])
    xo_d = nc.dram_tensor("xT_out", [D, NTOK], F32, kind="ExternalOutput").ap()
    xn_d = nc.dram_tensor("xTn_out", [D, NTOK], F32, kind="ExternalOutput").ap()
    dbg_d = nc.dram_tensor("dbg", [D, NTOK], F32, kind="ExternalOutput").ap()
    def scr(name, shape, dt):
        return nc.dram_tensor(name, shape, dt).ap()
    qT_s = scr("qT_s", [D, NTOK], BF16)
    gT_s = scr("gT_s", [2048, NTOK], BF16)
    uT_s = scr("uT_s", [512, NTOK + 16], F32)
    pg_s = scr("pg_s", [D, NTOK], F32)
    x1_s = scr("x1_s", [D, NTOK], F32)
    xf_s = scr("xf_s", [D, NTOK], BF16)
    lt_s = scr("lt_s", [3 * 128, NTOK], F32)
    utb_s = scr("utb_s", [D, 16384], BF16)
    evb_s = scr("evb_s", [16384, D], BF16)

    with ExitStack() as es:
        T = Tracker(nc, es)
        C = setup_common(nc, es, T)
        ps = C.ps
        src = T.buf("ext_src")
        qT_b, gT_b, uT_b, pg_b, x1_b, xf_b, lt_b, utb_b, evb_b = [T.buf(n) for n in
            ("qT_b", "gT_b", "uT_b", "pg_b", "x1_b", "xf_b", "lt_b", "utb_b", "evb_b")]
        xo_b, xn_b, dbg_b = T.buf("xo_b"), T.buf("xn_b"), T.buf("dbg_b")
        load_consts(C, vecs_d, consts_d)

        def dump_dbg():
            with phase(C):
                if dbg_sel == 'mc':
                    T.dma("sp", dbg_d[0:128, 0:48], C.mc[:], C.mc, dbg_b)
                elif dbg_sel == 'q':
                    T.dma("pool", dbg_d[:, :], qT_s[:, :], qT_b, dbg_b)
                elif dbg_sel == 'g':
                    T.dma("pool", dbg_d[:, :], gT_s[0:1024, :], gT_b, dbg_b)
                elif dbg_sel == 'gp':
                    T.dma("pool", dbg_d[:, :], gT_s[1024:2048, :], gT_b, dbg_b)
                elif dbg_sel == 'u':
                    T.dma("pool", dbg_d[0:512, :], uT_s[:, 8:8 + NTOK], uT_b, dbg_b)
                elif dbg_sel == 'pg':
                    T.dma("pool", dbg_d[:, :], pg_s[:, :], pg_b, dbg_b)
                else:
                    T.dma("pool", dbg_d[:, :], x1_s[:, :], x1_b, dbg_b)

        with phase(C):
            compute_mod(C, ada_w_d)
            for r in range(8):
                T.dma("pool", utb_s[r * 128:(r + 1) * 128, :], ut_d[r * 128:(r + 1) * 128, :], src, utb_b)
            for r in range(16):
                T.dma("pool", evb_s[r * 1024:(r + 1) * 1024, :], ev_d[r * 1024:(r + 1) * 1024, :], src, evb_b)
            T.dma("sp", uT_s[:, 0:8], uhalo_d[:, 0:8], src, uT_b)
            T.dma("sp", uT_s[:, NTOK + 8:NTOK + 16], uhalo_d[:, 8:16], src, uT_b)
        if stop_after <= 0:
            dump_dbg()
            return nc

        TB = 512
        nblk = NTOK // TB
        with phase(C):
            win = sb(C, "win", [128, 8, 4096], BF16)
            for kc in range(8):
                T.dma("pool", win[:, kc, :], w_in_d[kc * 128:(kc + 1) * 128, :], src, win)
            xts = [sb(C, "xt%d" % i, [128, 8, TB], F32) for i in range(2)]
            cosb = [sb(C, "cos%d" % i, [128, TB], F32) for i in range(2)]
            sinb = [sb(C, "sin%d" % i, [128, TB], F32) for i in range(2)]
            sq, tmp, rstd = alloc_rms_bufs(C, TB)
            xm = sb(C, "xm", [128, 8, TB], BF16)
            hb = alloc_head_bufs(C, TB)
            qst = [sb(C, "qst%d" % i, [128, TB], BF16) for i in range(2)]
            gst = [sb(C, "gst%d" % i, [128, TB], BF16) for i in range(2)]
            ust = [sb(C, "ust%d" % i, [128, TB], F32) for i in range(2)]

            def load_blk(b):
                t0 = b * TB
                T.dma("sp", xts[b % 2][:], xT_d[:, t0:t0 + TB].rearrange("(k p) t -> p k t", p=128), src, xts[b % 2])
                T.dma("sp", cosb[b % 2][:], cos_d[:, t0:t0 + TB], src, cosb[b % 2])
                T.dma("sp", sinb[b % 2][:], sin_d[:, t0:t0 + TB], src, sinb[b % 2])

            load_blk(0)
            for b in range(nblk):
                t0 = b * TB
                if b + 1 < nblk:
                    load_blk(b + 1)
                rms_mod_block(C, xts[b % 2], TB, (C.mc, 0), (C.mc, 8), sq, tmp, rstd, xm, ps[0])
                for h in range(8):
                    qps = ps[1 + h % 2]
                    for kc in range(8):
                        T.op("pe", lambda e, kc=kc, h=h, qps=qps: e.matmul(qps[:, 0:TB], lhsT=win[:, kc, h * 128:(h + 1) * 128],
                                                                           rhs=xm[:, kc, :], start=(kc == 0), stop=(kc == 7)),
                             [win, xm], [qps])
                    qo = qst[h % 2]
                    head_norm_rope(C, qps, TB, 80, 1.0 / math.sqrt(128.0), cosb[b % 2], sinb[b % 2], hb, qo[:], qo, ps[3], ps[4])
                    T.dma("pool", qT_s[h * 128:(h + 1) * 128, t0:t0 + TB], qo[:], qo, qT_b)
                for gc in range(16):
                    gps = ps[5 + gc % 2]
                    c0 = 2048 + gc * 128
                    for kc in range(8):
                        T.op("pe", lambda e, kc=kc, c0=c0, gps=gps: e.matmul(gps[:, 0:TB], lhsT=win[:, kc, c0:c0 + 128],
                                                                             rhs=xm[:, kc, :], start=(kc == 0), stop=(kc == 7)),
                             [win, xm], [gps])
                    go = gst[gc % 2]
                    T.op("act", lambda e, go=go, gps=gps: e.activation(out=go[:], in_=gps[:, 0:TB], func=AF.Sigmoid), [gps], [go])
                    T.dma("pool", gT_s[gc * 128:(gc + 1) * 128, t0:t0 + TB], go[:], go, gT_b)
                for g in range(4):
                    ups = ps[7]
                    c0 = 1536 + g * 128
                    for kc in range(8):
                        T.op("pe", lambda e, kc=kc, c0=c0, ups=ups: e.matmul(ups[:, 0:TB], lhsT=win[:, kc, c0:c0 + 128],
                                                                             rhs=xm[:, kc, :], start=(kc == 0), stop=(kc == 7)),
                             [win, xm], [ups])
                    uo = ust[g % 2]
                    T.op("dve", lambda e, uo=uo, ups=ups: e.tensor_copy(out=uo[:], in_=ups[:, 0:TB]), [ups], [uo])
                    T.dma("pool", uT_s[g * 128:(g + 1) * 128, 8 + t0:8 + t0 + TB], uo[:], uo, uT_b)
        if stop_after <= 1:
            dump_dbg()
            return nc

        with phase(C):
            mixw = sb(C, "mixw", [128, 4, 128], BF16)
            T.dma("pool", mixw[:], mixw_d.rearrange("(g p) n -> p g n", p=128), src, mixw)
            wpo = sb(C, "wpo", [128, 4, 1024], BF16)
            T.dma("pool", wpo[:], wpo_d.rearrange("(g p) n -> p g n", p=128), src, wpo)
            HB = TB + 16
            ub = [sb(C, "ub%d" % i, [128, 4, HB], F32) for i in range(2)]
            icb = [sb(C, "icb%d" % i, [128, 4, TB], F32) for i in range(2)]
            w2 = sb(C, "w2", [128, 4, HB], F32)
            w4 = sb(C, "w4", [128, 4, HB], F32)
            w8 = sb(C, "w8", [128, 4, HB], F32)
            w16 = sb(C, "w16", [128, HB], F32)
            mt = sb(C, "mt", [128, 4, TB], BF16)
            mtf = sb(C, "mtf", [128, 4, TB], F32)
            mx = sb(C, "mx", [128, 4, TB], BF16)
            gpb = [sb(C, "gpb%d" % i, [128, TB], BF16) for i in range(2)]
            pgo = [sb(C, "pgo%d" % i, [128, TB], F32) for i in range(2)]

            def load_u(b):
                t0 = b * TB
                T.dma("sp", ub[b % 2][:], uT_s[:, t0:t0 + HB].rearrange("(g p) t -> p g t", p=128), uT_b, ub[b % 2])
                T.dma("sp", icb[b % 2][:], invcnt_d.rearrange("p (g t) -> p g t", g=4)[:, :, t0:t0 + TB], src, icb[b % 2])

            load_u(0)
            for b in range(nblk):
                t0 = b * TB
                if b + 1 < nblk:
                    load_u(b + 1)
                u = ub[b % 2]
                ic = icb[b % 2]
                T.op("dve", lambda e, u=u: e.tensor_tensor(out=w2[:, :, 1:HB], in0=u[:, :, 0:HB - 1], in1=u[:, :, 1:HB], op=ALU.add), [u], [w2])
                T.op("dve", lambda e: e.tensor_tensor(out=w4[:, 1:4, 2:HB - 1], in0=w2[:, 1:4, 1:HB - 2], in1=w2[:, 1:4, 3:HB], op=ALU.add), [w2], [w4])
                T.op("dve", lambda e: e.tensor_tensor(out=w8[:, 2:4, 4:HB - 3], in0=w4[:, 2:4, 2:HB - 5], in1=w4[:, 2:4, 6:HB - 1], op=ALU.add), [w4], [w8])
                T.op("dve", lambda e: e.tensor_tensor(out=w16[:, 8:HB - 7], in0=w8[:, 3, 4:HB - 11], in1=w8[:, 3, 12:HB - 3], op=ALU.add), [w8], [w16])
                wins = [w2[:, 0, 8:8 + TB], w4[:, 1, 8:8 + TB], w8[:, 2, 8:8 + TB], w16[:, 8:8 + TB]]
                wbufs = [w2, w4, w8, w16]
                for g in range(4):
                    T.op("dve", lambda e, g=g, ic=ic: e.tensor_tensor(out=mtf[:, g, :], in0=wins[g], in1=ic[:, g, :], op=ALU.mult), [wbufs[g], ic], [mtf])
                    T.op("dve", lambda e, g=g, u=u: e.tensor_tensor(out=mt[:, g, :], in0=mtf[:, g, :], in1=u[:, g, 8:8 + TB], op=ALU.subtract), [mtf, u], [mt])
                    mps = ps[g % 2]
                    T.op("pe", lambda e, g=g, mps=mps: e.matmul(mps[:, 0:TB], lhsT=mixw[:, g, :], rhs=mt[:, g, :], start=True, stop=True), [mixw, mt], [mps])
                    T.op("act", lambda e, g=g, mps=mps: e.activation(out=mx[:, g, :], in_=mps[:, 0:TB], func=AF.Identity,
                                                                     scale=C.vecs[:, 82 + g:83 + g]), [mps, C.vecs], [mx])
                for dc in range(8):
                    pps = ps[2 + dc % 2]
                    gb = gpb[dc % 2]
                    T.dma("sp", gb[:], gT_s[1024 + dc * 128:1024 + (dc + 1) * 128, t0:t0 + TB], gT_b, gb)
                    for g in range(4):
                        T.op("pe", lambda e, g=g, dc=dc, pps=pps: e.matmul(pps[:, 0:TB], lhsT=wpo[:, g, dc * 128:(dc + 1) * 128],
                                                                           rhs=mx[:, g, :], start=(g == 0), stop=(g == 3)), [wpo, mx], [pps])
                    po = pgo[dc % 2]
                    T.op("dve", lambda e, po=po, pps=pps, gb=gb: e.tensor_tensor(out=po[:], in0=pps[:, 0:TB], in1=gb[:], op=ALU.mult), [pps, gb], [po])
                    T.dma("pool", pg_s[dc * 128:(dc + 1) * 128, t0:t0 + TB], po[:], po, pg_b)
        if stop_after <= 2:
            dump_dbg()
            return nc

        with phase(C):
            kt = sb(C, "kt", [128, 2, SEQ], BF16)
            for g in range(2):
                for hf in range(2):
                    T.dma("sp", kt[:, g, hf * 4096:(hf + 1) * 4096], kT_d[g * 128:(g + 1) * 128, hf * 4096:(hf + 1) * 4096], src, kt)
            vt = sb(C, "vt", [128, 64, 256], BF16)
            for q4 in range(4):
                T.dma("sp", vt[:, q4 * 16:(q4 + 1) * 16, :], v_d[q4 * 2048:(q4 + 1) * 2048, :].rearrange("(k p) n -> p k n", p=128), src, vt)
            wao = sb(C, "wao", [128, 8, 1024], BF16)
            wout = sb(C, "wout", [128, 8, 1024], BF16)
            for kc in range(8):
                T.dma("pool", wao[:, kc, :], wao_d[kc * 128:(kc + 1) * 128, :], src, wao)
                T.dma("pool", wout[:, kc, :], wout_d[kc * 128:(kc + 1) * 128, :], src, wout)
            qtb = [sb(C, "qtb%d" % i, [128, 8, TB], BF16) for i in range(2)]
            OT = sb(C, "OT", [128, 8, TB], BF16)
            mg = sb(C, "mg", [128, 8, TB], BF16)
            mgf = [sb(C, "mgf%d" % i, [128, TB], F32) for i in range(2)]
            pt = [sb(C, "pt%d" % i, [128, TB], BF16) for i in range(3)]
            rec = sb(C, "rec", [128, TB], F32)
            gab = [sb(C, "gab%d" % i, [128, TB], BF16) for i in range(2)]
            pgb = [sb(C, "pgb%d" % i, [128, TB], F32) for i in range(2)]
            xb = [sb(C, "xb%d" % i, [128, TB], F32) for i in range(2)]
            xob = [sb(C, "xob%d" % i, [128, TB], F32) for i in range(2)]

            def load_q(b):
                T.dma("sp", qtb[b % 2][:], qT_s[:, b * TB:(b + 1) * TB].rearrange("(h p) t -> p h t", p=128), qT_b, qtb[b % 2])

            load_q(0)
            for b in range(nblk):
                t0 = b * TB
                if b + 1 < nblk:
                    load_q(b + 1)
                qb = qtb[b % 2]
                for h in range(8):
                    g = h // 4
                    ops_, zps = ps[2 + h % 2], ps[4 + h % 2]
                    NKC = SEQ // 128

                    def s_mm(kc, h=h, g=g, qb=qb):
                        sp_ = ps[kc % 2]
                        T.op("pe", lambda e: e.matmul(sp_[:, 0:TB], lhsT=kt[:, g, kc * 128:(kc + 1) * 128], rhs=qb[:, h, :],
                                                      start=True, stop=True), [kt, qb], [sp_])
                    s_mm(0)
                    for kc in range(NKC):
                        if kc + 1 < NKC:
                            s_mm(kc + 1)
                        sp_ = ps[kc % 2]
                        p_ = pt[kc % 3]
                        T.op("act", lambda e, sp_=sp_, p_=p_: e.activation(out=p_[:], in_=sp_[:, 0:TB], func=AF.Exp), [sp_], [p_])
                        T.op("pe", lambda e, kc=kc, g=g, p_=p_, ops_=ops_: e.matmul(ops_[:, 0:TB], lhsT=vt[:, kc, g * 128:(g + 1) * 128], rhs=p_[:],
                                                                                   start=(kc == 0), stop=(kc == NKC - 1)), [vt, p_], [ops_])
                        T.op("pe", lambda e, kc=kc, p_=p_, zps=zps: e.matmul(zps[:, 0:TB], lhsT=C.ones_bf[:], rhs=p_[:],
                                                                            start=(kc == 0), stop=(kc == NKC - 1)), [C.ones_bf, p_], [zps])
                    T.op("dve", lambda e, zps=zps: e.reciprocal(out=rec[:], in_=zps[:, 0:TB]), [zps], [rec])
                    T.op("dve", lambda e, h=h, ops_=ops_: e.tensor_tensor(out=OT[:, h, :], in0=ops_[:, 0:TB], in1=rec[:], op=ALU.mult), [ops_, rec], [OT])
                for dc in range(8):
                    aps = ps[6 + dc % 2]
                    ga, pgt = gab[dc % 2], pgb[dc % 2]
                    T.dma("sp", ga[:], gT_s[dc * 128:(dc + 1) * 128, t0:t0 + TB], gT_b, ga)
                    T.dma("sp", pgt[:], pg_s[dc * 128:(dc + 1) * 128, t0:t0 + TB], pg_b, pgt)
                    for h in range(8):
                        T.op("pe", lambda e, h=h, dc=dc, aps=aps: e.matmul(aps[:, 0:TB], lhsT=wao[:, h, dc * 128:(dc + 1) * 128], rhs=OT[:, h, :],
                                                                           start=(h == 0), stop=(h == 7)), [wao, OT], [aps])
                    mf = mgf[dc % 2]
                    T.op("dve", lambda e, aps=aps, ga=ga, mf=mf: e.tensor_tensor(out=mf[:], in0=aps[:, 0:TB], in1=ga[:], op=ALU.mult), [aps, ga], [mf])
                    T.op("pool", lambda e, dc=dc, mf=mf, pgt=pgt: e.tensor_tensor(out=mg[:, dc, :], in0=mf[:], in1=pgt[:], op=ALU.add), [mf, pgt], [mg])
                for dc in range(8):
                    yps = ps[6 + dc % 2]
                    xi, xo = xb[dc % 2], xob[dc % 2]
                    T.dma("sp", xi[:], xT_d[dc * 128:(dc + 1) * 128, t0:t0 + TB], src, xi)
                    for k in range(8):
                        T.op("pe", lambda e, k=k, dc=dc, yps=yps: e.matmul(yps[:, 0:TB], lhsT=wout[:, k, dc * 128:(dc + 1) * 128], rhs=mg[:, k, :],
                                                                           start=(k == 0), stop=(k == 7)), [wout, mg], [yps])
                    T.op("dve", lambda e, dc=dc, yps=yps, xi=xi, xo=xo: e.scalar_tensor_tensor(
                        out=xo[:], in0=yps[:, 0:TB], scalar=C.mc[:, 16 + dc:17 + dc], in1=xi[:], op0=ALU.mult, op1=ALU.add), [yps, xi, C.mc], [xo])
                    T.dma("pool", x1_s[dc * 128:(dc + 1) * 128, t0:t0 + TB], xo[:], xo, x1_b)
        if stop_after <= 3:
            dump_dbg()
            return nc

        TA = 256
        nblka = NTOK // TA
        with phase(C):
            wq = sb(C, "wq", [128, 8, 4096], BF16)
            for kc in range(8):
                T.dma("pool", wq[:, kc, :], wq_d[kc * 128:(kc + 1) * 128, :], src, wq)
            skt = sb(C, "skt", [128, 32, 128], BF16)
            T.dma("pool", skt[:], skt_d.rearrange("(c p) n -> p c n", p=128), src, skt)
            xts = [sb(C, "xt%d" % i, [128, 8, TA], F32) for i in range(2)]
            sq, tmp, rstd = alloc_rms_bufs(C, TA)
            xf = sb(C, "xf", [128, 8, TA], BF16)
            qp = sb(C, "qp", [128, 32, TA], BF16)
            S = sb(C, "S", [128, 16, 128], F32)
            v16 = sb(C, "v16", [128, 16, 16], F32)
            i16 = sb(C, "i16", [128, 16, 16], U32)
            idxf = sb(C, "idxf", [128, 16, 16], F32)
            tmpS = sb(C, "tmpS", [128, 128], F32)
            cand = sb(C, "cand", [128, 8, 256], F32)
            tmpC = sb(C, "tmpC", [128, 256], F32)
            tv = sb(C, "tv", [128, 8, 16], F32)
            pos = sb(C, "pos", [128, 8, 16], U32)
            posf = sb(C, "posf", [128, 128], F32)
            pa = sb(C, "pa", [128, 128], F32)
            pb = sb(C, "pb", [128, 128], F32)
            big = sb(C, "big", [128, 2048], F32)
            big2 = sb(C, "big2", [128, 2048], F32)
            Il = sb(C, "Il", [128, 128], F32)
            Jl = sb(C, "Jl", [128, 128], F32)
            ee = sb(C, "ee", [128, 128], F32)
            zz = sb(C, "zz", [128, 8], F32)
            gl = sb(C, "gl", [128, 128], F32)
            LT = sb(C, "LT", [128, 3, TA], F32)

            def b4(t, off, dims):
                tt_ = t.t
                pstep = 1
                for d_ in tt_.shape[1:]:
                    pstep *= d_
                return bass.AP(tt_, off, [[pstep, 128]] + dims)

            def load_x1(b):
                T.dma("sp", xts[b % 2][:], x1_s[:, b * TA:(b + 1) * TA].rearrange("(k p) t -> p k t", p=128), x1_b, xts[b % 2])

            load_x1(0)
            for b in range(nblka):
                t0 = b * TA
                if b + 1 < nblka:
                    load_x1(b + 1)
                rms_mod_block(C, xts[b % 2], TA, (C.mc, 24), (C.mc, 32), sq, tmp, rstd, xf, ps[7])
                T.dma("pool", xf_s[:, t0:t0 + TA].rearrange("(k p) t -> p k t", p=128), xf[:], xf, xf_b)
                for cc in range(32):
                    qps = ps[4 + cc % 2]
                    for kc in range(8):
                        T.op("pe", lambda e, kc=kc, cc=cc, qps=qps: e.matmul(qps[:, 0:TA], lhsT=wq[:, kc, cc * 128:(cc + 1) * 128], rhs=xf[:, kc, :],
                                                                             start=(kc == 0), stop=(kc == 7)), [wq, xf], [qps])
                    if cc % 2 == 0:
                        T.op("act", lambda e, cc=cc, qps=qps: e.copy(out=qp[:, cc, :], in_=qps[:, 0:TA]), [qps], [qp])
                    else:
                        T.op("dve", lambda e, cc=cc, qps=qps: e.tensor_copy(out=qp[:, cc, :], in_=qps[:, 0:TA]), [qps], [qp])
                for tt in range(TA // 128):
                    for hp in range(16):
                        sps = ps[hp // 4]
                        for dk in range(2):
                            T.op("pe", lambda e, hp=hp, dk=dk, tt=tt, sps=sps: e.matmul(
                                sps[:, (hp % 4) * 128:(hp % 4 + 1) * 128], lhsT=qp[:, hp * 2 + dk, tt * 128:(tt + 1) * 128],
                                rhs=skt[:, hp * 2 + dk, :], start=(dk == 0), stop=(dk == 1)), [qp, skt], [sps])
                    for bk in range(4):
                        T.op("act", lambda e, bk=bk: e.copy(out=S[:, bk * 4:(bk + 1) * 4, :], in_=ps[bk][:, 0:512].rearrange("p (a n) -> p a n", a=4)),
                             [ps[bk]], [S])
                    for hp in range(16):
                        T.op("dve", lambda e, hp=hp: e.max(out=v16[:, hp, 0:8], in_=S[:, hp, :]), [S], [v16])
                        T.op("dve", lambda e, hp=hp: e.max_index(out=i16[:, hp, 0:8], in_max=v16[:, hp, 0:8], in_values=S[:, hp, :]), [S, v16], [i16])
                        T.op("dve", lambda e, hp=hp: e.match_replace(out=tmpS[:], in_to_replace=v16[:, hp, 0:8], in_values=S[:, hp, :], imm_value=-1e30),
                             [S, v16], [tmpS])
                        T.op("dve", lambda e, hp=hp: e.max(out=v16[:, hp, 8:16], in_=tmpS[:]), [tmpS], [v16])
                        T.op("dve", lambda e, hp=hp: e.max_index(out=i16[:, hp, 8:16], in_max=v16[:, hp, 8:16], in_values=tmpS[:]), [tmpS, v16], [i16])
                    T.op("dve", lambda e: e.tensor_copy(out=idxf[:], in_=i16[:]), [i16], [idxf])
                    T.op("dve", lambda e: e.tensor_tensor(out=b4(cand, 0, [[256, 8], [16, 16], [1, 16]]),
                                                          in0=b4(v16, 0, [[32, 8], [1, 16], [0, 16]]),
                                                          in1=b4(v16, 16, [[32, 8], [0, 16], [1, 16]]), op=ALU.add), [v16], [cand])
                    for h in range(8):
                        T.op("dve", lambda e, h=h: e.max(out=tv[:, h, 0:8], in_=cand[:, h, :]), [cand], [tv])
                        T.op("dve", lambda e, h=h: e.max_index(out=pos[:, h, 0:8], in_max=tv[:, h, 0:8], in_values=cand[:, h, :]), [cand, tv], [pos])
                        T.op("dve", lambda e, h=h: e.match_replace(out=tmpC[:], in_to_replace=tv[:, h, 0:8], in_values=cand[:, h, :], imm_value=-1e30),
                             [cand, tv], [tmpC])
                        T.op("dve", lambda e, h=h: e.max(out=tv[:, h, 8:16], in_=tmpC[:]), [tmpC], [tv])
                        T.op("dve", lambda e, h=h: e.max_index(out=pos[:, h, 8:16], in_max=tv[:, h, 8:16], in_values=tmpC[:]), [tmpC, tv], [pos])
                    T.op("dve", lambda e: e.tensor_copy(out=posf[:], in_=pos[:].rearrange("p h k -> p (h k)")), [pos], [posf])
                    T.op("dve", lambda e: e.tensor_tensor(out=b4(big, 0, [[16, 128], [1, 16]]), in0=b4(posf, 0, [[1, 128], [0, 16]]),
                                                          in1=bass.AP(C.cst.t, 529, [[640, 128], [0, 128], [1, 16]]), op=ALU.is_ge), [posf, C.cst], [big])
                    T.op("dve", lambda e: e.tensor_reduce(out=pa[:], in_=b4(big, 0, [[16, 128], [1, 16]]), axis=AX.X, op=ALU.add), [big], [pa])
                    T.op("dve", lambda e: e.scalar_tensor_tensor(out=pb[:], in0=pa[:], scalar=-16.0, in1=posf[:], op0=ALU.mult, op1=ALU.add), [pa, posf], [pb])
                    for (pp, poff, outl) in ((pa, 0, Il), (pb, 16, Jl)):
                        T.op("dve", lambda e, pp=pp: e.tensor_tensor(out=b4(big, 0, [[16, 128], [1, 16]]), in0=b4(pp, 0, [[1, 128], [0, 16]]),
                                                                     in1=bass.AP(C.cst.t, 513, [[640, 128], [0, 128], [1, 16]]), op=ALU.is_equal), [pp, C.cst], [big])
                        T.op("dve", lambda e, poff=poff: e.tensor_tensor(out=b4(big2, 0, [[256, 8], [16, 16], [1, 16]]), in0=b4(big, 0, [[256, 8], [16, 16], [1, 16]]),
                                                                         in1=b4(idxf, poff, [[32, 8], [0, 16], [1, 16]]), op=ALU.mult), [big, idxf], [big2])
                        T.op("dve", lambda e, outl=outl: e.tensor_reduce(out=outl[:], in_=b4(big2, 0, [[16, 128], [1, 16]]), axis=AX.X, op=ALU.add), [big2], [outl])
                    T.op("dve", lambda e: e.tensor_tensor(out=ee[:].rearrange("p (h k) -> p h k", h=8), in0=tv[:],
                                                          in1=b4(tv, 0, [[16, 8], [0, 16]]), op=ALU.subtract), [tv], [ee])
                    T.op("act", lambda e: e.activation(out=ee[:], in_=ee[:], func=AF.Exp), [ee], [ee])
                    T.op("dve", lambda e: e.tensor_reduce(out=zz[:], in_=ee[:].rearrange("p (h k) -> p h k", h=8), axis=AX.X, op=ALU.add), [ee], [zz])
                    T.op("dve", lambda e: e.reciprocal(out=zz[:], in_=zz[:]), [zz], [zz])
                    T.op("dve", lambda e: e.tensor_tensor(out=gl[:].rearrange("p (h k) -> p h k", h=8), in0=ee[:].rearrange("p (h k) -> p h k", h=8),
                                                          in1=b4(zz, 0, [[1, 8], [0, 16]]), op=ALU.mult), [ee, zz], [gl])
                    for li, lst in enumerate((Il, Jl, gl)):
                        tps = ps[5 + li]
                        T.op("pe", lambda e, lst=lst, tps=tps: e.transpose(out=tps[:, 0:128], in_=lst[:], identity=C.cst[:, 128:256]), [lst, C.cst], [tps])
                        T.op("act", lambda e, li=li, tt=tt, tps=tps: e.copy(out=LT[:, li, tt * 128:(tt + 1) * 128], in_=tps[:, 0:128]), [tps], [LT])
                T.dma("pool", lt_s[:, t0:t0 + TA].rearrange("(l p) t -> p l t", p=128), LT[:], LT, lt_b)
        if stop_after <= 4:
            with phase(C):
                cp = sb(C, "cp", [128, 3, NTOK], F32)
                T.dma("sp", cp[:], lt_s.rearrange("(l p) t -> p l t", p=128), lt_b, cp)
                T.dma("sp", dbg_d[0:384, :].rearrange("(l p) t -> p l t", p=128), cp[:], cp, dbg_b)
            return nc

        TP = 256
        with phase(C):
            wall = sb(C, "wall", [128, TP, 128], BF16)
            xfb = [sb(C, "xfb%d" % i, [128, 8, TP], BF16) for i in range(2)]
            ltb = [sb(C, "ltb%d" % i, [128, 3, TP], F32) for i in range(2)]
            x1b = [sb(C, "x1b%d" % i, [128, 8, TP], F32) for i in range(2)]
            NR = 8
            At = [sb(C, "At%d" % i, [128, 128], BF16) for i in range(NR)]
            Bt = [sb(C, "Bt%d" % i, [128, 128], BF16) for i in range(NR)]
            NW = 3
            utt = [sb(C, "utt%d" % i, [128, 8, 512], BF16) for i in range(NW)]
            evt = [sb(C, "evt%d" % i, [128, 4, 1024], BF16) for i in range(NW)]
            gel = [sb(C, "gel%d" % i, [128, TP], F32) for i in range(2)]
            abf = [sb(C, "abf%d" % i, [128, TP], BF16) for i in range(3)]
            x2 = sb(C, "x2", [128, 8, TP], F32)
            sq, tmp, rstd = alloc_rms_bufs(C, TP, "f")
            xnb = sb(C, "xnb", [128, 8, TP], F32)
            npb = NTOK // TP

            def load_pb(b):
                t0 = b * TP
                T.dma("sp", xfb[b % 2][:], xf_s[:, t0:t0 + TP].rearrange("(k p) t -> p k t", p=128), xf_b, xfb[b % 2])
                T.dma("sp", ltb[b % 2][:], lt_s[:, t0:t0 + TP].rearrange("(l p) t -> p l t", p=128), lt_b, ltb[b % 2])
                T.dma("sp", x1b[b % 2][:], x1_s[:, t0:t0 + TP].rearrange("(k p) t -> p k t", p=128), x1_b, x1b[b % 2])

            def load_w(gi):
                e0 = gi * 512
                T.dma("sp", utt[gi % NW][:], utb_s[:, e0:e0 + 512].rearrange("(k p) e -> p k e", p=128), utb_b, utt[gi % NW])
                T.dma("sp", evt[gi % NW][:], evb_s[e0:e0 + 512, :].rearrange("(c p) n -> p c n", p=128), evb_b, evt[gi % NW])

            load_pb(0)
            for b in range(npb):
                t0 = b * TP
                if b + 1 < npb:
                    load_pb(b + 1)
                xfq, lt, x1q = xfb[b % 2], ltb[b % 2], x1b[b % 2]
                load_w(0)
                load_w(1)
                for t in range(TP):
                    a_, b_ = At[t % NR], Bt[t % NR]
                    T.op("pool", lambda e, a_=a_, t=t, lt=lt: e.tensor_scalar(out=a_[:], in0=C.cst[:, 384:512], scalar1=lt[:, 0, t:t + 1], scalar2=None,
                                                                               op0=ALU.is_equal), [C.cst, lt], [a_])
                    T.op("dve", lambda e, b_=b_, t=t, lt=lt: e.tensor_scalar(out=b_[:], in0=C.cst[:, 384:512], scalar1=lt[:, 1, t:t + 1], scalar2=lt[:, 2, t:t + 1],
                                                                              op0=ALU.is_equal, op1=ALU.mult), [C.cst, lt], [b_])
                    wps = ps[6 + (t // 4) % 2]
                    T.op("pe", lambda e, a_=a_, b_=b_, t=t, wps=wps: e.matmul(wps[:, (t % 4) * 128:(t % 4 + 1) * 128], lhsT=b_[:], rhs=a_[:], start=True, stop=True),
                         [a_, b_], [wps])
                    if t % 4 == 3:
                        T.op("act", lambda e, t=t, wps=wps: e.copy(out=wall[:, t - 3:t + 1, :], in_=wps[:, 0:512].rearrange("p (a n) -> p a n", a=4)), [wps], [wall])
                def h_mm(i, xfq=xfq):
                    hps = ps[4 + i % 2]
                    ub_ = utt[(i // 4) % NW]
                    for kc in range(8):
                        T.op("pe", lambda e, kc=kc, i=i, hps=hps, ub_=ub_: e.matmul(hps[:, 0:TP], lhsT=ub_[:, kc, (i % 4) * 128:(i % 4 + 1) * 128], rhs=xfq[:, kc, :],
                                                                                    start=(kc == 0), stop=(kc == 7)), [ub_, xfq], [hps])
                h_mm(0)
                for i in range(128):
                    if i % 4 == 0 and i // 4 + 2 < 32:
                        load_w(i // 4 + 2)
                    if i + 1 < 128:
                        h_mm(i + 1)
                    hps = ps[4 + i % 2]
                    ge, ab = gel[i % 2], abf[i % 3]
                    T.op("act", lambda e, hps=hps, ge=ge: e.activation(out=ge[:], in_=hps[:, 0:TP], func=AF.Gelu), [hps], [ge])
                    meng = "dve" if i % 2 == 0 else "pool"
                    T.op(meng, lambda e, i=i, ge=ge, ab=ab: e.tensor_tensor(out=ab[:], in0=ge[:], in1=wall[:, :, i], op=ALU.mult), [ge, wall], [ab])
                    eb_ = evt[(i // 4) % NW]
                    for dc in range(8):
                        ops_ = ps[dc // 2]
                        T.op("pe", lambda e, dc=dc, i=i, ab=ab, eb_=eb_, ops_=ops_: e.matmul(
                            ops_[:, (dc % 2) * 256:(dc % 2) * 256 + TP], lhsT=eb_[:, i % 4, dc * 128:(dc + 1) * 128], rhs=ab[:],
                            start=(i == 0 and dc % 2 == 0), stop=(i == 127)), [eb_, ab], [ops_])
                for dc in range(8):
                    ops_ = ps[dc // 2]
                    T.op("dve", lambda e, dc=dc, ops_=ops_, x1q=x1q: e.scalar_tensor_tensor(
                        out=x2[:, dc, :], in0=ops_[:, (dc % 2) * 256:(dc % 2) * 256 + TP], scalar=C.mc[:, 40 + dc:41 + dc], in1=x1q[:, dc, :],
                        op0=ALU.mult, op1=ALU.add), [ops_, x1q, C.mc], [x2])
                T.dma("pool", xo_d[:, t0:t0 + TP].rearrange("(k p) t -> p k t", p=128), x2[:], x2, xo_b)
                rms_mod_block(C, x2, TP, (C.vecs, 72), None, sq, tmp, rstd, xnb, ps[4])
                T.dma("pool", xn_d[:, t0:t0 + TP].rearrange("(k p) t -> p k t", p=128), xnb[:], xnb, xn_b)
    return nc


def build_F():
    stop_after = 99
    nc = bass.Bass("TRN2", target_bir_lowering=False)
    def din(name, shape, dt=F32):
        return nc.dram_tensor(name, shape, dt, kind="ExternalInput").ap()
    xT_ext = din("xT", [D, NTOK])
    consts_d = din("consts", [128, 640])
    cos_d = din("cosT", [128, NTOK])
    sin_d = din("sinT", [128, NTOK])
    invcnt_d = din("invcnt", [128, 4 * NTOK])
    LW = []
    for l in range(2):
        LW.append(dict(
            vecs=din("vecs%d" % l, [128, NV]), ada_w=din("ada_w%d" % l, [D, 6 * D]), w_in=din("w_in%d" % l, [D, 4096]),
            wao=din("w_attn_o%d" % l, [D, D]), wpo=din("w_pool_o%d" % l, [512, D]), wout=din("w_out%d" % l, [D, D]),
            mixw=din("pool_mix_w%d" % l, [512, 128]), wq=din("w_query%d" % l, [D, 4096]), skt=din("skt%d" % l, [4096, 128]),
            ut=din("expert_uT%d" % l, [D, 16384]), ev=din("expert_v%d" % l, [16384, D])))
    xn_d = nc.dram_tensor("xTn_out", [D, NTOK], F32, kind="ExternalOutput").ap()
    def scr(name, shape, dt):
        return nc.dram_tensor(name, shape, dt).ap()
    qT_s = scr("qT_s", [D, NTOK], BF16)
    gT_s = scr("gT_s", [2048, NTOK], BF16)
    uT_s = scr("uT_s", [512, NTOK + 16], F32)
    pg_s = scr("pg_s", [D, NTOK], F32)
    x1_s = scr("x1_s", [D, NTOK], F32)
    xf_s = scr("xf_s", [D, NTOK], BF16)
    lt_s = scr("lt_s", [3 * 128, NTOK], F32)
    utb_s = scr("utb_s", [D, 16384], BF16)
    evb_s = scr("evb_s", [16384, D], BF16)
    xcur_s = scr("xcur_s", [D, NTOK], F32)
    kx_in = nc.dram_tensor("kx_in", [256, NTOK], BF16)
    kx_out = nc.dram_tensor("kx_out", [512, NTOK], BF16)
    vx_in = nc.dram_tensor("vx_in", [NTOK, 256], BF16)
    vx_out = nc.dram_tensor("vx_out", [SEQ, 256], BF16)
    ux_in = nc.dram_tensor("ux_in", [512, 16], F32)
    ux_out = nc.dram_tensor("ux_out", [1024, 16], F32)
    PAIRS = [[0, 1], [2, 3], [4, 5], [6, 7]]

    with ExitStack() as es:
        T = Tracker(nc, es)
        C = setup_common(nc, es, T)
        ps = C.ps
        src = T.buf("ext_src")
        qT_b, gT_b, uT_b, pg_b, x1_b, xf_b, lt_b, utb_b, evb_b = [T.buf(n) for n in
            ("qT_b", "gT_b", "uT_b", "pg_b", "x1_b", "xf_b", "lt_b", "utb_b", "evb_b")]
        xn_b, xcur_b = T.buf("xn_b"), T.buf("xcur_b")
        kx_b, vx_b, ux_b, kxo_b, vxo_b, uxo_b = [T.buf(n) for n in ("kx_b", "vx_b", "ux_b", "kxo_b", "vxo_b", "uxo_b")]
        vecs_l = load_consts_F(C, [LW[0]["vecs"], LW[1]["vecs"]], consts_d)
        utb_b.background = True
        evb_b.background = True

        def layer(l):
            W = LW[l]
            ada_w_d, w_in_d, wao_d, wpo_d, wout_d = W["ada_w"], W["w_in"], W["wao"], W["wpo"], W["wout"]
            mixw_d, wq_d, skt_d, ut_d, ev_d = W["mixw"], W["wq"], W["skt"], W["ut"], W["ev"]
            C.vecs = vecs_l[l]
            x_in_d, x_in_b = (xT_ext, src) if l == 0 else (xcur_s, xcur_b)
            last = (l == 1)
            with phase(C):
                compute_mod(C, ada_w_d)
                for r in range(8):
                    T.dma("pool", utb_s[r * 128:(r + 1) * 128, :], ut_d[r * 128:(r + 1) * 128, :], src, utb_b)
                for r in range(16):
                    T.dma("pool", evb_s[r * 1024:(r + 1) * 1024, :], ev_d[r * 1024:(r + 1) * 1024, :], src, evb_b)

            TB = 512
            nblk = NTOK // TB
            with phase(C):
                win = sb(C, "win", [128, 8, 4096], BF16)
                for kc in range(8):
                    T.dma("pool", win[:, kc, :], w_in_d[kc * 128:(kc + 1) * 128, :], src, win)
                xts = [sb(C, "xt%d" % i, [128, 8, TB], F32) for i in range(2)]
                cosb = [sb(C, "cos%d" % i, [128, TB], F32) for i in range(2)]
                sinb = [sb(C, "sin%d" % i, [128, TB], F32) for i in range(2)]
                sq, tmp, rstd = alloc_rms_bufs(C, TB)
                xm = sb(C, "xm", [128, 8, TB], BF16)
                hb = alloc_head_bufs(C, TB)
                qst = [sb(C, "qst%d" % i, [128, TB], BF16) for i in range(2)]
                gst = [sb(C, "gst%d" % i, [128, TB], BF16) for i in range(2)]
                ust = [sb(C, "ust%d" % i, [128, TB], F32) for i in range(2)]
                vst = [sb(C, "vst%d" % i, [128, 256], BF16) for i in range(2)]

                def load_blk(b):
                    t0 = b * TB
                    T.dma("sp", xts[b % 2][:], x_in_d[:, t0:t0 + TB].rearrange("(k p) t -> p k t", p=128), x_in_b, xts[b % 2])
                    T.dma("sp", cosb[b % 2][:], cos_d[:, t0:t0 + TB], src, cosb[b % 2])
                    T.dma("sp", sinb[b % 2][:], sin_d[:, t0:t0 + TB], src, sinb[b % 2])

                load_blk(0)
                for b in range(nblk):
                    t0 = b * TB
                    if b + 1 < nblk:
                        load_blk(b + 1)
                    rms_mod_block(C, xts[b % 2], TB, (C.mc, 0), (C.mc, 8), sq, tmp, rstd, xm, ps[0])
                    for h in range(8):
                        qps = ps[1 + h % 2]
                        for kc in range(8):
                            T.op("pe", lambda e, kc=kc, h=h, qps=qps: e.matmul(qps[:, 0:TB], lhsT=win[:, kc, h * 128:(h + 1) * 128],
                                                                               rhs=xm[:, kc, :], start=(kc == 0), stop=(kc == 7)),
                                 [win, xm], [qps])
                        qo = qst[h % 2]
                        head_norm_rope(C, qps, TB, 80, 1.0 / math.sqrt(128.0), cosb[b % 2], sinb[b % 2], hb, qo[:], qo, ps[3], ps[4])
                        T.dma("pool", qT_s[h * 128:(h + 1) * 128, t0:t0 + TB], qo[:], qo, qT_b)
                    for g in range(2):
                        qps = ps[1 + g % 2]
                        for kc in range(8):
                            T.op("pe", lambda e, kc=kc, g=g, qps=qps: e.matmul(qps[:, 0:TB], lhsT=win[:, kc, 1024 + g * 128:1024 + (g + 1) * 128],
                                                                               rhs=xm[:, kc, :], start=(kc == 0), stop=(kc == 7)),
                                 [win, xm], [qps])
                        ko = qst[g % 2]
                        head_norm_rope(C, qps, TB, 81, 1.0, cosb[b % 2], sinb[b % 2], hb, ko[:], ko, ps[3], ps[4])
                        T.dma("pool", kx_in.ap()[g * 128:(g + 1) * 128, t0:t0 + TB], ko[:], ko, kx_b)
                    for tt in range(TB // 128):
                        vps = ps[5 + tt % 2]
                        for kc in range(8):
                            T.op("pe", lambda e, kc=kc, tt=tt, vps=vps: e.matmul(vps[:, 0:256], lhsT=xm[:, kc, tt * 128:(tt + 1) * 128],
                                                                                 rhs=win[:, kc, 1280:1536], start=(kc == 0), stop=(kc == 7)),
                                 [win, xm], [vps])
                        vo = vst[tt % 2]
                        T.op("act", lambda e, vo=vo, vps=vps: e.copy(out=vo[:], in_=vps[:, 0:256]), [vps], [vo])
                        T.dma("pool", vx_in.ap()[t0 + tt * 128:t0 + (tt + 1) * 128, :], vo[:], vo, vx_b)
                    for gc in range(16):
                        gps = ps[5 + gc % 2]
                        c0 = 2048 + gc * 128
                        for kc in range(8):
                            T.op("pe", lambda e, kc=kc, c0=c0, gps=gps: e.matmul(gps[:, 0:TB], lhsT=win[:, kc, c0:c0 + 128],
                                                                                 rhs=xm[:, kc, :], start=(kc == 0), stop=(kc == 7)),
                                 [win, xm], [gps])
                        go = gst[gc % 2]
                        T.op("act", lambda e, go=go, gps=gps: e.activation(out=go[:], in_=gps[:, 0:TB], func=AF.Sigmoid), [gps], [go])
                        T.dma("pool", gT_s[gc * 128:(gc + 1) * 128, t0:t0 + TB], go[:], go, gT_b)
                    for g in range(4):
                        ups = ps[7]
                        c0 = 1536 + g * 128
                        for kc in range(8):
                            T.op("pe", lambda e, kc=kc, c0=c0, ups=ups: e.matmul(ups[:, 0:TB], lhsT=win[:, kc, c0:c0 + 128],
                                                                                 rhs=xm[:, kc, :], start=(kc == 0), stop=(kc == 7)),
                                 [win, xm], [ups])
                        uo = ust[g % 2]
                        T.op("dve", lambda e, uo=uo, ups=ups: e.tensor_copy(out=uo[:], in_=ups[:, 0:TB]), [ups], [uo])
                        T.dma("pool", uT_s[g * 128:(g + 1) * 128, 8 + t0:8 + t0 + TB], uo[:], uo, uT_b)
                        if b == 0:
                            T.dma("pool", ux_in.ap()[g * 128:(g + 1) * 128, 0:8], uo[:, 0:8], uo, ux_b)
                        if b == nblk - 1:
                            T.dma("pool", ux_in.ap()[g * 128:(g + 1) * 128, 8:16], uo[:, TB - 8:TB], uo, ux_b)

            with phase(C):
                T.cc(kx_in, kx_out, PAIRS, kx_b, kxo_b)
                T.cc(vx_in, vx_out, PAIRS, vx_b, vxo_b)
                T.cc(ux_in, ux_out, PAIRS, ux_b, uxo_b)
                hl = sb(C, "hl", [128, 4, 8], F32)
                hh = sb(C, "hh", [128, 4, 8], F32)
                T.dma("sp", hl[:], ux_out.ap()[0:512, 8:16].rearrange("(g p) t -> p g t", p=128), uxo_b, hl)
                T.dma("sp", hh[:], ux_out.ap()[512:1024, 0:8].rearrange("(g p) t -> p g t", p=128), uxo_b, hh)
                T.op("dve", lambda e: e.tensor_scalar(out=hl[:], in0=hl[:], scalar1=C.vecs[:, 86:87], scalar2=None, op0=ALU.mult), [hl, C.vecs], [hl])
                T.op("dve", lambda e: e.tensor_scalar(out=hh[:], in0=hh[:], scalar1=C.vecs[:, 87:88], scalar2=None, op0=ALU.mult), [hh, C.vecs], [hh])
                T.dma("pool", uT_s[:, 0:8].rearrange("(g p) t -> p g t", p=128), hl[:], hl, uT_b)
                T.dma("pool", uT_s[:, NTOK + 8:NTOK + 16].rearrange("(g p) t -> p g t", p=128), hh[:], hh, uT_b)

            with phase(C):
                mixw = sb(C, "mixw", [128, 4, 128], BF16)
                T.dma("pool", mixw[:], mixw_d.rearrange("(g p) n -> p g n", p=128), src, mixw)
                wpo = sb(C, "wpo", [128, 4, 1024], BF16)
                T.dma("pool", wpo[:], wpo_d.rearrange("(g p) n -> p g n", p=128), src, wpo)
                HB = TB + 16
                ub = [sb(C, "ub%d" % i, [128, 4, HB], F32) for i in range(2)]
                icb = [sb(C, "icb%d" % i, [128, 4, TB], F32) for i in range(2)]
                w2 = sb(C, "w2", [128, 4, HB], F32)
                w4 = sb(C, "w4", [128, 4, HB], F32)
                w8 = sb(C, "w8", [128, 4, HB], F32)
                w16 = sb(C, "w16", [128, HB], F32)
                mt = sb(C, "mt", [128, 4, TB], BF16)
                mtf = sb(C, "mtf", [128, 4, TB], F32)
                mx = sb(C, "mx", [128, 4, TB], BF16)
                gpb = [sb(C, "gpb%d" % i, [128, TB], BF16) for i in range(2)]
                pgo = [sb(C, "pgo%d" % i, [128, TB], F32) for i in range(2)]

                def load_u(b):
                    t0 = b * TB
                    T.dma("sp", ub[b % 2][:], uT_s[:, t0:t0 + HB].rearrange("(g p) t -> p g t", p=128), uT_b, ub[b % 2])
                    T.dma("sp", icb[b % 2][:], invcnt_d.rearrange("p (g t) -> p g t", g=4)[:, :, t0:t0 + TB], src, icb[b % 2])

                load_u(0)
                for b in range(nblk):
                    t0 = b * TB
                    if b + 1 < nblk:
                        load_u(b + 1)
                    u = ub[b % 2]
                    ic = icb[b % 2]
                    T.op("dve", lambda e, u=u: e.tensor_tensor(out=w2[:, :, 1:HB], in0=u[:, :, 0:HB - 1], in1=u[:, :, 1:HB], op=ALU.add), [u], [w2])
                    T.op("dve", lambda e: e.tensor_tensor(out=w4[:, 1:4, 2:HB - 1], in0=w2[:, 1:4, 1:HB - 2], in1=w2[:, 1:4, 3:HB], op=ALU.add), [w2], [w4])
                    T.op("dve", lambda e: e.tensor_tensor(out=w8[:, 2:4, 4:HB - 3], in0=w4[:, 2:4, 2:HB - 5], in1=w4[:, 2:4, 6:HB - 1], op=ALU.add), [w4], [w8])
                    T.op("dve", lambda e: e.tensor_tensor(out=w16[:, 8:HB - 7], in0=w8[:, 3, 4:HB - 11], in1=w8[:, 3, 12:HB - 3], op=ALU.add), [w8], [w16])
                    wins = [w2[:, 0, 8:8 + TB], w4[:, 1, 8:8 + TB], w8[:, 2, 8:8 + TB], w16[:, 8:8 + TB]]
                    wbufs = [w2, w4, w8, w16]
                    for g in range(4):
                        T.op("dve", lambda e, g=g, ic=ic: e.tensor_tensor(out=mtf[:, g, :], in0=wins[g], in1=ic[:, g, :], op=ALU.mult), [wbufs[g], ic], [mtf])
                        T.op("dve", lambda e, g=g, u=u: e.tensor_tensor(out=mt[:, g, :], in0=mtf[:, g, :], in1=u[:, g, 8:8 + TB], op=ALU.subtract), [mtf, u], [mt])
                        mps = ps[g % 2]
                        T.op("pe", lambda e, g=g, mps=mps: e.matmul(mps[:, 0:TB], lhsT=mixw[:, g, :], rhs=mt[:, g, :], start=True, stop=True), [mixw, mt], [mps])
                        T.op("act", lambda e, g=g, mps=mps: e.activation(out=mx[:, g, :], in_=mps[:, 0:TB], func=AF.Identity,
                                                                         scale=C.vecs[:, 82 + g:83 + g]), [mps, C.vecs], [mx])
                    for dc in range(8):
                        pps = ps[2 + dc % 2]
                        gb = gpb[dc % 2]
                        T.dma("sp", gb[:], gT_s[1024 + dc * 128:1024 + (dc + 1) * 128, t0:t0 + TB], gT_b, gb)
                        for g in range(4):
                            T.op("pe", lambda e, g=g, dc=dc, pps=pps: e.matmul(pps[:, 0:TB], lhsT=wpo[:, g, dc * 128:(dc + 1) * 128],
                                                                               rhs=mx[:, g, :], start=(g == 0), stop=(g == 3)), [wpo, mx], [pps])
                        po = pgo[dc % 2]
                        T.op("dve", lambda e, po=po, pps=pps, gb=gb: e.tensor_tensor(out=po[:], in0=pps[:, 0:TB], in1=gb[:], op=ALU.mult), [pps, gb], [po])
                        T.dma("pool", pg_s[dc * 128:(dc + 1) * 128, t0:t0 + TB], po[:], po, pg_b)

            with phase(C):
                kt = sb(C, "kt", [128, 2, SEQ], BF16)
                for g in range(2):
                    for hf in range(2):
                        T.dma("sp", kt[:, g, hf * 4096:(hf + 1) * 4096], kx_out.ap()[hf * 256 + g * 128:hf * 256 + (g + 1) * 128, :], kxo_b, kt)
                vt = sb(C, "vt", [128, 64, 256], BF16)
                for q4 in range(4):
                    T.dma("sp", vt[:, q4 * 16:(q4 + 1) * 16, :], vx_out.ap()[q4 * 2048:(q4 + 1) * 2048, :].rearrange("(k p) n -> p k n", p=128), vxo_b, vt)
                wao = sb(C, "wao", [128, 8, 1024], BF16)
                wout = sb(C, "wout", [128, 8, 1024], BF16)
                for kc in range(8):
                    T.dma("pool", wao[:, kc, :], wao_d[kc * 128:(kc + 1) * 128, :], src, wao)
                    T.dma("pool", wout[:, kc, :], wout_d[kc * 128:(kc + 1) * 128, :], src, wout)
                qtb = [sb(C, "qtb%d" % i, [128, 8, TB], BF16) for i in range(2)]
                OT = sb(C, "OT", [128, 8, TB], BF16)
                mg = sb(C, "mg", [128, 8, TB], BF16)
                mgf = [sb(C, "mgf%d" % i, [128, TB], F32) for i in range(2)]
                pt = [sb(C, "pt%d" % i, [128, TB], BF16) for i in range(4)]
                rec = sb(C, "rec", [128, TB], F32)
                zacc = [sb(C, "zacc%d" % i, [128, TB], F32) for i in range(2)]
                gab = [sb(C, "gab%d" % i, [128, TB], BF16) for i in range(2)]
                pgb = [sb(C, "pgb%d" % i, [128, TB], F32) for i in range(2)]
                xb = [sb(C, "xb%d" % i, [128, TB], F32) for i in range(2)]
                xob = [sb(C, "xob%d" % i, [128, TB], F32) for i in range(2)]

                def load_q(b):
                    T.dma("sp", qtb[b % 2][:], qT_s[:, b * TB:(b + 1) * TB].rearrange("(h p) t -> p h t", p=128), qT_b, qtb[b % 2])

                load_q(0)
                for b in range(nblk):
                    t0 = b * TB
                    if b + 1 < nblk:
                        load_q(b + 1)
                    qb = qtb[b % 2]
                    SB = (0, 1, 4)
                    for h in range(8):
                        g = h // 4
                        ops_, zps = ps[2 + h % 2], ps[5]
                        NKC = SEQ // 128

                        def s_mm(kc, h=h, g=g, qb=qb):
                            sp_ = ps[SB[kc % 3]]
                            T.op("pe", lambda e: e.matmul(sp_[:, 0:TB], lhsT=kt[:, g, kc * 128:(kc + 1) * 128], rhs=qb[:, h, :],
                                                          start=True, stop=True), [kt, qb], [sp_])
                        s_mm(0)
                        s_mm(1)
                        for kc in range(NKC):
                            if kc + 2 < NKC:
                                s_mm(kc + 2)
                            sp_ = ps[SB[kc % 3]]
                            p_ = pt[kc % 4]
                            T.op("act", lambda e, sp_=sp_, p_=p_: e.activation(out=p_[:], in_=sp_[:, 0:TB], func=AF.Exp), [sp_], [p_])
                            T.op("pe", lambda e, kc=kc, g=g, p_=p_, ops_=ops_: e.matmul(ops_[:, 0:TB], lhsT=vt[:, kc, g * 128:(g + 1) * 128], rhs=p_[:],
                                                                                       start=(kc == 0), stop=(kc == NKC - 1)), [vt, p_], [ops_])
                            za = zacc[0]
                            if kc % 2 == 1:
                                T.op("pe", lambda e, kc=kc, p_=p_, zps=zps: e.matmul(zps[:, 0:TB], lhsT=C.ones_bf[:], rhs=p_[:],
                                                                                    start=(kc == 1), stop=False), [C.ones_bf, p_], [zps])
                            elif kc == 0:
                                T.op("dve", lambda e, p_=p_, za=za: e.tensor_copy(out=za[:], in_=p_[:]), [p_], [za])
                            else:
                                T.op("dve", lambda e, p_=p_, za=za: e.tensor_tensor(out=za[:], in0=za[:], in1=p_[:], op=ALU.add), [p_, za], [za])
                        T.op("pe", lambda e, zps=zps: e.matmul(zps[:, 0:TB], lhsT=C.cst[:, 0:128], rhs=zacc[0][:], start=False, stop=True), [C.cst, zacc[0]], [zps])
                        T.op("dve", lambda e, zps=zps: e.reciprocal(out=rec[:], in_=zps[:, 0:TB]), [zps], [rec])
                        T.op("dve", lambda e, h=h, ops_=ops_: e.tensor_tensor(out=OT[:, h, :], in0=ops_[:, 0:TB], in1=rec[:], op=ALU.mult), [ops_, rec], [OT])
                    for dc in range(8):
                        aps = ps[6 + dc % 2]
                        ga, pgt = gab[dc % 2], pgb[dc % 2]
                        T.dma("sp", ga[:], gT_s[dc * 128:(dc + 1) * 128, t0:t0 + TB], gT_b, ga)
                        T.dma("sp", pgt[:], pg_s[dc * 128:(dc + 1) * 128, t0:t0 + TB], pg_b, pgt)
                        for h in range(8):
                            T.op("pe", lambda e, h=h, dc=dc, aps=aps: e.matmul(aps[:, 0:TB], lhsT=wao[:, h, dc * 128:(dc + 1) * 128], rhs=OT[:, h, :],
                                                                               start=(h == 0), stop=(h == 7)), [wao, OT], [aps])
                        mf = mgf[dc % 2]
                        T.op("dve", lambda e, aps=aps, ga=ga, mf=mf: e.tensor_tensor(out=mf[:], in0=aps[:, 0:TB], in1=ga[:], op=ALU.mult), [aps, ga], [mf])
                        T.op("pool", lambda e, dc=dc, mf=mf, pgt=pgt: e.tensor_tensor(out=mg[:, dc, :], in0=mf[:], in1=pgt[:], op=ALU.add), [mf, pgt], [mg])
                    for dc in range(8):
                        yps = ps[6 + dc % 2]
                        xi, xo = xb[dc % 2], xob[dc % 2]
                        T.dma("sp", xi[:], x_in_d[dc * 128:(dc + 1) * 128, t0:t0 + TB], x_in_b, xi)
                        for k in range(8):
                            T.op("pe", lambda e, k=k, dc=dc, yps=yps: e.matmul(yps[:, 0:TB], lhsT=wout[:, k, dc * 128:(dc + 1) * 128], rhs=mg[:, k, :],
                                                                               start=(k == 0), stop=(k == 7)), [wout, mg], [yps])
                        T.op("dve", lambda e, dc=dc, yps=yps, xi=xi, xo=xo: e.scalar_tensor_tensor(
                            out=xo[:], in0=yps[:, 0:TB], scalar=C.mc[:, 16 + dc:17 + dc], in1=xi[:], op0=ALU.mult, op1=ALU.add), [yps, xi, C.mc], [xo])
                        T.dma("pool", x1_s[dc * 128:(dc + 1) * 128, t0:t0 + TB], xo[:], xo, x1_b)

            TA = 256
            nblka = NTOK // TA
            with phase(C):
                wq = sb(C, "wq", [128, 8, 4096], BF16)
                for kc in range(8):
                    T.dma("pool", wq[:, kc, :], wq_d[kc * 128:(kc + 1) * 128, :], src, wq)
                skt = sb(C, "skt", [128, 32, 128], BF16)
                T.dma("pool", skt[:], skt_d.rearrange("(c p) n -> p c n", p=128), src, skt)
                xts = [sb(C, "xt%d" % i, [128, 8, TA], F32) for i in range(2)]
                sq, tmp, rstd = alloc_rms_bufs(C, TA)
                xf = sb(C, "xf", [128, 8, TA], BF16)
                qp = sb(C, "qp", [128, 32, TA], BF16)
                def alloc_set(k):
                    return dict(
                        S=sb(C, "S_%d" % k, [128, 16, 128], F32),
                        v16=sb(C, "v16_%d" % k, [128, 16, 16], F32),
                        i16=sb(C, "i16_%d" % k, [128, 16, 16], U32),
                        idxf=sb(C, "idxf_%d" % k, [128, 16, 16], F32),
                        tmpS=sb(C, "tmpS_%d" % k, [128, 128], F32),
                        cand=sb(C, "cand_%d" % k, [128, 8, 256], F32),
                        tmpC=sb(C, "tmpC_%d" % k, [128, 256], F32),
                        tv=sb(C, "tv_%d" % k, [128, 8, 16], F32),
                        pos=sb(C, "pos_%d" % k, [128, 8, 16], U32),
                        posf=sb(C, "posf_%d" % k, [128, 128], F32),
                        pa=sb(C, "pa_%d" % k, [128, 128], F32),
                        pb=sb(C, "pb_%d" % k, [128, 128], F32),
                        big=sb(C, "big_%d" % k, [128, 2048], F32),
                        Il=sb(C, "Il_%d" % k, [128, 128], F32),
                        Jl=sb(C, "Jl_%d" % k, [128, 128], F32),
                        ee=sb(C, "ee_%d" % k, [128, 128], F32),
                        zz=sb(C, "zz_%d" % k, [128, 8], F32),
                        gl=sb(C, "gl_%d" % k, [128, 128], F32),
                    )
                sets = [alloc_set(0), alloc_set(1)]
                LT = sb(C, "LT", [128, 3, TA], F32)

                def b4(t, off, dims):
                    tt_ = t.t
                    pstep = 1
                    for d_ in tt_.shape[1:]:
                        pstep *= d_
                    return bass.AP(tt_, off, [[pstep, 128]] + dims)

                def chain(tt, S, v16, i16, idxf, tmpS, cand, tmpC, tv, pos, posf, pa, pb, big, Il, Jl, ee, zz, gl):
                    for hp in range(16):
                        T.op("dve", lambda e, hp=hp: e.max(out=v16[:, hp, 0:8], in_=S[:, hp, :]), [S], [v16])
                        T.op("dve", lambda e, hp=hp: e.max_index(out=i16[:, hp, 0:8], in_max=v16[:, hp, 0:8], in_values=S[:, hp, :]), [S, v16], [i16])
                        T.op("dve", lambda e, hp=hp: e.match_replace(out=tmpS[:], in_to_replace=v16[:, hp, 0:8], in_values=S[:, hp, :], imm_value=-1e30),
                             [S, v16], [tmpS])
                        T.op("dve", lambda e, hp=hp: e.max(out=v16[:, hp, 8:16], in_=tmpS[:]), [tmpS], [v16])
                        T.op("dve", lambda e, hp=hp: e.max_index(out=i16[:, hp, 8:16], in_max=v16[:, hp, 8:16], in_values=tmpS[:]), [tmpS, v16], [i16])
                    T.op("dve", lambda e: e.tensor_copy(out=idxf[:], in_=i16[:]), [i16], [idxf])
                    T.op("dve", lambda e: e.tensor_tensor(out=b4(cand, 0, [[256, 8], [16, 16], [1, 16]]),
                                                          in0=b4(v16, 0, [[32, 8], [1, 16], [0, 16]]),
                                                          in1=b4(v16, 16, [[32, 8], [0, 16], [1, 16]]), op=ALU.add), [v16], [cand])
                    for h in range(8):
                        T.op("dve", lambda e, h=h: e.max(out=tv[:, h, 0:8], in_=cand[:, h, :]), [cand], [tv])
                        T.op("dve", lambda e, h=h: e.max_index(out=pos[:, h, 0:8], in_max=tv[:, h, 0:8], in_values=cand[:, h, :]), [cand, tv], [pos])
                        T.op("dve", lambda e, h=h: e.match_replace(out=tmpC[:], in_to_replace=tv[:, h, 0:8], in_values=cand[:, h, :], imm_value=-1e30),
                             [cand, tv], [tmpC])
                        T.op("dve", lambda e, h=h: e.max(out=tv[:, h, 8:16], in_=tmpC[:]), [tmpC], [tv])
                        T.op("dve", lambda e, h=h: e.max_index(out=pos[:, h, 8:16], in_max=tv[:, h, 8:16], in_values=tmpC[:]), [tmpC, tv], [pos])
                    T.op("dve", lambda e: e.tensor_copy(out=posf[:], in_=pos[:].rearrange("p h k -> p (h k)")), [pos], [posf])
                    T.op("dve", lambda e: e.tensor_tensor(out=b4(big, 0, [[16, 128], [1, 16]]), in0=b4(posf, 0, [[1, 128], [0, 16]]),
                                                          in1=bass.AP(C.cst.t, 529, [[640, 128], [0, 128], [1, 16]]), op=ALU.is_ge), [posf, C.cst], [big])
                    T.op("dve", lambda e: e.tensor_reduce(out=pa[:], in_=b4(big, 0, [[16, 128], [1, 16]]), axis=AX.X, op=ALU.add), [big], [pa])
                    T.op("dve", lambda e: e.scalar_tensor_tensor(out=pb[:], in0=pa[:], scalar=-16.0, in1=posf[:], op0=ALU.mult, op1=ALU.add), [pa, posf], [pb])
                    for (pp, poff, outl) in ((pa, 0, Il), (pb, 16, Jl)):
                        T.op("dve", lambda e, pp=pp: e.tensor_tensor(out=b4(big, 0, [[16, 128], [1, 16]]), in0=b4(pp, 0, [[1, 128], [0, 16]]),
                                                                     in1=bass.AP(C.cst.t, 513, [[640, 128], [0, 128], [1, 16]]), op=ALU.is_equal), [pp, C.cst], [big])
                        T.op("dve", lambda e, poff=poff: e.tensor_tensor(out=b4(big, 0, [[256, 8], [16, 16], [1, 16]]), in0=b4(big, 0, [[256, 8], [16, 16], [1, 16]]),
                                                                         in1=b4(idxf, poff, [[32, 8], [0, 16], [1, 16]]), op=ALU.mult), [big, idxf], [big])
                        T.op("dve", lambda e, outl=outl: e.tensor_reduce(out=outl[:], in_=b4(big, 0, [[16, 128], [1, 16]]), axis=AX.X, op=ALU.add), [big], [outl])
                    T.op("dve", lambda e: e.tensor_tensor(out=ee[:].rearrange("p (h k) -> p h k", h=8), in0=tv[:],
                                                          in1=b4(tv, 0, [[16, 8], [0, 16]]), op=ALU.subtract), [tv], [ee])
                    T.op("act", lambda e: e.activation(out=ee[:], in_=ee[:], func=AF.Exp), [ee], [ee])
                    T.op("dve", lambda e: e.tensor_reduce(out=zz[:], in_=ee[:].rearrange("p (h k) -> p h k", h=8), axis=AX.X, op=ALU.add), [ee], [zz])
                    T.op("dve", lambda e: e.reciprocal(out=zz[:], in_=zz[:]), [zz], [zz])
                    T.op("dve", lambda e: e.tensor_tensor(out=gl[:].rearrange("p (h k) -> p h k", h=8), in0=ee[:].rearrange("p (h k) -> p h k", h=8),
                                                          in1=b4(zz, 0, [[1, 8], [0, 16]]), op=ALU.mult), [ee, zz], [gl])
                    for li, lst in enumerate((Il, Jl, gl)):
                        tps = ps[5 + li]
                        T.op("pe", lambda e, lst=lst, tps=tps: e.transpose(out=tps[:, 0:128], in_=lst[:], identity=C.cst[:, 128:256]), [lst, C.cst], [tps])
                        T.op("act", lambda e, li=li, tt=tt, tps=tps: e.copy(out=LT[:, li, tt * 128:(tt + 1) * 128], in_=tps[:, 0:128]), [tps], [LT])

                def load_x1(b):
                    T.dma("sp", xts[b % 2][:], x1_s[:, b * TA:(b + 1) * TA].rearrange("(k p) t -> p k t", p=128), x1_b, xts[b % 2])

                load_x1(0)
                for b in range(nblka):
                    t0 = b * TA
                    if b + 1 < nblka:
                        load_x1(b + 1)
                    rms_mod_block(C, xts[b % 2], TA, (C.mc, 24), (C.mc, 32), sq, tmp, rstd, xf, ps[7])
                    T.dma("pool", xf_s[:, t0:t0 + TA].rearrange("(k p) t -> p k t", p=128), xf[:], xf, xf_b)
                    for cc in range(32):
                        qps = ps[4 + cc % 2]
                        for kc in range(8):
                            T.op("pe", lambda e, kc=kc, cc=cc, qps=qps: e.matmul(qps[:, 0:TA], lhsT=wq[:, kc, cc * 128:(cc + 1) * 128], rhs=xf[:, kc, :],
                                                                                 start=(kc == 0), stop=(kc == 7)), [wq, xf], [qps])
                        if cc % 2 == 0:
                            T.op("act", lambda e, cc=cc, qps=qps: e.copy(out=qp[:, cc, :], in_=qps[:, 0:TA]), [qps], [qp])
                        else:
                            T.op("dve", lambda e, cc=cc, qps=qps: e.tensor_copy(out=qp[:, cc, :], in_=qps[:, 0:TA]), [qps], [qp])
                    for tt in range(TA // 128):
                        for hp in range(16):
                            sps = ps[hp // 4]
                            for dk in range(2):
                                T.op("pe", lambda e, hp=hp, dk=dk, tt=tt, sps=sps: e.matmul(
                                    sps[:, (hp % 4) * 128:(hp % 4 + 1) * 128], lhsT=qp[:, hp * 2 + dk, tt * 128:(tt + 1) * 128],
                                    rhs=skt[:, hp * 2 + dk, :], start=(dk == 0), stop=(dk == 1)), [qp, skt], [sps])
                        S_ = sets[tt]["S"]
                        for bk in range(4):
                            T.op("act", lambda e, bk=bk, S_=S_: e.copy(out=S_[:, bk * 4:(bk + 1) * 4, :], in_=ps[bk][:, 0:512].rearrange("p (a n) -> p a n", a=4)),
                                 [ps[bk]], [S_])
                    chains = []
                    for tt in range(TA // 128):
                        T.defer = []
                        chain(tt, **sets[tt])
                        chains.append(T.defer)
                        T.defer = None
                    T.emit_interleaved(chains)
                    T.dma("pool", lt_s[:, t0:t0 + TA].rearrange("(l p) t -> p l t", p=128), LT[:], LT, lt_b)
            TP = 256
            with phase(C):
                walls = [sb(C, "wall%d" % i, [128, TP, 128], BF16) for i in range(2)]
                xfb = [sb(C, "xfb%d" % i, [128, 8, TP], BF16) for i in range(1)]
                ltb = [sb(C, "ltb%d" % i, [128, 3, TP], F32) for i in range(2)]
                NR = 4
                TG = 8
                iobf = sb(C, "iobf", [128, 128], BF16)
                T.op("dve", lambda e: e.tensor_copy(out=iobf[:], in_=C.cst[:, 384:512]), [C.cst], [iobf])
                ltbf = sb(C, "ltbf", [128, 2, TP], BF16)
                A16 = [sb(C, "A16_%d" % i, [128, TG, 128], BF16) for i in range(2)]
                E16 = [sb(C, "E16_%d" % i, [128, TG, 128], BF16) for i in range(2)]
                Bt = [sb(C, "Bt%d" % i, [128, 128], BF16) for i in range(NR)]
                NW = 3
                utt = [sb(C, "utt%d" % i, [128, 8, 512], BF16) for i in range(NW)]
                evt = [sb(C, "evt%d" % i, [128, 4, 1024], BF16) for i in range(NW)]
                gel = [sb(C, "gel%d" % i, [128, TP], F32) for i in range(3)]
                abf = [sb(C, "abf%d" % i, [128, TP], BF16) for i in range(3)]
                x1s = [sb(C, "x1s%d" % i, [128, TP], F32) for i in range(2)]
                x2s = [sb(C, "x2s%d" % i, [128, TP], F32) for i in range(1)]
                npb = NTOK // TP

                def load_xf(b):
                    t0 = b * TP
                    T.dma("sp", xfb[0][:], xf_s[:, t0:t0 + TP].rearrange("(k p) t -> p k t", p=128), xf_b, xfb[0])

                def load_lt(b):
                    t0 = b * TP
                    T.dma("sp", ltb[b % 2][:], lt_s[:, t0:t0 + TP].rearrange("(l p) t -> p l t", p=128), lt_b, ltb[b % 2])

                def load_w(gi):
                    e0 = gi * 512
                    T.dma("sp", utt[gi % NW][:], utb_s[:, e0:e0 + 512].rearrange("(k p) e -> p k e", p=128), utb_b, utt[gi % NW])
                    T.dma("sp", evt[gi % NW][:], evb_s[e0:e0 + 512, :].rearrange("(c p) n -> p c n", p=128), evb_b, evt[gi % NW])

                def wb_produce(b, tg):
                    lt = ltb[b % 2]
                    a16, e16 = A16[tg % 2], E16[tg % 2]
                    if tg == 0:
                        T.op("dve", lambda e: e.tensor_copy(out=ltbf[:], in_=lt[:, 0:2, :]), [lt], [ltbf])
                    iota_b = bass.AP(iobf.t, 0, [[128, 128], [0, TG], [1, 128]])
                    I_b = bass.AP(ltbf.t, 0 * TP + tg * TG, [[2 * TP, 128], [1, TG], [0, 128]])
                    J_b = bass.AP(ltbf.t, 1 * TP + tg * TG, [[2 * TP, 128], [1, TG], [0, 128]])
                    T.op("dve", lambda e: e.tensor_tensor(out=a16[:], in0=iota_b, in1=I_b, op=ALU.is_equal), [iobf, ltbf], [a16])
                    T.op("dve", lambda e: e.tensor_tensor(out=e16[:], in0=iota_b, in1=J_b, op=ALU.is_equal), [iobf, ltbf], [e16])

                def wb_consume(b, tg, j0, j1):
                    lt = ltb[b % 2]
                    wall = walls[b % 2]
                    a16, e16 = A16[tg % 2], E16[tg % 2]
                    for j in range(j0, j1):
                        t = tg * TG + j
                        b_ = Bt[t % NR]
                        T.op("act", lambda e, b_=b_, j=j, t=t: e.activation(out=b_[:], in_=e16[:, j, :], func=AF.Identity,
                                                                            scale=lt[:, 2, t:t + 1]), [e16, lt], [b_])
                        wps = ps[6 + (t // 4) % 2]
                        T.op("pe", lambda e, b_=b_, j=j, t=t, wps=wps: e.matmul(wps[:, (t % 4) * 128:(t % 4 + 1) * 128], lhsT=b_[:], rhs=a16[:, j, :],
                                                                                start=True, stop=True), [a16, b_], [wps])
                        if t % 4 == 3:
                            T.op("act", lambda e, t=t, wps=wps: e.copy(out=wall[:, t - 3:t + 1, :], in_=wps[:, 0:512].rearrange("p (a n) -> p a n", a=4)), [wps], [wall])

                def h_mm(i, xfq):
                    hps = ps[4 + i % 2]
                    ub_ = utt[(i // 4) % NW]
                    for kc in range(8):
                        T.op("pe", lambda e, kc=kc: e.matmul(hps[:, 0:TP], lhsT=ub_[:, kc, (i % 4) * 128:(i % 4 + 1) * 128], rhs=xfq[:, kc, :],
                                                             start=(kc == 0), stop=(kc == 7)), [ub_, xfq], [hps])

                def dense_act(i, wall):
                    hps = ps[4 + i % 2]
                    ge, ab = gel[i % 3], abf[i % 3]
                    T.op("act", lambda e: e.activation(out=ge[:], in_=hps[:, 0:TP], func=AF.Gelu), [hps], [ge])
                    meng = "dve" if i % 2 == 0 else "pool"
                    T.op(meng, lambda e: e.tensor_tensor(out=ab[:], in0=ge[:], in1=wall[:, :, i], op=ALU.mult), [ge, wall], [ab])

                def dense_out(i):
                    ab = abf[i % 3]
                    eb_ = evt[(i // 4) % NW]
                    for dc in range(8):
                        ops_ = ps[dc // 2]
                        T.op("pe", lambda e, dc=dc, ops_=ops_: e.matmul(
                            ops_[:, (dc % 2) * 256:(dc % 2) * 256 + TP], lhsT=eb_[:, i % 4, dc * 128:(dc + 1) * 128], rhs=ab[:],
                            start=(i == 0 and dc % 2 == 0), stop=(i == 127)), [eb_, ab], [ops_])

                def epi_load(b, dc):
                    t0 = b * TP
                    T.dma("sp", x1s[dc % 2][:], x1_s[dc * 128:(dc + 1) * 128, t0:t0 + TP], x1_b, x1s[dc % 2])

                def epilogue(b):
                    t0 = b * TP
                    for dc in range(8):
                        ops_ = ps[dc // 2]
                        xi, xo = x1s[dc % 2], x2s[0]
                        T.op("dve", lambda e, dc=dc, ops_=ops_, xi=xi, xo=xo: e.scalar_tensor_tensor(
                            out=xo[:], in0=ops_[:, (dc % 2) * 256:(dc % 2) * 256 + TP], scalar=C.mc[:, 40 + dc:41 + dc], in1=xi[:],
                            op0=ALU.mult, op1=ALU.add), [ops_, xi, C.mc], [xo])
                        T.dma("pool", xcur_s[dc * 128:(dc + 1) * 128, t0:t0 + TP], xo[:], xo, xcur_b)
                        if dc + 2 < 8:
                            epi_load(b, dc + 2)

                load_lt(0)
                load_xf(0)
                for tg in range(TP // TG):
                    wb_produce(0, tg)
                    wb_consume(0, tg, 0, TG)
                for b in range(npb):
                    if b + 1 < npb:
                        load_lt(b + 1)
                    xfq = xfb[0]
                    wall = walls[b % 2]
                    load_w(0)
                    load_w(1)
                    h_mm(0, xfq)
                    for i in range(128):
                        if i + 1 < 128:
                            h_mm(i + 1, xfq)
                        dense_act(i, wall)
                        if i >= 1:
                            dense_out(i - 1)
                        if i % 4 == 0 and i // 4 + 2 < 32:
                            load_w(i // 4 + 2)
                        if b + 1 < npb:
                            tg = i // 4
                            if i % 4 == 0:
                                wb_produce(b + 1, tg)
                            elif i % 4 >= 2:
                                q4 = i % 4 - 2
                                wb_consume(b + 1, tg, q4 * 4, q4 * 4 + 4)
                    epi_load(b, 0)
                    epi_load(b, 1)
                    dense_out(127)
                    if b + 1 < npb:
                        load_xf(b + 1)
                    epilogue(b)

        layer(0)
        layer(1)
        TBf = 512
        with phase(C):
            C.vecs = vecs_l[1]
            xts = [sb(C, "xtf%d" % i, [128, 8, TBf], F32) for i in range(2)]
            sq, tmp, rstd = alloc_rms_bufs(C, TBf, "f")
            xnb = [sb(C, "xnb%d" % i, [128, 8, TBf], F32) for i in range(2)]
            for b in range(NTOK // TBf):
                t0 = b * TBf
                T.dma("sp", xts[b % 2][:], xcur_s[:, t0:t0 + TBf].rearrange("(k p) t -> p k t", p=128), xcur_b, xts[b % 2])
                rms_mod_block(C, xts[b % 2], TBf, (C.vecs, 72), None, sq, tmp, rstd, xnb[b % 2], ps[0])
                T.dma("pool", xn_d[:, t0:t0 + TBf].rearrange("(k p) t -> p k t", p=128), xnb[b % 2][:], xnb[b % 2], xn_b)
    return nc


def rope_tables(half):
    t = np.arange(half * NTOK, (half + 1) * NTOK)
    row = (t // 64).astype(np.float32)
    col = (t % 64).astype(np.float32)
    inv = (1.0 / (np.float32(10000.0) ** (np.arange(0, 64, 2, dtype=np.float32) / np.float32(64)))).astype(np.float32)
    ang = np.zeros((128, NTOK), np.float32)
    for d in range(128):
        pos = row if d < 64 else col
        ang[d] = pos * inv[d % 32]
    return np.cos(ang).astype(np.float32), np.sin(ang).astype(np.float32)


def make_consts():
    c = np.zeros((128, 640), np.float32)
    c[:, 0:128] = 1.0
    c[:, 128:256] = np.eye(128, dtype=np.float32)
    for m in range(128):
        if m % 64 < 32:
            c[m + 32, 256 + m] = -1.0
        else:
            c[m - 32, 256 + m] = 1.0
    c[:, 384:512] = np.arange(128, dtype=np.float32)[None, :]
    c[:, 512] = EPS
    c[:, 513:529] = np.arange(16, dtype=np.float32)[None, :]
    c[:, 529:545] = 16.0 * (np.arange(16, dtype=np.float32)[None, :] + 1.0)
    return c


def col8(v):
    return np.ascontiguousarray(v.reshape(-1, 128).T)


def make_vecs(inp, l, b):
    v = np.zeros((128, NV), np.float32)
    v[:, 0:8] = col8(inp["c"][b])
    v[:, 8:56] = col8(inp["ada_b"][l])
    v[:, 56:64] = col8(inp["norm_mix_w"][l])
    v[:, 64:72] = col8(inp["norm_ffn_w"][l])
    v[:, 72:80] = col8(inp["final_norm_w"])
    v[:, 80] = inp["q_norm_w"][l]
    v[:, 81] = inp["k_norm_w"][l]
    v[:, 82:86] = col8(inp["pool_scale"][l])
    return v


def make_vecs_F(inp, l, b, half):
    v = make_vecs(inp, l, b)
    v[:, 86] = 1.0 if half == 1 else 0.0
    v[:, 87] = 1.0 if half == 0 else 0.0
    return v


_PROGS = {}


def get_prog(name, **kw):
    key = (name, tuple(sorted(kw.items())))
    if key not in _PROGS:
        _PROGS[key] = {"A": build_A, "B": build_B, "F": build_F}[name](**kw)
    return _PROGS[key]


def run_A(inp, l, xTs):
    consts = make_consts()
    maps = []
    for c in range(NCORES):
        b, half = c // 2, c % 2
        cs, sn = rope_tables(half)
        maps.append({"xT": xTs[c], "vecs": make_vecs(inp, l, b), "consts": consts,
                     "ada_w": np.ascontiguousarray(inp["ada_w"][l]), "w_in": np.ascontiguousarray(inp["w_in"][l]),
                     "cosT": cs, "sinT": sn})
    res = run_bass_kernel_spmd(get_prog("A"), maps, core_ids=list(range(NCORES)))
    return res.results


def invcnt_table(half):
    t = np.arange(half * NTOK, (half + 1) * NTOK)
    out = np.zeros((4, NTOK), np.float32)
    for gi, w in enumerate((2, 4, 8, 16)):
        lo = np.clip(t - w // 2, 0, SEQ)
        hi = np.clip(t + w // 2, 0, SEQ)
        out[gi] = (1.0 / (hi - lo).astype(np.float32)).astype(np.float32)
    return np.ascontiguousarray(np.broadcast_to(out.reshape(1, 4 * NTOK), (128, 4 * NTOK)))


def run_B(inp, l, xTs, ra, **kw):
    consts = make_consts()
    ada_w = np.ascontiguousarray(inp["ada_w"][l])
    w_in = np.ascontiguousarray(inp["w_in"][l])
    wao = np.ascontiguousarray(inp["w_attn_o"][l])
    wpo = np.ascontiguousarray(inp["w_pool_o"][l])
    wout = np.ascontiguousarray(inp["w_out"][l])
    mixw = np.ascontiguousarray(inp["pool_mix_w"][l].reshape(512, 128))
    wq = np.ascontiguousarray(inp["w_query"][l])
    skt = np.ascontiguousarray(inp["sub_keys"][l].reshape(8, 2, 128, 2, 128).transpose(0, 1, 3, 4, 2).reshape(4096, 128))
    ut = np.ascontiguousarray(inp["expert_u"][l].T)
    ev = np.ascontiguousarray(inp["expert_v"][l])
    maps = []
    for c in range(NCORES):
        b, half = c // 2, c % 2
        p = c ^ 1
        lo, hi = (c, p) if half == 0 else (p, c)
        cs, sn = rope_tables(half)
        kTf = np.ascontiguousarray(np.concatenate([ra[lo]["kT"], ra[hi]["kT"]], axis=1))
        vf = np.ascontiguousarray(np.concatenate([ra[lo]["v"], ra[hi]["v"]], axis=0))
        uh = np.zeros((512, 16), np.float32)
        if half == 1:
            uh[:, 0:8] = ra[p]["uh"][:, 8:16]
        else:
            uh[:, 8:16] = ra[p]["uh"][:, 0:8]
        maps.append({"xT": xTs[c], "vecs": make_vecs(inp, l, b), "consts": consts, "ada_w": ada_w, "w_in": w_in,
                     "cosT": cs, "sinT": sn, "kTf": kTf, "vf": vf, "uhalo": uh, "invcnt": invcnt_table(half),
                     "w_attn_o": wao, "w_pool_o": wpo, "w_out": wout, "pool_mix_w": mixw, "w_query": wq, "skt": skt,
                     "expert_uT": ut, "expert_v": ev})
    res = run_bass_kernel_spmd(get_prog("B", **kw), maps, core_ids=list(range(NCORES)))
    return res.results


def layer_weights(inp, l):
    return {
        "ada_w%d" % l: np.ascontiguousarray(inp["ada_w"][l]),
        "w_in%d" % l: np.ascontiguousarray(inp["w_in"][l]),
        "w_attn_o%d" % l: np.ascontiguousarray(inp["w_attn_o"][l]),
        "w_pool_o%d" % l: np.ascontiguousarray(inp["w_pool_o"][l]),
        "w_out%d" % l: np.ascontiguousarray(inp["w_out"][l]),
        "pool_mix_w%d" % l: np.ascontiguousarray(inp["pool_mix_w"][l].reshape(512, 128)),
        "w_query%d" % l: np.ascontiguousarray(inp["w_query"][l]),
        "skt%d" % l: np.ascontiguousarray(inp["sub_keys"][l].reshape(8, 2, 128, 2, 128).transpose(0, 1, 3, 4, 2).reshape(4096, 128)),
        "expert_uT%d" % l: np.ascontiguousarray(inp["expert_u"][l].T),
        "expert_v%d" % l: np.ascontiguousarray(inp["expert_v"][l]),
    }


def kernel(**inputs):
    inp = {k: np.asarray(v) for k, v in inputs.items()}
    x = inp["x"]
    consts = make_consts()
    shared = {}
    for l in range(2):
        shared.update(layer_weights(inp, l))
    maps = []
    for c in range(NCORES):
        b, half = c // 2, c % 2
        cs, sn = rope_tables(half)
        m = {"xT": np.ascontiguousarray(x[b, half * NTOK:(half + 1) * NTOK, :].T), "consts": consts, "cosT": cs, "sinT": sn,
             "invcnt": invcnt_table(half), "vecs0": make_vecs_F(inp, 0, b, half), "vecs1": make_vecs_F(inp, 1, b, half)}
        m.update(shared)
        maps.append(m)
    res = run_bass_kernel_spmd(get_prog("F"), maps, core_ids=list(range(NCORES)))
    out = np.zeros((4, SEQ, D), np.float32)
    for c in range(NCORES):
        out[c // 2, (c % 2) * NTOK:(c % 2 + 1) * NTOK, :] = res.results[c]["xTn_out"].T
    return out
```
